# Optimizing a Trainium2 kernel written in Bass

```python
import jax, jax.numpy as jnp
from jax import lax
import numpy as np

D_MODEL = 2048
BATCH = 4
SEQ = 4096
DEPTH = 4

HEAD_DIM = 128
GROUP_HEADS = 4
GROUP_WIDTH = GROUP_HEADS * HEAD_DIM
N_GROUPS = 4
MIX_WIDTH = N_GROUPS * GROUP_WIDTH
Q_BLOCK = 128
MOBA_BLOCK = 256
MOBA_TOP_K = 3
MOBA_Q_CHUNK = 32
MLA_Q_RANK = 512
MLA_KV_RANK = 256
MLA_NOPE_DIM = 128
MLA_ROPE_DIM = 64
MLA_V_DIM = 128
MLA_QK_DIM = MLA_NOPE_DIM + MLA_ROPE_DIM
D_FF = 5632
CONV_WIDTH = 3
ROPE_THETA = 10000.0
EPS = 1e-6
FORGET_BIAS_LO = 1.0
FORGET_BIAS_HI = 4.0
IN_WIDTHS = (GROUP_WIDTH, GROUP_WIDTH, GROUP_WIDTH, GROUP_HEADS,
             GROUP_WIDTH, GROUP_WIDTH, GROUP_WIDTH,
             GROUP_WIDTH, GROUP_WIDTH, GROUP_WIDTH,
             MLA_Q_RANK, MLA_KV_RANK, MLA_ROPE_DIM)
IN_WIDTH = sum(IN_WIDTHS)

kernel_name = "hybrid_fox_stickbreak_moba_mla_convglu"


def rms_norm(x, g):
    xf = x.astype(jnp.float32)
    y = xf * lax.rsqrt(jnp.mean(xf * xf, axis=-1, keepdims=True) + EPS)
    return (y * g.astype(jnp.float32)).astype(x.dtype)


def to_heads(t, n_heads):
    b, s, _ = t.shape
    return t.reshape(b, s, n_heads, -1).transpose(0, 2, 1, 3)


def from_heads(t):
    b, h, s, e = t.shape
    return t.transpose(0, 2, 1, 3).reshape(b, s, h * e)


def rope_angles(seq, dim):
    inv_freq = ROPE_THETA ** (-jnp.arange(0, dim, 2, dtype=jnp.float32) / dim)
    ang = jnp.arange(seq, dtype=jnp.float32)[:, None] * inv_freq[None, :]
    return jnp.cos(ang), jnp.sin(ang)


def apply_rope(t, cos, sin):
    t1, t2 = jnp.split(t, 2, axis=-1)
    c, s = cos.astype(t.dtype), sin.astype(t.dtype)
    return jnp.concatenate([t1 * c - t2 * s, t1 * s + t2 * c], axis=-1)


def sweep_queries(block_fn, seq, chunk):
    out = lax.map(block_fn, jnp.arange(seq // chunk, dtype=jnp.int32) * chunk)
    n, b, h, c, e = out.shape
    return jnp.moveaxis(out, 0, 2).reshape(b, h, n * c, e)


def causal_softmax_attention(q, k, v, cum_log_forget=None):
    seq = q.shape[2]
    scale = q.shape[-1] ** -0.5
    key_pos = jnp.arange(seq)

    def block(start):
        qb = lax.dynamic_slice_in_dim(q, start, Q_BLOCK, axis=2)
        q_pos = start + jnp.arange(Q_BLOCK)
        logits = jnp.einsum("bhqd,bhkd->bhqk", qb, k).astype(jnp.float32) * scale
        if cum_log_forget is not None:
            cq = lax.dynamic_slice_in_dim(cum_log_forget, start, Q_BLOCK, axis=2)
            logits = logits + (cq[..., :, None] - cum_log_forget[..., None, :])
        logits = jnp.where(key_pos[None, :] <= q_pos[:, None], logits, -jnp.inf)
        p = jax.nn.softmax(logits, axis=-1)
        return jnp.einsum("bhqk,bhkd->bhqd", p.astype(v.dtype), v)

    return sweep_queries(block, seq, Q_BLOCK)


def stick_breaking_attention(q, k, v):
    seq = q.shape[2]
    scale = q.shape[-1] ** -0.5
    key_pos = jnp.arange(seq)

    def block(start):
        qb = lax.dynamic_slice_in_dim(q, start, Q_BLOCK, axis=2)
        q_pos = start + jnp.arange(Q_BLOCK)
        z = jnp.einsum("bhqd,bhkd->bhqk", qb, k).astype(jnp.float32) * scale
        strict = key_pos[None, :] < q_pos[:, None]
        log_beta = jax.nn.log_sigmoid(z)
        log_rest = jnp.where(strict, log_beta - z, 0.0)
        log_stick = lax.cumsum(log_rest, axis=3, reverse=True) - log_rest
        w = jnp.where(strict, jnp.exp(log_beta + log_stick), 0.0)
        return jnp.einsum("bhqk,bhkd->bhqd", w.astype(v.dtype), v)

    return sweep_queries(block, seq, Q_BLOCK)


def moba_attention(q, k, v):
    b, h, seq, d = q.shape
    scale = d ** -0.5
    n_blocks = -(-seq // MOBA_BLOCK)
    pad = n_blocks * MOBA_BLOCK - seq
    kp = jnp.pad(k, ((0, 0), (0, 0), (0, pad), (0, 0)))
    vp = jnp.pad(v, ((0, 0), (0, 0), (0, pad), (0, 0)))
    k_blocks = kp.reshape(b, h, n_blocks, MOBA_BLOCK, d)
    v_blocks = vp.reshape(b, h, n_blocks, MOBA_BLOCK, d)
    k_mean = jnp.mean(k_blocks.astype(jnp.float32), axis=3).astype(k.dtype)
    n_sel = min(MOBA_TOP_K, n_blocks)
    b_ix = jnp.arange(b)[:, None, None, None]
    h_ix = jnp.arange(h)[None, :, None, None]
    block_ids = jnp.arange(n_blocks)

    def chunk(start):
        qc = lax.dynamic_slice_in_dim(q, start, MOBA_Q_CHUNK, axis=2)
        q_pos = start + jnp.arange(MOBA_Q_CHUNK)
        own = start // MOBA_BLOCK
        gate = jnp.einsum("bhcd,bhnd->bhcn", qc, k_mean).astype(jnp.float32)
        gate = jnp.where(block_ids < own, gate, -jnp.inf)
        _, sel = lax.top_k(gate, n_sel)
        sel_valid = jnp.arange(n_sel) < own
        k_sel = k_blocks[b_ix, h_ix, sel]
        v_sel = v_blocks[b_ix, h_ix, sel]
        s_sel = jnp.einsum("bhcd,bhcnkd->bhcnk", qc, k_sel).astype(jnp.float32) * scale
        s_sel = jnp.where(sel_valid[:, None], s_sel, -jnp.inf)
        s_sel = s_sel.reshape(b, h, MOBA_Q_CHUNK, n_sel * MOBA_BLOCK)
        k_own = lax.dynamic_slice_in_dim(kp, own * MOBA_BLOCK, MOBA_BLOCK, axis=2)
        v_own = lax.dynamic_slice_in_dim(vp, own * MOBA_BLOCK, MOBA_BLOCK, axis=2)
        s_own = jnp.einsum("bhcd,bhkd->bhck", qc, k_own).astype(jnp.float32) * scale
        own_pos = own * MOBA_BLOCK + jnp.arange(MOBA_BLOCK)
        s_own = jnp.where(own_pos[None, :] <= q_pos[:, None], s_own, -jnp.inf)
        p = jax.nn.softmax(jnp.concatenate([s_sel, s_own], axis=-1), axis=-1)
        p_sel = p[..., :n_sel * MOBA_BLOCK].reshape(b, h, MOBA_Q_CHUNK, n_sel, MOBA_BLOCK)
        p_own = p[..., n_sel * MOBA_BLOCK:]
        return (jnp.einsum("bhcnk,bhcnkd->bhcd", p_sel.astype(v.dtype), v_sel)
                + jnp.einsum("bhck,bhkd->bhcd", p_own.astype(v.dtype), v_own))

    return sweep_queries(chunk, seq, MOBA_Q_CHUNK)


def mla_attention(c_q, c_kv, k_rope, cq_norm, ckv_norm, w_uq, w_ukv, q_norm, k_norm, cos, sin):
    b, s, _ = c_q.shape
    q = (rms_norm(c_q, cq_norm) @ w_uq).reshape(b, s, GROUP_HEADS, MLA_QK_DIM)
    kv = (rms_norm(c_kv, ckv_norm) @ w_ukv).reshape(b, s, GROUP_HEADS, MLA_NOPE_DIM + MLA_V_DIM)
    k_nope, v = jnp.split(kv, [MLA_NOPE_DIM], axis=-1)
    k_r = jnp.broadcast_to(k_rope[:, :, None, :], (b, s, GROUP_HEADS, MLA_ROPE_DIM))
    k = jnp.concatenate([k_nope, k_r], axis=-1)
    q = rms_norm(q, q_norm).transpose(0, 2, 1, 3)
    k = rms_norm(k, k_norm).transpose(0, 2, 1, 3)
    q = jnp.concatenate([q[..., :MLA_NOPE_DIM], apply_rope(q[..., MLA_NOPE_DIM:], cos, sin)], axis=-1)
    k = jnp.concatenate([k[..., :MLA_NOPE_DIM], apply_rope(k[..., MLA_NOPE_DIM:], cos, sin)], axis=-1)
    return causal_softmax_attention(q, k, v.transpose(0, 2, 1, 3))


def causal_depthwise_conv(u, w, bias):
    seq = u.shape[1]
    up = jnp.pad(u, ((0, 0), (CONV_WIDTH - 1, 0), (0, 0)))
    out = bias.astype(u.dtype) + up[:, 0:seq] * w[0]
    for j in range(1, CONV_WIDTH):
        out = out + up[:, j:j + seq] * w[j]
    return out


def mixer_sublayer(x, rope_c, rope_r, attn_norm, w_in, b_forget, fox_q_norm, fox_k_norm,
                   moba_q_norm, moba_k_norm, mla_cq_norm, mla_ckv_norm, w_uq, w_ukv,
                   mla_q_norm, mla_k_norm, mix_out_norm, w_out):
    b, s, _ = x.shape
    h = rms_norm(x, attn_norm)
    proj = h @ w_in
    offsets = np.cumsum(IN_WIDTHS)[:-1].tolist()
    (a_q, a_k, a_v, a_f, b_q, b_k, b_v, c_q, c_k, c_v,
     d_cq, d_ckv, d_kr) = jnp.split(proj, offsets, axis=-1)

    log_f = jax.nn.log_sigmoid((a_f + b_forget).astype(jnp.float32))
    cum_log_f = jnp.cumsum(log_f, axis=1).transpose(0, 2, 1)
    qa = rms_norm(to_heads(a_q, GROUP_HEADS), fox_q_norm)
    ka = rms_norm(to_heads(a_k, GROUP_HEADS), fox_k_norm)
    o_a = causal_softmax_attention(qa, ka, to_heads(a_v, GROUP_HEADS), cum_log_f)

    o_b = stick_breaking_attention(to_heads(b_q, GROUP_HEADS), to_heads(b_k, GROUP_HEADS),
                                   to_heads(b_v, GROUP_HEADS))

    cos_c, sin_c = rope_c
    qc = apply_rope(rms_norm(to_heads(c_q, GROUP_HEADS), moba_q_norm), cos_c, sin_c)
    kc = apply_rope(rms_norm(to_heads(c_k, GROUP_HEADS), moba_k_norm), cos_c, sin_c)
    o_c = moba_attention(qc, kc, to_heads(c_v, GROUP_HEADS))

    cos_r, sin_r = rope_r
    o_d = mla_attention(d_cq, d_ckv, d_kr, mla_cq_norm, mla_ckv_norm, w_uq, w_ukv,
                        mla_q_norm, mla_k_norm, cos_r, sin_r)

    y = jnp.stack([from_heads(o_a), from_heads(o_b), from_heads(o_c), from_heads(o_d)], axis=2)
    y = rms_norm(y, mix_out_norm.reshape(N_GROUPS, GROUP_WIDTH))
    return x + y.reshape(b, s, MIX_WIDTH) @ w_out


def ffn_sublayer(x, ffn_norm, w_up, conv_w, conv_b, w_down):
    h = rms_norm(x, ffn_norm)
    u = causal_depthwise_conv(h @ w_up, conv_w, conv_b)
    gate, val = jnp.split(u, 2, axis=-1)
    return x + (jax.nn.silu(gate) * val) @ w_down


def setup_inputs(seed: int = 0) -> dict:
    key = jax.random.key(seed)
    ks = jax.random.split(key, 21)
    f32 = jnp.float32

    def normal(k, shape, scale):
        return jax.random.normal(k, shape, f32) * scale

    def gain(k, shape):
        return 1.0 + 0.02 * jax.random.normal(k, shape, f32)

    out_scale = (2 * DEPTH) ** -0.5
    return {
        "x": normal(ks[0], (BATCH, SEQ, D_MODEL), 1.0),
        "attn_norm": gain(ks[1], (DEPTH, D_MODEL)),
        "w_in": normal(ks[2], (DEPTH, D_MODEL, IN_WIDTH), D_MODEL ** -0.5),
        "b_forget": jax.random.uniform(ks[3], (DEPTH, GROUP_HEADS), f32, FORGET_BIAS_LO, FORGET_BIAS_HI),
        "fox_q_norm": gain(ks[4], (DEPTH, HEAD_DIM)),
        "fox_k_norm": gain(ks[5], (DEPTH, HEAD_DIM)),
        "moba_q_norm": gain(ks[6], (DEPTH, HEAD_DIM)),
        "moba_k_norm": gain(ks[7], (DEPTH, HEAD_DIM)),
        "mla_cq_norm": gain(ks[8], (DEPTH, MLA_Q_RANK)),
        "mla_ckv_norm": gain(ks[9], (DEPTH, MLA_KV_RANK)),
        "w_uq": normal(ks[10], (DEPTH, MLA_Q_RANK, GROUP_HEADS * MLA_QK_DIM), MLA_Q_RANK ** -0.5),
        "w_ukv": normal(ks[11], (DEPTH, MLA_KV_RANK, GROUP_HEADS * (MLA_NOPE_DIM + MLA_V_DIM)), MLA_KV_RANK ** -0.5),
        "mla_q_norm": gain(ks[12], (DEPTH, MLA_QK_DIM)),
        "mla_k_norm": gain(ks[13], (DEPTH, MLA_QK_DIM)),
        "mix_out_norm": gain(ks[14], (DEPTH, MIX_WIDTH)),
        "w_out": normal(ks[15], (DEPTH, MIX_WIDTH, D_MODEL), MIX_WIDTH ** -0.5 * out_scale),
        "ffn_norm": gain(ks[16], (DEPTH, D_MODEL)),
        "w_up": normal(ks[17], (DEPTH, D_MODEL, 2 * D_FF), D_MODEL ** -0.5),
        "conv_w": normal(ks[18], (DEPTH, CONV_WIDTH, 2 * D_FF), CONV_WIDTH ** -0.5),
        "conv_b": normal(ks[19], (DEPTH, 2 * D_FF), 0.01),
        "w_down": normal(ks[20], (DEPTH, D_FF, D_MODEL), D_FF ** -0.5 * out_scale),
    }


def reference(x, attn_norm, w_in, b_forget, fox_q_norm, fox_k_norm, moba_q_norm, moba_k_norm,
              mla_cq_norm, mla_ckv_norm, w_uq, w_ukv, mla_q_norm, mla_k_norm, mix_out_norm,
              w_out, ffn_norm, w_up, conv_w, conv_b, w_down):
    seq = x.shape[1]
    rope_c = rope_angles(seq, HEAD_DIM)
    rope_r = rope_angles(seq, MLA_ROPE_DIM)
    for l in range(DEPTH):
        x = mixer_sublayer(x, rope_c, rope_r, attn_norm[l], w_in[l], b_forget[l],
                           fox_q_norm[l], fox_k_norm[l], moba_q_norm[l], moba_k_norm[l],
                           mla_cq_norm[l], mla_ckv_norm[l], w_uq[l], w_ukv[l],
                           mla_q_norm[l], mla_k_norm[l], mix_out_norm[l], w_out[l])
        x = ffn_sublayer(x, ffn_norm[l], w_up[l], conv_w[l], conv_b[l], w_down[l])
    return x
```

```python
import contextlib
from functools import partial

import ml_dtypes
import numpy as np

import concourse.bass as bass
import concourse.mybir as mybir
from concourse.bass_utils import run_bass_kernel_spmd

F32 = mybir.dt.float32
BF16 = mybir.dt.bfloat16
AF = mybir.ActivationFunctionType
ALU = mybir.AluOpType
AX = mybir.AxisListType

D = 2048
INW = 5444
DFF = 5632
EPS = 1e-6
TBK = 1024
G1, G2, GM, CW, CB = 0, 16, 32, 48, 312
FOXQ, FOXK, MOBQ, MOBK, CQ, CKV, MQN, MQR, MKN, MKR, BFC = 400, 401, 402, 403, 404, 408, 410, 411, 412, 413, 414
NP = 416
NEG = -30000.0


class Op:
    __slots__ = ("eng", "fn", "deps", "signal", "seq", "dma", "sem", "target", "semkey")


class Sched:
    COMPUTE = ("pe", "act", "dve", "pool")

    def __init__(self, nc, es, nslots=None):
        self.nc = nc
        self.engs = {"pe": nc.tensor, "act": nc.scalar, "dve": nc.vector, "pool": nc.gpsimd, "sp": nc.sync}
        self.ops = []
        self.lastw = {}
        self.readers = {}
        self.last_op = {}
        self.esem = {e: es.enter_context(nc.semaphore(f"s_{e}")) for e in self.COMPUTE}
        nslots = nslots or {"sp": 16, "pool": 8}
        self.dsem = {q: [es.enter_context(nc.semaphore(f"d_{q}{i}")) for i in range(n)] for q, n in nslots.items()}
        self.dcount = {q: [0] * n for q, n in nslots.items()}
        self.dlast = {q: [None] * n for q, n in nslots.items()}
        self.dnext = {q: 0 for q in nslots}
        self.outstanding = []

    def add(self, eng, fn, reads=(), writes=(), dma=False):
        op = Op()
        op.eng, op.fn, op.dma, op.signal, op.seq = eng, fn, dma, False, None
        deps = {}

        def dep(d, war):
            if d is None:
                return
            if (not d.dma) and (not dma) and d.eng == eng:
                if eng == "pe" or war:
                    return
            deps[id(d)] = d

        for k in reads:
            dep(self.lastw.get(k), False)
        for k in writes:
            dep(self.lastw.get(k), False)
            rr = self.readers.get(k)
            if rr:
                for r in rr.values():
                    dep(r, True)
        if dma:
            q = eng
            i = self.dnext[q]
            self.dnext[q] = (i + 1) % len(self.dsem[q])
            prev = self.dlast[q][i]
            if prev is not None:
                deps[id(prev)] = prev
            self.dcount[q][i] += 16
            op.sem, op.target, op.semkey = self.dsem[q][i], self.dcount[q][i], (q, i)
            self.dlast[q][i] = op
            self.outstanding.append(op)
        else:
            self.last_op[eng] = op
        rk = id(op) if dma else eng
        for k in reads:
            self.readers.setdefault(k, {})[rk] = op
        for k in writes:
            self.lastw[k] = op
            self.readers[k] = {}
        op.deps = list(deps.values())
        for d in op.deps:
            if not d.dma:
                d.signal = True
        self.ops.append(op)
        return op

    def barrier(self):
        targets = [op for op in self.last_op.values()] + self.outstanding
        for e in ("pe", "act", "dve", "pool", "sp"):
            b = Op()
            b.eng, b.fn, b.dma, b.signal, b.seq = e, None, False, False, None
            b.deps = [t for t in targets if t.dma or t.eng != e]
            for d in b.deps:
                if not d.dma:
                    d.signal = True
            self.ops.append(b)
        self.outstanding = []
        self.lastw = {}
        self.readers = {}

    def emit(self):
        known = {e: {} for e in self.engs}
        seq = {e: 0 for e in self.COMPUTE}
        nwait = 0
        for op in self.ops:
            E = self.engs[op.eng]
            kn = known[op.eng]
            need = {}
            for d in op.deps:
                if d.dma:
                    key, sem, val = d.semkey, d.sem, d.target
                else:
                    key, sem, val = d.eng, self.esem[d.eng], d.seq
                if kn.get(key, 0) < val and need.get(key, (None, 0))[1] < val:
                    need[key] = (sem, val)
            for key, (sem, val) in need.items():
                E.wait_ge(sem, val)
                kn[key] = val
                nwait += 1
            if op.fn is None:
                continue
            inst = op.fn()
            if op.dma:
                inst.then_inc(op.sem, 16)
            elif op.signal:
                seq[op.eng] += 1
                op.seq = seq[op.eng]
                inst.then_inc(self.esem[op.eng], 1)
        return nwait


class Arena:
    def __init__(self, t, nbytes):
        self.t, self.n, self.top = t, nbytes, 0

    def alloc(self, shape, dt):
        esz = 4 if dt == F32 else 2
        n = int(np.prod(shape[1:]))
        nb = n * esz
        off = self.top
        self.top += (nb + 63) // 64 * 64
        assert self.top <= self.n, f"arena overflow {self.top} > {self.n}"
        ap = self.t[0:shape[0], off // 2:(off + nb) // 2]
        if dt == F32:
            ap = ap.bitcast(F32)
        if len(shape) == 3:
            ap = ap.rearrange("p (a b) -> p a b", a=shape[1])
        return ap


class Rot:
    def __init__(self, name, bufs):
        self.name, self.bufs, self.i = name, bufs, 0

    def next(self):
        i = self.i % len(self.bufs)
        self.i += 1
        return self.bufs[i], (self.name, i)


def build(S, L, debug=False):
    assert S % TBK == 0
    NTB = S // TBK
    NQC = S // 512
    nc = bass.Bass("TRN2", target_bir_lowering=False)
    es = contextlib.ExitStack()

    def din(name, shape, dt=F32):
        return nc.dram_tensor(name, list(shape), dt, kind="ExternalInput").ap()

    def dscr(name, shape, dt):
        return nc.dram_tensor(name, list(shape), dt, kind=("ExternalOutput" if debug else "Internal")).ap()

    xT = din("xT", [D, S])
    w_in = din("w_in", [L, D, INW])
    w_uq = din("w_uq", [L, 512, 768])
    w_ukv = din("w_ukv", [L, 256, 1024])
    w_out = din("w_out", [L, D, D])
    w_up = din("w_up", [L, D, 2 * DFF])
    w_down = din("w_down", [L, DFF, D])
    pp_d = din("pp", [128, L, NP])
    cbf_d = din("cbf", [128, 768], BF16)
    attm_d = din("attm", [128, 6144], BF16)
    esel_d = din("esel", [16, 2048], BF16)
    bmask_d = din("bmask", [128, 256])
    cosC_d, sinC_d = din("cosC", [128, S]), din("sinC", [128, S])
    cosR_d, sinR_d = din("cosR", [64, S]), din("sinR", [64, S])
    outT = nc.dram_tensor("outT", [D, S], F32, kind="ExternalOutput").ap()

    xA, xB = dscr("xA", [D, S], F32), dscr("xB", [D, S], F32)
    QT, KT = dscr("QT", [16, 128, S], BF16), dscr("KT", [16, 128, S], BF16)
    QR, KR = dscr("QR", [4, 64, S], BF16), dscr("KR", [4, 64, S], BF16)
    VV = dscr("VV", [16, 128, S // 128, 128], BF16)
    FQ, FK = dscr("FQ", [4, 3, S], BF16), dscr("FK", [4, 3, S], BF16)
    OT = dscr("OT", [16, 128, S], F32)

    ARENA_BYTES = 206 * 1024
    arena_t = es.enter_context(nc.sbuf_tensor("arena", [128, ARENA_BYTES // 2], BF16))
    A = Arena(arena_t, ARENA_BYTES)
    ps = [es.enter_context(nc.psum_tensor(f"ps{i}", [128, 512], F32))[:] for i in range(8)]
    SC = Sched(nc, es)
    engs = SC.engs

    def MM(out, lhsT, rhs, start, stop, reads, writes):
        SC.add("pe", partial(nc.tensor.matmul, out, lhsT=lhsT, rhs=rhs, start=start, stop=stop), reads, writes)

    def ACT(out, in_, func, reads, writes, bias=None, scale=None):
        kw = {}
        if bias is not None:
            kw["bias"] = bias
        if scale is not None:
            kw["scale"] = scale
        SC.add("act", partial(nc.scalar.activation, out=out, in_=in_, func=func, **kw), reads, writes)

    def STT(out, in0, scalar, in1, op0, op1, reads, writes):
        SC.add("dve", partial(nc.vector.scalar_tensor_tensor, out=out, in0=in0, scalar=scalar, in1=in1, op0=op0, op1=op1),
               reads, writes)

    def TT(out, in0, in1, op, reads, writes, eng="dve"):
        SC.add(eng, partial(engs[eng].tensor_tensor, out=out, in0=in0, in1=in1, op=op), reads, writes)

    def TS(out, in0, s1, s2, op0, op1, reads, writes):
        SC.add("dve", partial(nc.vector.tensor_scalar, out=out, in0=in0, scalar1=s1, scalar2=s2, op0=op0, op1=op1),
               reads, writes)

    def DMA(q, out, in_, reads, writes):
        SC.add(q, partial(engs[q].dma_start, out=out, in_=in_), reads, writes, dma=True)

    def MEMSET(ap, val, writes, eng="dve"):
        SC.add(eng, partial(engs[eng].memset, ap, val), (), writes)

    cbf = A.alloc([128, 768], BF16)
    ident, ones_b, r128T, r64T, negtri, negones = (cbf[:, i * 128:(i + 1) * 128] for i in range(6))
    ppt = A.alloc([128, L, NP], F32)
    cf = A.alloc([128, 8], F32)
    DMA("sp", cbf, cbf_d, [], ["cbf"])
    DMA("sp", ppt, pp_d, [], ["ppt"])
    cvals = [EPS, -8.0, float(np.log(128.0 ** -0.5)), float(np.log(192.0 ** -0.5)), 1.0, 0.0]
    for i, v in enumerate(cvals):
        MEMSET(cf[:, i:i + 1], v, ["cf"])
    c_eps, c_m8, c_ls128, c_ls192, c_one = (cf[:, i:i + 1] for i in range(5))
    fcarry = [A.alloc([4, 512], F32) for _ in range(2)]
    ones4 = A.alloc([4, 512], F32)
    MEMSET(ones4, 1.0, ["ones4"])
    mark = A.top
    SC.barrier()

    wctr = [0]
    pmr = Rot("ps", [0, 1, 2, 3])
    par = Rot("ps", [4, 5, 6, 7])

    def psm():
        b, _ = pmr.next()
        return ps[b], ("ps", b)

    def psa():
        b, _ = par.next()
        return ps[b], ("ps", b)

    def norm_pro(src_v, chunks, dst, gains, groups, Dg):
        XS = [A.alloc([128, 16, 256], F32) for _ in range(2)]
        SQ = A.alloc([128, 16, 256], BF16)
        LNV = [A.alloc([128, 256], F32) for _ in groups]
        RSTD = [A.alloc([128, 256], F32) for _ in groups]
        for i, (ts, n, dc) in enumerate(chunks):
            b = i % 2
            xs = XS[b]
            DMA("sp", xs[:, :, 0:n], src_v[:, :, ts:ts + n], [], [("xs", b)])
            ACT(SQ[:, :, 0:n], xs[:, :, 0:n], AF.Square, [("xs", b)], ["sq"])
            for gi, (c0, c1) in enumerate(groups):
                pb = 4 + gi
                for c in range(c0, c1):
                    MM(ps[pb][:, 0:n], ones_b, SQ[:, c, 0:n], c == c0, c == c1 - 1, ["sq", "cbf"], [("ps", pb)])
                ACT(LNV[gi][:, 0:n], ps[pb][:, 0:n], AF.Ln, [("ps", pb), "cf"], [("lnv", gi)], bias=c_eps, scale=1.0 / Dg)
                ACT(RSTD[gi][:, 0:n], LNV[gi][:, 0:n], AF.Exp, [("lnv", gi)], [("rstd", gi)], scale=-0.5)
                for c in range(c0, c1):
                    STT(dst[:, c, dc:dc + n], xs[:, c, 0:n], gains[:, c:c + 1], RSTD[gi][:, 0:n], ALU.mult, ALU.mult,
                        [("xs", b), ("rstd", gi), "ppt"], ["actT"])

    def n1_block(l, tb, xin):
        t0 = tb * TBK
        A.top = mark
        P = ppt[:, l, :]
        actT = A.alloc([128, 16, TBK], BF16)
        wb = [A.alloc([128, 12288], BF16) for _ in range(2)]
        wuq = A.alloc([128, 4, 768], BF16)
        wukv = A.alloc([128, 2, 1024], BF16)
        m2 = A.top
        SC.barrier()
        DMA("pool", wuq, w_uq[l].rearrange("(c p) n -> p c n", p=128), [], ["wuq"])
        DMA("pool", wukv, w_ukv[l].rearrange("(c p) n -> p c n", p=128), [], ["wukv"])
        xin_v = xin.rearrange("(c p) t -> p c t", p=128)
        norm_pro(xin_v, [(t0 + i * 256, 256, i * 256) for i in range(4)], actT, P[:, G1:G1 + 16], [(0, 16)], float(D))
        SC.barrier()
        A.top = m2
        def rot(name, shape, dt, n=2):
            return Rot(name, [A.alloc(shape, dt) for _ in range(n)])
        sqh = rot("sqh", [128, 512], BF16)
        lnv = rot("lnv", [128, 512], F32)
        rstd = rot("rstd", [128, 512], F32)
        xn = rot("xn", [128, 512], F32)
        xnb = rot("xnb", [128, 512], BF16)
        t1 = rot("t1", [128, 512], F32)
        t2 = rot("t2", [128, 512], F32)
        cosb = rot("cosb", [128, 512], F32)
        sinb = rot("sinb", [128, 512], F32)
        outb = rot("outb", [128, 512], BF16, 3)
        vout = rot("vout", [128, 512], BF16)
        fx = rot("fx", [4, 512], F32, 3)
        fqk = rot("fqk", [4, 6, 512], BF16)
        rawcq = A.alloc([128, 4, 512], F32)
        cqn = A.alloc([128, 4, TBK], BF16)
        ckvn = A.alloc([128, 2, TBK], BF16)
        krraw = A.alloc([64, TBK], F32)
        sqkr = A.alloc([64, 512], BF16)

        def wload(col0, ncols):
            i = wctr[0] % 2
            wctr[0] += 1
            v = wb[i][:, 0:16 * ncols].rearrange("p (c n) -> p c n", c=16)
            DMA("pool", v, w_in[l, :, col0:col0 + ncols].rearrange("(c p) n -> p c n", p=128), [], [("wb", i)])
            return v, ("wb", i)

        def rstd_of(ssp, ssk, Dn, lnscale=None):
            lv, lk = lnv.next()
            ACT(lv, ssp, AF.Ln, [ssk, "cf"], [lk], bias=c_eps, scale=1.0 / Dn)
            rs, rk = rstd.next()
            if lnscale is None:
                ACT(rs, lv, AF.Exp, [lk], [rk], scale=-0.5)
            else:
                ACT(rs, lv, AF.Exp, [lk, "cf"], [rk], scale=-0.5, bias=lnscale)
            return rs, rk

        def rope_out(xn_t, xn_k, np_, tok, cos_d, sin_d, rT, dst):
            xb, xbk = xnb.next()
            ACT(xb[0:np_], xn_t, AF.Identity, [xn_k], [xbk])
            rp, rpk = psa()
            MM(rp[0:np_], rT[0:np_, 0:np_], xb[0:np_], True, True, [xbk, "cbf"], [rpk])
            cb, cbk = cosb.next()
            sb, sbk = sinb.next()
            DMA("sp", cb[0:np_], cos_d[:, tok:tok + 512], [], [cbk])
            DMA("sp", sb[0:np_], sin_d[:, tok:tok + 512], [], [sbk])
            a1, a1k = t1.next()
            TT(a1[0:np_], xn_t, cb[0:np_], ALU.mult, [xn_k, cbk], [a1k])
            a2, a2k = t2.next()
            TT(a2[0:np_], rp[0:np_], sb[0:np_], ALU.mult, [rpk, sbk], [a2k])
            ob, obk = outb.next()
            TT(ob[0:np_], a1[0:np_], a2[0:np_], ALU.add, [a1k, a2k], [obk])
            DMA("sp", dst, ob[0:np_], [obk], [])

        def head_fm(pst, psk, tok, kind, gain, lnscale, dst, scale=None):
            if kind == "plain":
                ob, obk = outb.next()
                ACT(ob, pst, AF.Identity, [psk], [obk], scale=scale)
                DMA("sp", dst, ob, [obk], [])
                return
            sq, sqk = sqh.next()
            ACT(sq, pst, AF.Square, [psk], [sqk])
            sp_, spk = psa()
            MM(sp_, ones_b, sq, True, True, [sqk, "cbf"], [spk])
            rs, rk = rstd_of(sp_, spk, 128.0, lnscale)
            if kind == "norm":
                ob, obk = outb.next()
                STT(ob, pst, gain, rs, ALU.mult, ALU.mult, [psk, rk, "ppt"], [obk])
                DMA("sp", dst, ob, [obk], [])
            else:
                x_, xk = xn.next()
                STT(x_, pst, gain, rs, ALU.mult, ALU.mult, [psk, rk, "ppt"], [xk])
                rope_out(x_, xk, 128, tok, cosC_d, sinC_d, r128T, dst)

        def fm_group(col0, kind, gain, lnscale, dstT, hd0, scale=None):
            wv, wk = wload(col0, 512)
            for tc in range(2):
                tok = t0 + tc * 512
                for h in range(4):
                    pt, pk = psm()
                    for c in range(16):
                        MM(pt, wv[:, c, h * 128:(h + 1) * 128], actT[:, c, tc * 512:(tc + 1) * 512], c == 0, c == 15,
                           [wk, "actT"], [pk])
                    head_fm(pt, pk, tok, kind, gain, lnscale, dstT[hd0 + h, :, tok:tok + 512], scale)

        def v_group(col0, ncols, g, fox=False):
            wv, wk = wload(col0, ncols)
            for j in range(TBK // 128):
                pt, pk = psm()
                for c in range(16):
                    MM(pt, actT[:, c, j * 128:(j + 1) * 128], wv[:, c, 0:512], c == 0, c == 15, [wk, "actT"], [pk])
                vo, vk = vout.next()
                ACT(vo, pt, AF.Identity, [pk], [vk])
                jg = (t0 // 128) + j
                DMA("sp", VV[g * 4:(g + 1) * 4, :, jg, :].rearrange("h p d -> p h d"),
                    vo.rearrange("p (h d) -> p h d", h=4), [vk], [])
            if not fox:
                return
            bfv = P[0:4, BFC:BFC + 1]
            for tc in range(2):
                tok = t0 + tc * 512
                ci = tok // 512
                pt, pk = psm()
                for c in range(16):
                    MM(pt[0:4], wv[:, c, 512:516], actT[:, c, tc * 512:(tc + 1) * 512], c == 0, c == 15, [wk, "actT"], [pk])
                e_, ek = fx.next()
                ACT(e_, pt[0:4], AF.Exp, [pk, "ppt"], [ek], bias=bfv)
                s_, sk = fx.next()
                ACT(s_, e_, AF.Ln, [ek, "cf"], [sk], bias=c_one[0:4])
                lf, lfk = fx.next()
                STT(lf, pt[0:4], bfv, s_, ALU.add, ALU.subtract, [pk, sk, "ppt"], [lfk])
                cur, prev = fcarry[ci % 2], fcarry[(ci + 1) % 2]
                init = 0.0 if ci == 0 else prev[:, 511:512]
                SC.add("dve", partial(nc.vector.tensor_tensor_scan, out=cur, data0=ones4, data1=lf, initial=init,
                                      op0=ALU.mult, op1=ALU.add), [lfk, "ones4", ("fc", (ci + 1) % 2)], [("fc", ci % 2)])
                ck = ("fc", ci % 2)
                q6, q6k = fqk.next()
                ACT(q6[:, 0, :], cur, AF.Identity, [ck], [q6k])
                r1, r1k = fx.next()
                TT(r1, cur, q6[:, 0, :], ALU.subtract, [ck, q6k], [r1k])
                ACT(q6[:, 1, :], r1, AF.Identity, [r1k], [q6k])
                r2, r2k = fx.next()
                TT(r2, r1, q6[:, 1, :], ALU.subtract, [r1k, q6k], [r2k])
                ACT(q6[:, 2, :], r2, AF.Identity, [r2k], [q6k])
                ACT(q6[:, 3:6, :], q6[:, 0:3, :], AF.Identity, [q6k], [q6k], scale=-1.0)
                DMA("sp", FQ[:, :, tok:tok + 512], q6[:, 0:3, :], [q6k], [])
                DMA("sp", FK[:, :, tok:tok + 512], q6[:, 3:6, :], [q6k], [])

        fm_group(0, "norm", P[:, FOXQ:FOXQ + 1], c_ls128, QT, 0)
        fm_group(512, "norm", P[:, FOXK:FOXK + 1], None, KT, 0)
        v_group(1024, 516, 0, fox=True)
        fm_group(1540, "plain", None, None, QT, 4, scale=float(128.0 ** -0.5))
        fm_group(2052, "plain", None, None, KT, 4, scale=1.0)
        v_group(2564, 512, 1)
        fm_group(3076, "rope", P[:, MOBQ:MOBQ + 1], c_ls128, QT, 8)
        fm_group(3588, "rope", P[:, MOBK:MOBK + 1], None, KT, 8)
        v_group(4100, 512, 2)

        wv, wk = wload(4612, 512)
        for tc in range(2):
            ss, ssk = psa()
            for c4 in range(4):
                pt, pk = psm()
                for c in range(16):
                    MM(pt, wv[:, c, c4 * 128:(c4 + 1) * 128], actT[:, c, tc * 512:(tc + 1) * 512], c == 0, c == 15,
                       [wk, "actT"], [pk])
                ACT(rawcq[:, c4, :], pt, AF.Identity, [pk], [("rawcq", c4)])
                sq, sqk = sqh.next()
                ACT(sq, pt, AF.Square, [pk], [sqk])
                MM(ss, ones_b, sq, c4 == 0, c4 == 3, [sqk, "cbf"], [ssk])
            rs, rk = rstd_of(ss, ssk, 512.0)
            for c4 in range(4):
                STT(cqn[:, c4, tc * 512:(tc + 1) * 512], rawcq[:, c4, :], P[:, CQ + c4:CQ + c4 + 1], rs, ALU.mult, ALU.mult,
                    [("rawcq", c4), rk, "ppt"], [("cqn", tc)])
        wv, wk = wload(5124, 320)
        for tc in range(2):
            ss, ssk = psa()
            for c2 in range(2):
                pt, pk = psm()
                for c in range(16):
                    MM(pt, wv[:, c, c2 * 128:(c2 + 1) * 128], actT[:, c, tc * 512:(tc + 1) * 512], c == 0, c == 15,
                       [wk, "actT"], [pk])
                ACT(rawcq[:, c2, :], pt, AF.Identity, [pk], [("rawcq", c2)])
                sq, sqk = sqh.next()
                ACT(sq, pt, AF.Square, [pk], [sqk])
                MM(ss, ones_b, sq, c2 == 0, c2 == 1, [sqk, "cbf"], [ssk])
            rs, rk = rstd_of(ss, ssk, 256.0)
            for c2 in range(2):
                STT(ckvn[:, c2, tc * 512:(tc + 1) * 512], rawcq[:, c2, :], P[:, CKV + c2:CKV + c2 + 1], rs, ALU.mult, ALU.mult,
                    [("rawcq", c2), rk, "ppt"], [("ckvn", tc)])
            pt, pk = psm()
            for c in range(16):
                MM(pt[0:64], wv[:, c, 256:320], actT[:, c, tc * 512:(tc + 1) * 512], c == 0, c == 15, [wk, "actT"], [pk])
            ACT(krraw[:, tc * 512:(tc + 1) * 512], pt[0:64], AF.Identity, [pk], [("krraw", tc)])
        for tc in range(2):
            tok = t0 + tc * 512
            tsl = slice(tc * 512, (tc + 1) * 512)
            ACT(sqkr, krraw[:, tsl], AF.Square, [("krraw", tc)], ["sqkr"])
            for h in range(4):
                pn, pnk = psm()
                for c4 in range(4):
                    MM(pn, wuq[:, c4, h * 192:h * 192 + 128], cqn[:, c4, tsl], c4 == 0, c4 == 3, ["wuq", ("cqn", tc)], [pnk])
                pr, prk = psm()
                for c4 in range(4):
                    MM(pr[0:64], wuq[:, c4, h * 192 + 128:h * 192 + 192], cqn[:, c4, tsl], c4 == 0, c4 == 3,
                       ["wuq", ("cqn", tc)], [prk])
                sq, sqk = sqh.next()
                ACT(sq, pn, AF.Square, [pnk], [sqk])
                sq2, sq2k = sqh.next()
                ACT(sq2[0:64], pr[0:64], AF.Square, [prk], [sq2k])
                ss, ssk = psa()
                MM(ss, ones_b, sq, True, False, [sqk, "cbf"], [ssk])
                MM(ss, ones_b[0:64, :], sq2[0:64], False, True, [sq2k, "cbf"], [ssk])
                rs, rk = rstd_of(ss, ssk, 192.0, c_ls192)
                ob, obk = outb.next()
                STT(ob, pn, P[:, MQN:MQN + 1], rs, ALU.mult, ALU.mult, [pnk, rk, "ppt"], [obk])
                DMA("sp", QT[12 + h, :, tok:tok + 512], ob, [obk], [])
                x_, xk = xn.next()
                STT(x_[0:64], pr[0:64], P[0:64, MQR:MQR + 1], rs[0:64], ALU.mult, ALU.mult, [prk, rk, "ppt"], [xk])
                rope_out(x_[0:64], xk, 64, tok, cosR_d, sinR_d, r64T, QR[h, :, tok:tok + 512])
                pn, pnk = psm()
                for c2 in range(2):
                    MM(pn, wukv[:, c2, h * 256:h * 256 + 128], ckvn[:, c2, tsl], c2 == 0, c2 == 1, ["wukv", ("ckvn", tc)], [pnk])
                sq, sqk = sqh.next()
                ACT(sq, pn, AF.Square, [pnk], [sqk])
                ss, ssk = psa()
                MM(ss, ones_b, sq, True, False, [sqk, "cbf"], [ssk])
                MM(ss, ones_b[0:64, :], sqkr, False, True, ["sqkr", "cbf"], [ssk])
                rs, rk = rstd_of(ss, ssk, 192.0, None)
                ob, obk = outb.next()
                STT(ob, pn, P[:, MKN:MKN + 1], rs, ALU.mult, ALU.mult, [pnk, rk, "ppt"], [obk])
                DMA("sp", KT[12 + h, :, tok:tok + 512], ob, [obk], [])
                x_, xk = xn.next()
                STT(x_[0:64], krraw[:, tsl], P[0:64, MKR:MKR + 1], rs[0:64], ALU.mult, ALU.mult, [("krraw", tc), rk, "ppt"], [xk])
                rope_out(x_[0:64], xk, 64, tok, cosR_d, sinR_d, r64T, KR[h, :, tok:tok + 512])
            for j in range(4):
                pt, pk = psm()
                jl = tc * 4 + j
                for c2 in range(2):
                    rhs = wukv[:, c2, :].rearrange("p (h x) -> p h x", h=4)[:, :, 128:256]
                    MM(pt, ckvn[:, c2, jl * 128:(jl + 1) * 128], rhs, c2 == 0, c2 == 1, ["wukv", ("ckvn", tc)], [pk])
                vo, vk = vout.next()
                ACT(vo, pt, AF.Identity, [pk], [vk])
                jg = (t0 // 128) + jl
                DMA("sp", VV[12:16, :, jg, :].rearrange("h p d -> p h d"), vo.rearrange("p (h d) -> p h d", h=4), [vk], [])

    def att_phase(l):
        A.top = mark
        SC.barrier()
        attm = A.alloc([128, 12, 512], BF16)
        esel = A.alloc([16, 16, 128], BF16)
        bmask = A.alloc([128, 16, 16], F32)
        DMA("sp", attm, attm_d.rearrange("p (a b) -> p a b", a=12), [], ["attm"])
        DMA("sp", esel, esel_d.rearrange("p (a b) -> p a b", a=16), [], ["esel"])
        DMA("sp", bmask, bmask_d.rearrange("p (a b) -> p a b", a=16), [], ["bmask"])
        Mc = [attm[:, j, :] for j in range(4)]
        Ms = [attm[:, 4 + j, :] for j in range(4)]
        M01 = [attm[:, 8 + j, :] for j in range(4)]
        NJ = S // 128
        qt = Rot("qt", [A.alloc([128, S], BF16) for _ in range(2)])
        kt = Rot("kt", [A.alloc([128, S], BF16) for _ in range(2)])
        vv = Rot("vv", [A.alloc([128, NJ, 128], BF16) for _ in range(2)])
        qr = A.alloc([64, S], BF16)
        kr = A.alloc([64, S], BF16)
        kb = A.alloc([6, S], BF16)
        qb = A.alloc([6, S], BF16)
        mbT = A.alloc([16, S], BF16)
        pT = Rot("pT", [A.alloc([128, 512], BF16) for _ in range(3)])
        ef = Rot("ef", [A.alloc([128, 512], F32) for _ in range(2)])
        spb = Rot("spb", [A.alloc([128, 512], BF16) for _ in range(2)])
        spacc = A.alloc([128, 512], BF16)
        ost = Rot("ost", [A.alloc([128, 512], F32) for _ in range(2)])
        rl = Rot("rl", [A.alloc([128, 512], F32) for _ in range(2)])
        ksum = A.alloc([128, 16], F32)
        kmb = A.alloc([128, 16], BF16)
        gm = Rot("gm", [A.alloc([128, 16], F32) for _ in range(2)])
        m8 = Rot("m8", [A.alloc([128, 8], F32) for _ in range(2)])
        mbt = Rot("mbt", [A.alloc([128, 16], BF16) for _ in range(2)])
        sbank = Rot("ps", [0, 1, 6, 7])
        obank = Rot("ps", [2, 3])
        lbank = Rot("ps", [4, 5])
        zbank = Rot("ps", [0, 1])
        xbank = Rot("ps", [6, 7])

        for hd in range(16):
            g, h = hd // 4, hd % 4
            q_, qk_ = qt.next()
            k_, kk_ = kt.next()
            v_, vk_ = vv.next()
            DMA("sp", q_, QT[hd], [], [qk_])
            DMA("sp", k_, KT[hd], [], [kk_])
            DMA("sp", v_, VV[hd], [], [vk_])
            if g == 3:
                DMA("sp", qr, QR[h], [], ["qr"])
                DMA("sp", kr, KR[h], [], ["kr"])
            if g == 0:
                MEMSET(kb, 1.0, ["kb"])
                MEMSET(qb, 1.0, ["qb"])
                DMA("sp", kb[0:3, :], FK[h], [], ["kb"])
                DMA("sp", qb[3:6, :], FQ[h], [], ["qb"])
            if g == 2:
                SC.add("dve", partial(nc.vector.tensor_reduce, out=ksum[:, 0:S // 256],
                                      in_=k_.rearrange("p (n k) -> p n k", k=256), axis=AX.X, op=ALU.add), [kk_], ["ksum"])
                ACT(kmb[:, 0:S // 256], ksum[:, 0:S // 256], AF.Identity, ["ksum"], ["kmb"], scale=1.0 / 256.0)
                NBK = S // 256
                for j in range(NJ):
                    own = j // 2
                    gb, gk = lbank.next()
                    MM(ps[gb][:, 0:NBK], q_[:, j * 128:(j + 1) * 128], kmb[:, 0:NBK], True, True, [qk_, "kmb"], [("ps", gb)])
                    g_, gmk = gm.next()
                    if NBK < 16:
                        MEMSET(g_, -1e30, [gmk])
                    TT(g_[:, 0:NBK], ps[gb][:, 0:NBK], bmask[:, own, 0:NBK], ALU.add, [("ps", gb), "bmask"], [gmk])
                    m_, mk = m8.next()
                    SC.add("dve", partial(nc.vector.max, out=m_, in_=g_), [gmk], [mk])
                    b_, bk = mbt.next()
                    TS(b_, g_, m_[:, 2:3], 1.0, ALU.is_ge, ALU.subtract, [gmk, mk], [bk])
                    tb_, _ = lbank.next()
                    MM(ps[tb_][0:16, 0:128], b_, ident, True, True, [bk, "cbf"], [("ps", tb_)])
                    ACT(mbT[:, j * 128:(j + 1) * 128], ps[tb_][0:16, 0:128], AF.Identity, [("ps", tb_)], ["mbT"])

            for qc in range(NQC):
                qs = slice(qc * 512, (qc + 1) * 512)
                ob_, _ = obank.next()
                ops_, opk = ps[ob_], ("ps", ob_)
                if g != 1:
                    lb_, _ = lbank.next()
                    lps, lpk = ps[lb_], ("ps", lb_)
                    tiles = [(kt_i, None) for kt_i in range(4 * qc)] + [(4 * qc + j, j) for j in range(4)]
                    for ti, (kti, dj) in enumerate(tiles):
                        ks = slice(kti * 128, (kti + 1) * 128)
                        sb_, _ = sbank.next()
                        sps, spk = ps[sb_], ("ps", sb_)
                        extra = []
                        if g == 0:
                            extra.append((kb[0:6, ks], qb[0:6, qs], ["kb", "qb"], None))
                        if g == 3:
                            extra.append((kr[:, ks], qr[:, qs], ["kr", "qr"], None))
                        if g == 2:
                            n = kti // 2
                            if dj is None:
                                extra.append((esel[:, n, :], mbT[:, qs], ["esel", "mbT"], None))
                            elif dj < 2:
                                extra.append((esel[:, n, :], mbT[:, qc * 512 + 256:(qc + 1) * 512], ["esel", "mbT"], (256, 512)))
                        if dj is not None:
                            if g == 2 and dj < 2:
                                extra.append((ident, Mc[dj][:, 0:256], ["cbf", "attm"], (0, 256)))
                            else:
                                extra.append((ident, Mc[dj], ["cbf", "attm"], None))
                        MM(sps, k_[:, ks], q_[:, qs], True, len(extra) == 0, [kk_, qk_], [spk])
                        for ei, (lt, rh, rk_, cr) in enumerate(extra):
                            o_ = sps if cr is None else sps[:, cr[0]:cr[1]]
                            MM(o_, lt, rh, False, ei == len(extra) - 1, rk_, [spk])
                        p_, pk_ = pT.next()
                        ACT(p_, sps, AF.Exp, [spk, "cf"], [pk_], bias=c_m8)
                        MM(ops_, v_[:, kti, :], p_, ti == 0, ti == len(tiles) - 1, [vk_, pk_], [opk])
                        MM(lps, ones_b, p_, ti == 0, ti == len(tiles) - 1, ["cbf", pk_], [lpk])
                    r_, rk2 = rl.next()
                    SC.add("dve", partial(nc.vector.reciprocal, out=r_, in_=lps), [lpk], [rk2])
                    o_, ok_ = ost.next()
                    TT(o_, ops_, r_, ALU.mult, [opk, rk2], [ok_])
                    DMA("sp", OT[hd, :, qs], o_, [ok_], [])
                else:
                    tiles = [(4 * qc + j, j) for j in (3, 2, 1, 0)] + [(kti, None) for kti in range(4 * qc - 1, -1, -1)]
                    for ti, (kti, dj) in enumerate(tiles):
                        ks = slice(kti * 128, (kti + 1) * 128)
                        zb, _ = zbank.next()
                        zps, zk = ps[zb], ("ps", zb)
                        MM(zps, k_[:, ks], q_[:, qs], True, True, [kk_, qk_], [zk])
                        e_, ek = ef.next()
                        ACT(e_, zps, AF.Exp, [zk], [ek])
                        s_, sk = spb.next()
                        ACT(s_, e_, AF.Ln, [ek, "cf"], [sk], bias=c_one)
                        if dj is not None:
                            TT(s_, s_, M01[dj], ALU.mult, [sk, "attm"], [sk])
                        xb_, _ = xbank.next()
                        xps, xk = ps[xb_], ("ps", xb_)
                        MM(xps, k_[:, ks], q_[:, qs], True, False, [kk_, qk_], [xk])
                        MM(xps, negtri, s_, False, False, ["cbf", sk], [xk])
                        if ti > 0:
                            MM(xps, negones, spacc, False, dj is None, ["cbf", "spacc"], [xk])
                        if dj is not None:
                            MM(xps, ident, Ms[dj], False, True, ["cbf", "attm"], [xk])
                        w_, wk_ = pT.next()
                        ACT(w_, xps, AF.Exp, [xk], [wk_])
                        MM(ops_, v_[:, kti, :], w_, ti == 0, ti == len(tiles) - 1, [vk_, wk_], [opk])
                        if ti == 0:
                            SC.add("dve", partial(nc.vector.tensor_copy, out=spacc, in_=s_), [sk], ["spacc"])
                        elif ti < len(tiles) - 1:
                            TT(spacc, spacc, s_, ALU.add, ["spacc", sk], ["spacc"])
                    o_, ok_ = ost.next()
                    ACT(o_, ops_, AF.Identity, [opk], [ok_])
                    DMA("sp", OT[hd, :, qs], o_, [ok_], [])

    def n2_block(l, tb, xin, xout):
        t0 = tb * TBK
        A.top = mark
        P = ppt[:, l, :]
        actT = A.alloc([128, 16, TBK], BF16)
        wb = [A.alloc([128, 12288], BF16) for _ in range(2)]
        m2 = A.top
        SC.barrier()
        norm_pro(OT.rearrange("c p t -> p c t"), [(t0 + i * 256, 256, i * 256) for i in range(4)], actT,
                 P[:, GM:GM + 16], [(0, 4), (4, 8), (8, 12), (12, 16)], 512.0)
        SC.barrier()
        A.top = m2
        res = Rot("res", [A.alloc([128, 512], F32) for _ in range(3)])
        osb = Rot("osb", [A.alloc([128, 512], F32) for _ in range(3)])
        xin_v = xin.rearrange("(c p) t -> p c t", p=128)
        xout_v = xout.rearrange("(c p) t -> p c t", p=128)
        for gq in range(4):
            i = wctr[0] % 2
            wctr[0] += 1
            wv = wb[i][:, 0:16 * 512].rearrange("p (c n) -> p c n", c=16)
            wk = ("wb", i)
            DMA("pool", wv, w_out[l, :, gq * 512:(gq + 1) * 512].rearrange("(c p) n -> p c n", p=128), [], [wk])
            for tc in range(2):
                tok = t0 + tc * 512
                for n in range(4):
                    cc = gq * 4 + n
                    r_, rk = res.next()
                    DMA("sp", r_, xin_v[:, cc, tok:tok + 512], [], [rk])
                    pt, pk = psm()
                    for c in range(16):
                        MM(pt, wv[:, c, n * 128:(n + 1) * 128], actT[:, c, tc * 512:(tc + 1) * 512], c == 0, c == 15,
                           [wk, "actT"], [pk])
                    o_, ok_ = osb.next()
                    TT(o_, pt, r_, ALU.add, [pk, rk], [ok_])
                    DMA("sp", xout_v[:, cc, tok:tok + 512], o_, [ok_], [])

    def ffn_block(l, tb, xin, xout):
        t0 = tb * TBK
        A.top = mark
        P = ppt[:, l, :]
        h2T = A.alloc([128, 16, TBK + 2], BF16)
        wb = [A.alloc([128, 8192], BF16) for _ in range(2)]
        gT = A.alloc([128, 44, TBK], BF16)
        m2 = A.top
        A.top = m2 - 44 * TBK * 2
        SC.barrier()
        xin_v = xin.rearrange("(c p) t -> p c t", p=128)
        chunks = [(t0 + i * 256, 256, 2 + i * 256) for i in range(4)]
        if tb == 0:
            MEMSET(h2T[:, :, 0:2], 0.0, ["actT"])
        else:
            chunks = [(t0 - 2, 2, 0)] + chunks
        norm_pro(xin_v, chunks, h2T, P[:, G2:G2 + 16], [(0, 16)], float(D))
        SC.barrier()
        A.top = m2
        U = [Rot(f"U{p}", [A.alloc([128, TBK + 2], F32) for _ in range(2)]) for p in range(2)]
        av = Rot("av", [A.alloc([128, TBK], F32) for _ in range(1)])
        ag = Rot("ag", [A.alloc([128, TBK], F32) for _ in range(1)])
        sg = Rot("sg", [A.alloc([128, TBK], F32) for _ in range(1)])
        res = Rot("res", [A.alloc([128, 512], F32) for _ in range(2)])
        osb = Rot("osb", [A.alloc([128, 512], F32) for _ in range(2)])
        CS = [(0, 342), (342, 342), (684, 342)]
        for jg in range(22):
            i = wctr[0] % 2
            wctr[0] += 1
            wv = wb[i][:, 0:16 * 512].rearrange("p (c n) -> p c n", c=16)
            wk = ("wb", i)
            DMA("pool", wv[:, :, 0:256], w_up[l, :, jg * 256:(jg + 1) * 256].rearrange("(c p) n -> p c n", p=128), [], [wk])
            DMA("pool", wv[:, :, 256:512], w_up[l, :, DFF + jg * 256:DFF + (jg + 1) * 256].rearrange("(c p) n -> p c n", p=128),
                [], [wk])
            for jj in range(2):
                j = jg * 2 + jj
                acc = []
                for part in range(2):
                    u_, uk = U[part].next()
                    for (cs, cn) in CS:
                        pt, pk = psm()
                        for c in range(16):
                            MM(pt[:, 0:cn], wv[:, c, part * 256 + jj * 128:part * 256 + (jj + 1) * 128], h2T[:, c, cs:cs + cn],
                               c == 0, c == 15, [wk, "actT"], [pk])
                        ACT(u_[:, cs:cs + cn], pt[:, 0:cn], AF.Identity, [pk], [uk])
                    ch = part * 44 + j
                    a_, ak = (ag if part == 0 else av).next()
                    TS(a_, u_[:, 2:TBK + 2], P[:, CW + 2 * 88 + ch:CW + 2 * 88 + ch + 1], P[:, CB + ch:CB + ch + 1], ALU.mult, ALU.add,
                       [uk, "ppt"], [ak])
                    STT(a_, u_[:, 1:TBK + 1], P[:, CW + 88 + ch:CW + 88 + ch + 1], a_, ALU.mult, ALU.add, [uk, ak, "ppt"], [ak])
                    STT(a_, u_[:, 0:TBK], P[:, CW + ch:CW + ch + 1], a_, ALU.mult, ALU.add, [uk, ak, "ppt"], [ak])
                    acc.append((a_, ak))
                s_, sk = sg.next()
                ACT(s_, acc[0][0], AF.Silu, [acc[0][1]], [sk])
                TT(gT[:, j, :], s_, acc[1][0], ALU.mult, [sk, acc[1][1]], [("gT", j)])
        xout_v = xout.rearrange("(c p) t -> p c t", p=128)
        gkeys = [("gT", j) for j in range(44)]
        for cc in range(16):
            i = wctr[0] % 2
            wctr[0] += 1
            wv = wb[i][:, 0:44 * 128].rearrange("p (c n) -> p c n", c=44)
            wk = ("wb", i)
            DMA("pool", wv, w_down[l, :, cc * 128:(cc + 1) * 128].rearrange("(c p) n -> p c n", p=128), [], [wk])
            for tc in range(2):
                tok = t0 + tc * 512
                r_, rk = res.next()
                DMA("sp", r_, xin_v[:, cc, tok:tok + 512], [], [rk])
                pt, pk = psm()
                for jx in range(44):
                    MM(pt, wv[:, jx, :], gT[:, jx, tc * 512:(tc + 1) * 512], jx == 0, jx == 43, [wk, gkeys[jx]], [pk])
                o_, ok_ = osb.next()
                TT(o_, pt, r_, ALU.add, [pk, rk], [ok_])
                DMA("sp", xout_v[:, cc, tok:tok + 512], o_, [ok_], [])

    for l in range(L):
        xin = xT if l == 0 else xA
        for tb in range(NTB):
            n1_block(l, tb, xin)
        att_phase(l)
        for tb in range(NTB):
            n2_block(l, tb, xin, xB)
        xo = outT if l == L - 1 else xA
        for tb in range(NTB):
            ffn_block(l, tb, xB, xo)
    SC.barrier()
    nw = SC.emit()
    es.close()
    return nc, len(SC.ops), nw


def host_consts(S):
    bf = ml_dtypes.bfloat16
    cb = np.zeros((128, 768), np.float32)
    cb[:, 0:128] = np.eye(128)
    cb[:, 128:256] = 1.0
    r = np.zeros((128, 128), np.float32)
    for i in range(64):
        r[i + 64, i] = -1.0
        r[i, i + 64] = 1.0
    cb[:, 256:384] = r
    r = np.zeros((128, 128), np.float32)
    for i in range(32):
        r[i + 32, i] = -1.0
        r[i, i + 32] = 1.0
    cb[:, 384:512] = r
    jj, ss = np.meshgrid(np.arange(128), np.arange(128), indexing="ij")
    cb[:, 512:640] = np.where(jj >= ss, -1.0, 0.0)
    cb[:, 640:768] = -1.0
    k = np.arange(128)[:, None]
    q = np.arange(512)[None, :]
    am = np.zeros((128, 12, 512), np.float32)
    for j in range(4):
        am[:, j, :] = np.where(128 * j + k <= q, 0.0, NEG)
        am[:, 4 + j, :] = np.where(128 * j + k < q, 0.0, NEG)
        am[:, 8 + j, :] = np.where(128 * j + k < q, 1.0, 0.0)
    es_ = np.zeros((16, 16, 128), np.float32)
    for n in range(16):
        es_[n, n, :] = -NEG
    bm = np.zeros((128, 16, 16), np.float32)
    for own in range(16):
        bm[:, own, own:] = -1e30
    def rope(dim):
        inv = (10000.0 ** (-np.arange(0, dim, 2, dtype=np.float32) / np.float32(dim))).astype(np.float32)
        ang = np.arange(S, dtype=np.float32)[:, None] * inv[None, :]
        c, s_ = np.cos(ang).astype(np.float32).T, np.sin(ang).astype(np.float32).T
        return np.ascontiguousarray(np.concatenate([c, c], 0)), np.ascontiguousarray(np.concatenate([s_, s_], 0))
    cC, sC = rope(128)
    cR, sR = rope(64)
    return dict(cbf=cb.astype(bf), attm=am.reshape(128, 6144).astype(bf), esel=es_.reshape(16, 2048).astype(bf),
                bmask=bm.reshape(128, 256), cosC=cC, sinC=sC, cosR=cR, sinR=sR)


def pack_params(inp, L):
    pp = np.zeros((128, L, NP), np.float32)
    def cm(a):
        return a.reshape(L, -1, 128).transpose(2, 0, 1)
    pp[:, :, G1:G1 + 16] = cm(inp["attn_norm"])
    pp[:, :, G2:G2 + 16] = cm(inp["ffn_norm"])
    pp[:, :, GM:GM + 16] = cm(inp["mix_out_norm"])
    cw = inp["conv_w"].reshape(L, 3, 88, 128).transpose(3, 0, 1, 2)
    pp[:, :, CW:CW + 264] = cw.reshape(128, L, 264)
    pp[:, :, CB:CB + 88] = cm(inp["conv_b"])
    pp[:, :, FOXQ] = inp["fox_q_norm"].T
    pp[:, :, FOXK] = inp["fox_k_norm"].T
    pp[:, :, MOBQ] = inp["moba_q_norm"].T
    pp[:, :, MOBK] = inp["moba_k_norm"].T
    pp[:, :, CQ:CQ + 4] = cm(inp["mla_cq_norm"])
    pp[:, :, CKV:CKV + 2] = cm(inp["mla_ckv_norm"])
    pp[:, :, MQN] = inp["mla_q_norm"][:, 0:128].T
    pp[0:64, :, MQR] = inp["mla_q_norm"][:, 128:192].T
    pp[:, :, MKN] = inp["mla_k_norm"][:, 0:128].T
    pp[0:64, :, MKR] = inp["mla_k_norm"][:, 128:192].T
    pp[0:4, :, BFC] = inp["b_forget"].T
    return pp


_CACHE = {}


def run(inputs, debug=False, ncores=None):
    inp = {k: np.asarray(v) for k, v in inputs.items()}
    x = inp["x"]
    B, S, _ = x.shape
    L = inp["w_in"].shape[0]
    key = (S, L, debug)
    if key not in _CACHE:
        _CACHE[key] = build(S, L, debug)
    nc = _CACHE[key][0]
    common = host_consts(S)
    common["pp"] = pack_params(inp, L)
    for k in ("w_in", "w_uq", "w_ukv", "w_out", "w_up", "w_down"):
        common[k] = np.ascontiguousarray(inp[k], dtype=np.float32)
    ncores = ncores or B
    in_maps = []
    for c in range(ncores):
        m = dict(common)
        m["xT"] = np.ascontiguousarray(x[c % B].T)
        in_maps.append(m)
    res = run_bass_kernel_spmd(nc, in_maps, core_ids=list(range(ncores)))
    out = np.stack([np.ascontiguousarray(res.results[b]["outT"].T) for b in range(B)], 0)
    return out.astype(np.float32), res


def kernel(**inputs):
    out, _ = run(inputs)
    return out
```

```python
import contextlib
from functools import partial

import ml_dtypes
import numpy as np

import concourse.bass as bass
import concourse.mybir as mybir
from concourse.bass_utils import run_bass_kernel_spmd

F32 = mybir.dt.float32
BF16 = mybir.dt.bfloat16
AF = mybir.ActivationFunctionType
ALU = mybir.AluOpType
AX = mybir.AxisListType

D = 2048
INW = 5444
DFF = 5632
EPS = 1e-6
TBK = 1024
G1, G2, GM, CW, CB = 0, 16, 32, 48, 312
FOXQ, FOXK, MOBQ, MOBK, CQ, CKV, MQN, MQR, MKN, MKR, BFC = 400, 401, 402, 403, 404, 408, 410, 411, 412, 413, 414
NP = 416
NEG = -30000.0


class Op:
    __slots__ = ("eng", "fn", "deps", "signal", "seq", "dma", "sem", "target", "semkey")


class Sched:
    COMPUTE = ("pe", "act", "dve", "pool")

    def __init__(self, nc, es, nslots=None):
        self.nc = nc
        self.engs = {"pe": nc.tensor, "act": nc.scalar, "dve": nc.vector, "pool": nc.gpsimd, "sp": nc.sync}
        self.ops = []
        self.lastw = {}
        self.readers = {}
        self.last_op = {}
        self.esem = {e: es.enter_context(nc.semaphore(f"s_{e}")) for e in self.COMPUTE}
        nslots = nslots or {"sp": 16, "pool": 8}
        self.dsem = {q: [es.enter_context(nc.semaphore(f"d_{q}{i}")) for i in range(n)] for q, n in nslots.items()}
        self.dcount = {q: [0] * n for q, n in nslots.items()}
        self.dlast = {q: [None] * n for q, n in nslots.items()}
        self.dnext = {q: 0 for q in nslots}
        self.outstanding = []

    def add(self, eng, fn, reads=(), writes=(), dma=False):
        op = Op()
        op.eng, op.fn, op.dma, op.signal, op.seq = eng, fn, dma, False, None
        deps = {}

        def dep(d, war):
            if d is None:
                return
            if (not d.dma) and (not dma) and d.eng == eng:
                if eng == "pe" or war:
                    return
            deps[id(d)] = d

        for k in reads:
            dep(self.lastw.get(k), False)
        for k in writes:
            dep(self.lastw.get(k), False)
            rr = self.readers.get(k)
            if rr:
                for r in rr.values():
                    dep(r, True)
        if dma:
            q = eng
            i = self.dnext[q]
            self.dnext[q] = (i + 1) % len(self.dsem[q])
            prev = self.dlast[q][i]
            if prev is not None:
                deps[id(prev)] = prev
            self.dcount[q][i] += 16
            op.sem, op.target, op.semkey = self.dsem[q][i], self.dcount[q][i], (q, i)
            self.dlast[q][i] = op
            self.outstanding.append(op)
        else:
            self.last_op[eng] = op
        rk = id(op) if dma else eng
        for k in reads:
            self.readers.setdefault(k, {})[rk] = op
        for k in writes:
            self.lastw[k] = op
            self.readers[k] = {}
        op.deps = list(deps.values())
        for d in op.deps:
            if not d.dma:
                d.signal = True
        self.ops.append(op)
        return op

    def barrier(self):
        targets = [op for op in self.last_op.values()] + self.outstanding
        for e in ("pe", "act", "dve", "pool", "sp"):
            b = Op()
            b.eng, b.fn, b.dma, b.signal, b.seq = e, None, False, False, None
            b.deps = [t for t in targets if t.dma or t.eng != e]
            for d in b.deps:
                if not d.dma:
                    d.signal = True
            self.ops.append(b)
        self.outstanding = []
        self.lastw = {}
        self.readers = {}

    def emit(self):
        known = {e: {} for e in self.engs}
        seq = {e: 0 for e in self.COMPUTE}
        nwait = 0
        for op in self.ops:
            E = self.engs[op.eng]
            kn = known[op.eng]
            need = {}
            for d in op.deps:
                if d.dma:
                    key, sem, val = d.semkey, d.sem, d.target
                else:
                    key, sem, val = d.eng, self.esem[d.eng], d.seq
                if kn.get(key, 0) < val and need.get(key, (None, 0))[1] < val:
                    need[key] = (sem, val)
            for key, (sem, val) in need.items():
                E.wait_ge(sem, val)
                kn[key] = val
                nwait += 1
            if op.fn is None:
                continue
            inst = op.fn()
            if op.dma:
                inst.then_inc(op.sem, 16)
            elif op.signal:
                seq[op.eng] += 1
                op.seq = seq[op.eng]
                inst.then_inc(self.esem[op.eng], 1)
        return nwait


class Arena:
    def __init__(self, t, nbytes):
        self.t, self.n, self.top = t, nbytes, 0

    def alloc(self, shape, dt):
        esz = 4 if dt == F32 else 2
        n = int(np.prod(shape[1:]))
        nb = n * esz
        off = self.top
        self.top += (nb + 63) // 64 * 64
        assert self.top <= self.n, f"arena overflow {self.top} > {self.n}"
        ap = self.t[0:shape[0], off // 2:(off + nb) // 2]
        if dt == F32:
            ap = ap.bitcast(F32)
        if len(shape) == 3:
            ap = ap.rearrange("p (a b) -> p a b", a=shape[1])
        return ap


class Rot:
    def __init__(self, name, bufs):
        self.name, self.bufs, self.i = name, bufs, 0

    def next(self):
        i = self.i % len(self.bufs)
        self.i += 1
        return self.bufs[i], (self.name, i)


def build(S, L, debug=False):
    assert S % TBK == 0
    NTB = S // TBK
    NQC = S // 512
    nc = bass.Bass("TRN2", target_bir_lowering=False)
    es = contextlib.ExitStack()

    def din(name, shape, dt=F32):
        return nc.dram_tensor(name, list(shape), dt, kind="ExternalInput").ap()

    def dscr(name, shape, dt):
        return nc.dram_tensor(name, list(shape), dt, kind=("ExternalOutput" if debug else "Internal")).ap()

    xT = din("xT", [D, S])
    w_in = din("w_in", [L, D, INW])
    w_uq = din("w_uq", [L, 512, 768])
    w_ukv = din("w_ukv", [L, 256, 1024])
    w_out = din("w_out", [L, D, D])
    w_up = din("w_up", [L, D, 2 * DFF])
    w_down = din("w_down", [L, DFF, D])
    pp_d = din("pp", [128, L, NP])
    cbf_d = din("cbf", [128, 768], BF16)
    attm_d = din("attm", [128, 6144], BF16)
    esel_d = din("esel", [16, 2048], BF16)
    bmask_d = din("bmask", [128, 256])
    cosC_d, sinC_d = din("cosC", [128, S]), din("sinC", [128, S])
    cosR_d, sinR_d = din("cosR", [64, S]), din("sinR", [64, S])
    outT = nc.dram_tensor("outT", [D, S], F32, kind="ExternalOutput").ap()

    xA, xB = dscr("xA", [D, S], F32), dscr("xB", [D, S], F32)
    QT, KT = dscr("QT", [16, 128, S], BF16), dscr("KT", [16, 128, S], BF16)
    QR, KR = dscr("QR", [4, 64, S], BF16), dscr("KR", [4, 64, S], BF16)
    VV = dscr("VV", [16, 128, S // 128, 128], BF16)
    FQ, FK = dscr("FQ", [4, 3, S], BF16), dscr("FK", [4, 3, S], BF16)
    OT = dscr("OT", [16, 128, S], F32)

    ARENA_BYTES = 206 * 1024
    arena_t = es.enter_context(nc.sbuf_tensor("arena", [128, ARENA_BYTES // 2], BF16))
    A = Arena(arena_t, ARENA_BYTES)
    ps = [es.enter_context(nc.psum_tensor(f"ps{i}", [128, 512], F32))[:] for i in range(8)]
    SC = Sched(nc, es)
    engs = SC.engs

    def MM(out, lhsT, rhs, start, stop, reads, writes):
        SC.add("pe", partial(nc.tensor.matmul, out, lhsT=lhsT, rhs=rhs, start=start, stop=stop), reads, writes)

    def ACT(out, in_, func, reads, writes, bias=None, scale=None):
        kw = {}
        if bias is not None:
            kw["bias"] = bias
        if scale is not None:
            kw["scale"] = scale
        SC.add("act", partial(nc.scalar.activation, out=out, in_=in_, func=func, **kw), reads, writes)

    def STT(out, in0, scalar, in1, op0, op1, reads, writes):
        SC.add("dve", partial(nc.vector.scalar_tensor_tensor, out=out, in0=in0, scalar=scalar, in1=in1, op0=op0, op1=op1),
               reads, writes)

    def TT(out, in0, in1, op, reads, writes, eng="dve"):
        SC.add(eng, partial(engs[eng].tensor_tensor, out=out, in0=in0, in1=in1, op=op), reads, writes)

    def TS(out, in0, s1, s2, op0, op1, reads, writes):
        SC.add("dve", partial(nc.vector.tensor_scalar, out=out, in0=in0, scalar1=s1, scalar2=s2, op0=op0, op1=op1),
               reads, writes)

    def DMA(q, out, in_, reads, writes):
        SC.add(q, partial(engs[q].dma_start, out=out, in_=in_), reads, writes, dma=True)

    def MEMSET(ap, val, writes, eng="dve"):
        SC.add(eng, partial(engs[eng].memset, ap, val), (), writes)

    cbf = A.alloc([128, 768], BF16)
    ident, ones_b, r128T, r64T, negtri, negones = (cbf[:, i * 128:(i + 1) * 128] for i in range(6))
    ppt = A.alloc([128, L, NP], F32)
    cf = A.alloc([128, 8], F32)
    DMA("sp", cbf, cbf_d, [], ["cbf"])
    DMA("sp", ppt, pp_d, [], ["ppt"])
    cvals = [EPS, -8.0, float(np.log(128.0 ** -0.5)), float(np.log(192.0 ** -0.5)), 1.0, 0.0]
    for i, v in enumerate(cvals):
        MEMSET(cf[:, i:i + 1], v, ["cf"])
    c_eps, c_m8, c_ls128, c_ls192, c_one = (cf[:, i:i + 1] for i in range(5))
    fcarry = [A.alloc([4, 512], F32) for _ in range(2)]
    ones4 = A.alloc([4, 512], F32)
    MEMSET(ones4, 1.0, ["ones4"])
    mark = A.top
    SC.barrier()

    wctr = [0]
    pmr = Rot("ps", [0, 1, 2, 3])
    par = Rot("ps", [4, 5, 6, 7])

    def psm():
        b, _ = pmr.next()
        return ps[b], ("ps", b)

    def psa():
        b, _ = par.next()
        return ps[b], ("ps", b)

    def norm_pro(src_v, chunks, dst, gains, groups, Dg):
        XS = [A.alloc([128, 16, 256], F32) for _ in range(2)]
        SQ = A.alloc([128, 16, 256], BF16)
        LNV = [A.alloc([128, 256], F32) for _ in groups]
        RSTD = [A.alloc([128, 256], F32) for _ in groups]
        for i, (ts, n, dc) in enumerate(chunks):
            b = i % 2
            xs = XS[b]
            DMA("sp", xs[:, :, 0:n], src_v[:, :, ts:ts + n], [], [("xs", b)])
            ACT(SQ[:, :, 0:n], xs[:, :, 0:n], AF.Square, [("xs", b)], ["sq"])
            for gi, (c0, c1) in enumerate(groups):
                pb = 4 + gi
                for c in range(c0, c1):
                    MM(ps[pb][:, 0:n], ones_b, SQ[:, c, 0:n], c == c0, c == c1 - 1, ["sq", "cbf"], [("ps", pb)])
                ACT(LNV[gi][:, 0:n], ps[pb][:, 0:n], AF.Ln, [("ps", pb), "cf"], [("lnv", gi)], bias=c_eps, scale=1.0 / Dg)
                ACT(RSTD[gi][:, 0:n], LNV[gi][:, 0:n], AF.Exp, [("lnv", gi)], [("rstd", gi)], scale=-0.5)
                for c in range(c0, c1):
                    STT(dst[:, c, dc:dc + n], xs[:, c, 0:n], gains[:, c:c + 1], RSTD[gi][:, 0:n], ALU.mult, ALU.mult,
                        [("xs", b), ("rstd", gi), "ppt"], ["actT"])

    def n1_block(l, tb, xin):
        t0 = tb * TBK
        A.top = mark
        P = ppt[:, l, :]
        actT = A.alloc([128, 16, TBK], BF16)
        wb = [A.alloc([128, 12288], BF16) for _ in range(2)]
        wuq = A.alloc([128, 4, 768], BF16)
        wukv = A.alloc([128, 2, 1024], BF16)
        m2 = A.top
        SC.barrier()
        DMA("pool", wuq, w_uq[l].rearrange("(c p) n -> p c n", p=128), [], ["wuq"])
        DMA("pool", wukv, w_ukv[l].rearrange("(c p) n -> p c n", p=128), [], ["wukv"])
        xin_v = xin.rearrange("(c p) t -> p c t", p=128)
        norm_pro(xin_v, [(t0 + i * 256, 256, i * 256) for i in range(4)], actT, P[:, G1:G1 + 16], [(0, 16)], float(D))
        SC.barrier()
        A.top = m2
        def rot(name, shape, dt, n=2):
            return Rot(name, [A.alloc(shape, dt) for _ in range(n)])
        sqh = rot("sqh", [128, 512], BF16)
        lnv = rot("lnv", [128, 512], F32)
        rstd = rot("rstd", [128, 512], F32)
        xn = rot("xn", [128, 512], F32)
        xnb = rot("xnb", [128, 512], BF16)
        t1 = rot("t1", [128, 512], F32)
        t2 = rot("t2", [128, 512], F32)
        cosb = rot("cosb", [128, 512], F32)
        sinb = rot("sinb", [128, 512], F32)
        outb = rot("outb", [128, 512], BF16, 3)
        vout = rot("vout", [128, 512], BF16)
        fx = rot("fx", [4, 512], F32, 3)
        fqk = rot("fqk", [4, 6, 512], BF16)
        rawcq = A.alloc([128, 4, 512], F32)
        cqn = A.alloc([128, 4, TBK], BF16)
        ckvn = A.alloc([128, 2, TBK], BF16)
        krraw = A.alloc([64, TBK], F32)
        sqkr = A.alloc([64, 512], BF16)

        def wload(col0, ncols):
            i = wctr[0] % 2
            wctr[0] += 1
            v = wb[i][:, 0:16 * ncols].rearrange("p (c n) -> p c n", c=16)
            DMA("pool", v, w_in[l, :, col0:col0 + ncols].rearrange("(c p) n -> p c n", p=128), [], [("wb", i)])
            return v, ("wb", i)

        def rstd_of(ssp, ssk, Dn, lnscale=None):
            lv, lk = lnv.next()
            ACT(lv, ssp, AF.Ln, [ssk, "cf"], [lk], bias=c_eps, scale=1.0 / Dn)
            rs, rk = rstd.next()
            if lnscale is None:
                ACT(rs, lv, AF.Exp, [lk], [rk], scale=-0.5)
            else:
                ACT(rs, lv, AF.Exp, [lk, "cf"], [rk], scale=-0.5, bias=lnscale)
            return rs, rk

        def rope_out(xn_t, xn_k, np_, tok, cos_d, sin_d, rT, dst):
            xb, xbk = xnb.next()
            ACT(xb[0:np_], xn_t, AF.Identity, [xn_k], [xbk])
            rp, rpk = psa()
            MM(rp[0:np_], rT[0:np_, 0:np_], xb[0:np_], True, True, [xbk, "cbf"], [rpk])
            cb, cbk = cosb.next()
            sb, sbk = sinb.next()
            DMA("sp", cb[0:np_], cos_d[:, tok:tok + 512], [], [cbk])
            DMA("sp", sb[0:np_], sin_d[:, tok:tok + 512], [], [sbk])
            a1, a1k = t1.next()
            TT(a1[0:np_], xn_t, cb[0:np_], ALU.mult, [xn_k, cbk], [a1k])
            a2, a2k = t2.next()
            TT(a2[0:np_], rp[0:np_], sb[0:np_], ALU.mult, [rpk, sbk], [a2k])
            ob, obk = outb.next()
            TT(ob[0:np_], a1[0:np_], a2[0:np_], ALU.add, [a1k, a2k], [obk])
            DMA("sp", dst, ob[0:np_], [obk], [])

        def head_fm(pst, psk, tok, kind, gain, lnscale, dst, scale=None):
            if kind == "plain":
                ob, obk = outb.next()
                ACT(ob, pst, AF.Identity, [psk], [obk], scale=scale)
                DMA("sp", dst, ob, [obk], [])
                return
            sq, sqk = sqh.next()
            ACT(sq, pst, AF.Square, [psk], [sqk])
            sp_, spk = psa()
            MM(sp_, ones_b, sq, True, True, [sqk, "cbf"], [spk])
            rs, rk = rstd_of(sp_, spk, 128.0, lnscale)
            if kind == "norm":
                ob, obk = outb.next()
                STT(ob, pst, gain, rs, ALU.mult, ALU.mult, [psk, rk, "ppt"], [obk])
                DMA("sp", dst, ob, [obk], [])
            else:
                x_, xk = xn.next()
                STT(x_, pst, gain, rs, ALU.mult, ALU.mult, [psk, rk, "ppt"], [xk])
                rope_out(x_, xk, 128, tok, cosC_d, sinC_d, r128T, dst)

        def fm_group(col0, kind, gain, lnscale, dstT, hd0, scale=None):
            wv, wk = wload(col0, 512)
            for tc in range(2):
                tok = t0 + tc * 512
                for h in range(4):
                    pt, pk = psm()
                    for c in range(16):
                        MM(pt, wv[:, c, h * 128:(h + 1) * 128], actT[:, c, tc * 512:(tc + 1) * 512], c == 0, c == 15,
                           [wk, "actT"], [pk])
                    head_fm(pt, pk, tok, kind, gain, lnscale, dstT[hd0 + h, :, tok:tok + 512], scale)

        def v_group(col0, ncols, g, fox=False):
            wv, wk = wload(col0, ncols)
            for j in range(TBK // 128):
                pt, pk = psm()
                for c in range(16):
                    MM(pt, actT[:, c, j * 128:(j + 1) * 128], wv[:, c, 0:512], c == 0, c == 15, [wk, "actT"], [pk])
                vo, vk = vout.next()
                ACT(vo, pt, AF.Identity, [pk], [vk])
                jg = (t0 // 128) + j
                DMA("sp", VV[g * 4:(g + 1) * 4, :, jg, :].rearrange("h p d -> p h d"),
                    vo.rearrange("p (h d) -> p h d", h=4), [vk], [])
            if not fox:
                return
            bfv = P[0:4, BFC:BFC + 1]
            for tc in range(2):
                tok = t0 + tc * 512
                ci = tok // 512
                pt, pk = psm()
                for c in range(16):
                    MM(pt[0:4], wv[:, c, 512:516], actT[:, c, tc * 512:(tc + 1) * 512], c == 0, c == 15, [wk, "actT"], [pk])
                e_, ek = fx.next()
                ACT(e_, pt[0:4], AF.Exp, [pk, "ppt"], [ek], bias=bfv)
                s_, sk = fx.next()
                ACT(s_, e_, AF.Ln, [ek, "cf"], [sk], bias=c_one[0:4])
                lf, lfk = fx.next()
                STT(lf, pt[0:4], bfv, s_, ALU.add, ALU.subtract, [pk, sk, "ppt"], [lfk])
                cur, prev = fcarry[ci % 2], fcarry[(ci + 1) % 2]
                init = 0.0 if ci == 0 else prev[:, 511:512]
                SC.add("dve", partial(nc.vector.tensor_tensor_scan, out=cur, data0=ones4, data1=lf, initial=init,
                                      op0=ALU.mult, op1=ALU.add), [lfk, "ones4", ("fc", (ci + 1) % 2)], [("fc", ci % 2)])
                ck = ("fc", ci % 2)
                q6, q6k = fqk.next()
                ACT(q6[:, 0, :], cur, AF.Identity, [ck], [q6k])
                r1, r1k = fx.next()
                TT(r1, cur, q6[:, 0, :], ALU.subtract, [ck, q6k], [r1k])
                ACT(q6[:, 1, :], r1, AF.Identity, [r1k], [q6k])
                r2, r2k = fx.next()
                TT(r2, r1, q6[:, 1, :], ALU.subtract, [r1k, q6k], [r2k])
                ACT(q6[:, 2, :], r2, AF.Identity, [r2k], [q6k])
                ACT(q6[:, 3:6, :], q6[:, 0:3, :], AF.Identity, [q6k], [q6k], scale=-1.0)
                DMA("sp", FQ[:, :, tok:tok + 512], q6[:, 0:3, :], [q6k], [])
                DMA("sp", FK[:, :, tok:tok + 512], q6[:, 3:6, :], [q6k], [])

        fm_group(0, "norm", P[:, FOXQ:FOXQ + 1], c_ls128, QT, 0)
        fm_group(512, "norm", P[:, FOXK:FOXK + 1], None, KT, 0)
        v_group(1024, 516, 0, fox=True)
        fm_group(1540, "plain", None, None, QT, 4, scale=float(128.0 ** -0.5))
        fm_group(2052, "plain", None, None, KT, 4, scale=1.0)
        v_group(2564, 512, 1)
        fm_group(3076, "rope", P[:, MOBQ:MOBQ + 1], c_ls128, QT, 8)
        fm_group(3588, "rope", P[:, MOBK:MOBK + 1], None, KT, 8)
        v_group(4100, 512, 2)

        wv, wk = wload(4612, 512)
        for tc in range(2):
            ss, ssk = psa()
            for c4 in range(4):
                pt, pk = psm()
                for c in range(16):
                    MM(pt, wv[:, c, c4 * 128:(c4 + 1) * 128], actT[:, c, tc * 512:(tc + 1) * 512], c == 0, c == 15,
                       [wk, "actT"], [pk])
                ACT(rawcq[:, c4, :], pt, AF.Identity, [pk], [("rawcq", c4)])
                sq, sqk = sqh.next()
                ACT(sq, pt, AF.Square, [pk], [sqk])
                MM(ss, ones_b, sq, c4 == 0, c4 == 3, [sqk, "cbf"], [ssk])
            rs, rk = rstd_of(ss, ssk, 512.0)
            for c4 in range(4):
                STT(cqn[:, c4, tc * 512:(tc + 1) * 512], rawcq[:, c4, :], P[:, CQ + c4:CQ + c4 + 1], rs, ALU.mult, ALU.mult,
                    [("rawcq", c4), rk, "ppt"], [("cqn", tc)])
        wv, wk = wload(5124, 320)
        for tc in range(2):
            ss, ssk = psa()
            for c2 in range(2):
                pt, pk = psm()
                for c in range(16):
                    MM(pt, wv[:, c, c2 * 128:(c2 + 1) * 128], actT[:, c, tc * 512:(tc + 1) * 512], c == 0, c == 15,
                       [wk, "actT"], [pk])
                ACT(rawcq[:, c2, :], pt, AF.Identity, [pk], [("rawcq", c2)])
                sq, sqk = sqh.next()
                ACT(sq, pt, AF.Square, [pk], [sqk])
                MM(ss, ones_b, sq, c2 == 0, c2 == 1, [sqk, "cbf"], [ssk])
            rs, rk = rstd_of(ss, ssk, 256.0)
            for c2 in range(2):
                STT(ckvn[:, c2, tc * 512:(tc + 1) * 512], rawcq[:, c2, :], P[:, CKV + c2:CKV + c2 + 1], rs, ALU.mult, ALU.mult,
                    [("rawcq", c2), rk, "ppt"], [("ckvn", tc)])
            pt, pk = psm()
            for c in range(16):
                MM(pt[0:64], wv[:, c, 256:320], actT[:, c, tc * 512:(tc + 1) * 512], c == 0, c == 15, [wk, "actT"], [pk])
            ACT(krraw[:, tc * 512:(tc + 1) * 512], pt[0:64], AF.Identity, [pk], [("krraw", tc)])
        for tc in range(2):
            tok = t0 + tc * 512
            tsl = slice(tc * 512, (tc + 1) * 512)
            ACT(sqkr, krraw[:, tsl], AF.Square, [("krraw", tc)], ["sqkr"])
            for h in range(4):
                pn, pnk = psm()
                for c4 in range(4):
                    MM(pn, wuq[:, c4, h * 192:h * 192 + 128], cqn[:, c4, tsl], c4 == 0, c4 == 3, ["wuq", ("cqn", tc)], [pnk])
                pr, prk = psm()
                for c4 in range(4):
                    MM(pr[0:64], wuq[:, c4, h * 192 + 128:h * 192 + 192], cqn[:, c4, tsl], c4 == 0, c4 == 3,
                       ["wuq", ("cqn", tc)], [prk])
                sq, sqk = sqh.next()
                ACT(sq, pn, AF.Square, [pnk], [sqk])
                sq2, sq2k = sqh.next()
                ACT(sq2[0:64], pr[0:64], AF.Square, [prk], [sq2k])
                ss, ssk = psa()
                MM(ss, ones_b, sq, True, False, [sqk, "cbf"], [ssk])
                MM(ss, ones_b[0:64, :], sq2[0:64], False, True, [sq2k, "cbf"], [ssk])
                rs, rk = rstd_of(ss, ssk, 192.0, c_ls192)
                ob, obk = outb.next()
                STT(ob, pn, P[:, MQN:MQN + 1], rs, ALU.mult, ALU.mult, [pnk, rk, "ppt"], [obk])
                DMA("sp", QT[12 + h, :, tok:tok + 512], ob, [obk], [])
                x_, xk = xn.next()
                STT(x_[0:64], pr[0:64], P[0:64, MQR:MQR + 1], rs[0:64], ALU.mult, ALU.mult, [prk, rk, "ppt"], [xk])
                rope_out(x_[0:64], xk, 64, tok, cosR_d, sinR_d, r64T, QR[h, :, tok:tok + 512])
                pn, pnk = psm()
                for c2 in range(2):
                    MM(pn, wukv[:, c2, h * 256:h * 256 + 128], ckvn[:, c2, tsl], c2 == 0, c2 == 1, ["wukv", ("ckvn", tc)], [pnk])
                sq, sqk = sqh.next()
                ACT(sq, pn, AF.Square, [pnk], [sqk])
                ss, ssk = psa()
                MM(ss, ones_b, sq, True, False, [sqk, "cbf"], [ssk])
                MM(ss, ones_b[0:64, :], sqkr, False, True, ["sqkr", "cbf"], [ssk])
                rs, rk = rstd_of(ss, ssk, 192.0, None)
                ob, obk = outb.next()
                STT(ob, pn, P[:, MKN:MKN + 1], rs, ALU.mult, ALU.mult, [pnk, rk, "ppt"], [obk])
                DMA("sp", KT[12 + h, :, tok:tok + 512], ob, [obk], [])
                x_, xk = xn.next()
                STT(x_[0:64], krraw[:, tsl], P[0:64, MKR:MKR + 1], rs[0:64], ALU.mult, ALU.mult, [("krraw", tc), rk, "ppt"], [xk])
                rope_out(x_[0:64], xk, 64, tok, cosR_d, sinR_d, r64T, KR[h, :, tok:tok + 512])
            for j in range(4):
                pt, pk = psm()
                jl = tc * 4 + j
                for c2 in range(2):
                    rhs = wukv[:, c2, :].rearrange("p (h x) -> p h x", h=4)[:, :, 128:256]
                    MM(pt, ckvn[:, c2, jl * 128:(jl + 1) * 128], rhs, c2 == 0, c2 == 1, ["wukv", ("ckvn", tc)], [pk])
                vo, vk = vout.next()
                ACT(vo, pt, AF.Identity, [pk], [vk])
                jg = (t0 // 128) + jl
                DMA("sp", VV[12:16, :, jg, :].rearrange("h p d -> p h d"), vo.rearrange("p (h d) -> p h d", h=4), [vk], [])

    def att_phase(l):
        A.top = mark
        SC.barrier()
        attm = A.alloc([128, 12, 512], BF16)
        esel = A.alloc([16, 16, 128], BF16)
        bmask = A.alloc([128, 16, 16], F32)
        DMA("sp", attm, attm_d.rearrange("p (a b) -> p a b", a=12), [], ["attm"])
        DMA("sp", esel, esel_d.rearrange("p (a b) -> p a b", a=16), [], ["esel"])
        DMA("sp", bmask, bmask_d.rearrange("p (a b) -> p a b", a=16), [], ["bmask"])
        Mc = [attm[:, j, :] for j in range(4)]
        Ms = [attm[:, 4 + j, :] for j in range(4)]
        M01 = [attm[:, 8 + j, :] for j in range(4)]
        NJ = S // 128
        qt = Rot("qt", [A.alloc([128, S], BF16) for _ in range(2)])
        kt = Rot("kt", [A.alloc([128, S], BF16) for _ in range(2)])
        vv = Rot("vv", [A.alloc([128, NJ, 128], BF16) for _ in range(2)])
        qr = A.alloc([64, S], BF16)
        kr = A.alloc([64, S], BF16)
        kb = A.alloc([6, S], BF16)
        qb = A.alloc([6, S], BF16)
        mbT = A.alloc([16, S], BF16)
        pT = Rot("pT", [A.alloc([128, 512], BF16) for _ in range(5)])
        ef = Rot("ef", [A.alloc([128, 512], F32) for _ in range(3)])
        spb = Rot("spb", [A.alloc([128, 512], BF16) for _ in range(4)])
        spacc = Rot("spacc", [A.alloc([128, 512], BF16) for _ in range(3)])
        ost = Rot("ost", [A.alloc([128, 512], F32) for _ in range(2)])
        rl = Rot("rl", [A.alloc([128, 512], F32) for _ in range(2)])
        ksum = A.alloc([128, 16], F32)
        kmb = A.alloc([128, 16], BF16)
        gm = Rot("gm", [A.alloc([128, 16], F32) for _ in range(2)])
        m8 = Rot("m8", [A.alloc([128, 8], F32) for _ in range(2)])
        mbt = Rot("mbt", [A.alloc([128, 16], BF16) for _ in range(2)])
        sbank = Rot("ps", [0, 1, 6, 7])
        obank = Rot("ps", [2, 3])
        lbank = Rot("ps", [4, 5])
        zbank = Rot("ps", [0, 1])
        xbank = Rot("ps", [6, 7])

        for hd in range(16):
            g, h = hd // 4, hd % 4
            q_, qk_ = qt.next()
            k_, kk_ = kt.next()
            v_, vk_ = vv.next()
            DMA("sp", q_, QT[hd], [], [qk_])
            DMA("sp", k_, KT[hd], [], [kk_])
            DMA("sp", v_, VV[hd], [], [vk_])
            if g == 3:
                DMA("sp", qr, QR[h], [], ["qr"])
                DMA("sp", kr, KR[h], [], ["kr"])
            if g == 0:
                MEMSET(kb, 1.0, ["kb"])
                MEMSET(qb, 1.0, ["qb"])
                DMA("sp", kb[0:3, :], FK[h], [], ["kb"])
                DMA("sp", qb[3:6, :], FQ[h], [], ["qb"])
            if g == 2:
                SC.add("dve", partial(nc.vector.tensor_reduce, out=ksum[:, 0:S // 256],
                                      in_=k_.rearrange("p (n k) -> p n k", k=256), axis=AX.X, op=ALU.add), [kk_], ["ksum"])
                ACT(kmb[:, 0:S // 256], ksum[:, 0:S // 256], AF.Identity, ["ksum"], ["kmb"], scale=1.0 / 256.0)
                NBK = S // 256
                for j in range(NJ):
                    own = j // 2
                    gb, gk = lbank.next()
                    MM(ps[gb][:, 0:NBK], q_[:, j * 128:(j + 1) * 128], kmb[:, 0:NBK], True, True, [qk_, "kmb"], [("ps", gb)])
                    g_, gmk = gm.next()
                    if NBK < 16:
                        MEMSET(g_, -1e30, [gmk])
                    TT(g_[:, 0:NBK], ps[gb][:, 0:NBK], bmask[:, own, 0:NBK], ALU.add, [("ps", gb), "bmask"], [gmk])
                    m_, mk = m8.next()
                    SC.add("dve", partial(nc.vector.max, out=m_, in_=g_), [gmk], [mk])
                    b_, bk = mbt.next()
                    TS(b_, g_, m_[:, 2:3], 1.0, ALU.is_ge, ALU.subtract, [gmk, mk], [bk])
                    tb_, _ = lbank.next()
                    MM(ps[tb_][0:16, 0:128], b_, ident, True, True, [bk, "cbf"], [("ps", tb_)])
                    ACT(mbT[:, j * 128:(j + 1) * 128], ps[tb_][0:16, 0:128], AF.Identity, [("ps", tb_)], ["mbT"])

            if g != 1:
                LA = 2
                jobs = []
                for qc in range(NQC):
                    tiles = [(kt_i, None) for kt_i in range(4 * qc)] + [(4 * qc + j, j) for j in range(4)]
                    for ti, (kti, dj) in enumerate(tiles):
                        jobs.append((qc, ti, len(tiles), kti, dj))
                pend, cur = {}, {}
                for idx in range(len(jobs) + LA):
                    if idx < len(jobs):
                        qc, ti, ntl, kti, dj = jobs[idx]
                        qs = slice(qc * 512, (qc + 1) * 512)
                        ks = slice(kti * 128, (kti + 1) * 128)
                        sb_, _ = sbank.next()
                        sps, spk = ps[sb_], ("ps", sb_)
                        extra = []
                        if g == 0:
                            extra.append((kb[0:6, ks], qb[0:6, qs], ["kb", "qb"], None))
                        if g == 3:
                            extra.append((kr[:, ks], qr[:, qs], ["kr", "qr"], None))
                        if g == 2:
                            n = kti // 2
                            if dj is None:
                                extra.append((esel[:, n, :], mbT[:, qs], ["esel", "mbT"], None))
                            elif dj < 2:
                                extra.append((esel[:, n, :], mbT[:, qc * 512 + 256:(qc + 1) * 512], ["esel", "mbT"], (256, 512)))
                        if dj is not None:
                            if g == 2 and dj < 2:
                                extra.append((ident, Mc[dj][:, 0:256], ["cbf", "attm"], (0, 256)))
                            else:
                                extra.append((ident, Mc[dj], ["cbf", "attm"], None))
                        MM(sps, k_[:, ks], q_[:, qs], True, len(extra) == 0, [kk_, qk_], [spk])
                        for ei, (lt, rh, rk_, cr) in enumerate(extra):
                            o_ = sps if cr is None else sps[:, cr[0]:cr[1]]
                            MM(o_, lt, rh, False, ei == len(extra) - 1, rk_, [spk])
                        p_, pk_ = pT.next()
                        ACT(p_, sps, AF.Exp, [spk, "cf"], [pk_], bias=c_m8)
                        pend[idx] = (p_, pk_)
                    j2 = idx - LA
                    if j2 >= 0:
                        qc, ti, ntl, kti, dj = jobs[j2]
                        qs = slice(qc * 512, (qc + 1) * 512)
                        if ti == 0:
                            ob_, _ = obank.next()
                            lb_, _ = lbank.next()
                            cur[qc] = (ps[ob_], ("ps", ob_), ps[lb_], ("ps", lb_))
                        ops_, opk, lps, lpk = cur[qc]
                        p_, pk_ = pend.pop(j2)
                        MM(ops_, v_[:, kti, :], p_, ti == 0, ti == ntl - 1, [vk_, pk_], [opk])
                        MM(lps, ones_b, p_, ti == 0, ti == ntl - 1, ["cbf", pk_], [lpk])
                        if ti == ntl - 1:
                            r_, rk2 = rl.next()
                            SC.add("dve", partial(nc.vector.reciprocal, out=r_, in_=lps), [lpk], [rk2])
                            o_, ok_ = ost.next()
                            TT(o_, ops_, r_, ALU.mult, [opk, rk2], [ok_])
                            DMA("sp", OT[hd, :, qs], o_, [ok_], [])
            else:
                jobs = []
                for qc in range(NQC):
                    tiles = [(4 * qc + j, j) for j in (3, 2, 1, 0)] + [(kti, None) for kti in range(4 * qc - 1, -1, -1)]
                    for ti, (kti, dj) in enumerate(tiles):
                        jobs.append((qc, ti, len(tiles), kti, dj))
                spend, wpend, cur = {}, {}, {}
                accst = [None, None]
                for idx in range(len(jobs) + 2):
                    if idx < len(jobs):
                        qc, ti, ntl, kti, dj = jobs[idx]
                        qs = slice(qc * 512, (qc + 1) * 512)
                        ks = slice(kti * 128, (kti + 1) * 128)
                        zb, _ = zbank.next()
                        zps, zk = ps[zb], ("ps", zb)
                        MM(zps, k_[:, ks], q_[:, qs], True, True, [kk_, qk_], [zk])
                        e_, ek = ef.next()
                        ACT(e_, zps, AF.Exp, [zk], [ek])
                        s_, sk = spb.next()
                        ACT(s_, e_, AF.Ln, [ek, "cf"], [sk], bias=c_one)
                        if dj is not None:
                            TT(s_, s_, M01[dj], ALU.mult, [sk, "attm"], [sk])
                        spend[idx] = (s_, sk)
                    j1 = idx - 1
                    if 0 <= j1 < len(jobs):
                        qc, ti, ntl, kti, dj = jobs[j1]
                        qs = slice(qc * 512, (qc + 1) * 512)
                        ks = slice(kti * 128, (kti + 1) * 128)
                        s_, sk = spend.pop(j1)
                        xb_, _ = xbank.next()
                        xps, xk = ps[xb_], ("ps", xb_)
                        MM(xps, k_[:, ks], q_[:, qs], True, False, [kk_, qk_], [xk])
                        MM(xps, negtri, s_, False, False, ["cbf", sk], [xk])
                        if ti > 0:
                            MM(xps, negones, accst[0], False, dj is None, ["cbf", accst[1]], [xk])
                        if dj is not None:
                            MM(xps, ident, Ms[dj], False, True, ["cbf", "attm"], [xk])
                        w_, wk_ = pT.next()
                        ACT(w_, xps, AF.Exp, [xk], [wk_])
                        wpend[j1] = (w_, wk_)
                        if ti == 0:
                            na, nk = spacc.next()
                            SC.add("dve", partial(nc.vector.tensor_copy, out=na, in_=s_), [sk], [nk])
                            accst = [na, nk]
                        elif ti < ntl - 1:
                            na, nk = spacc.next()
                            TT(na, accst[0], s_, ALU.add, [accst[1], sk], [nk])
                            accst = [na, nk]
                    j2 = idx - 2
                    if j2 >= 0:
                        qc, ti, ntl, kti, dj = jobs[j2]
                        qs = slice(qc * 512, (qc + 1) * 512)
                        if ti == 0:
                            ob_, _ = obank.next()
                            cur[qc] = (ps[ob_], ("ps", ob_))
                        ops_, opk = cur[qc]
                        w_, wk_ = wpend.pop(j2)
                        MM(ops_, v_[:, kti, :], w_, ti == 0, ti == ntl - 1, [vk_, wk_], [opk])
                        if ti == ntl - 1:
                            o_, ok_ = ost.next()
                            ACT(o_, ops_, AF.Identity, [opk], [ok_])
                            DMA("sp", OT[hd, :, qs], o_, [ok_], [])

    def n2_block(l, tb, xin, xout):
        t0 = tb * TBK
        A.top = mark
        P = ppt[:, l, :]
        actT = A.alloc([128, 16, TBK], BF16)
        wb = [A.alloc([128, 12288], BF16) for _ in range(2)]
        m2 = A.top
        SC.barrier()
        norm_pro(OT.rearrange("c p t -> p c t"), [(t0 + i * 256, 256, i * 256) for i in range(4)], actT,
                 P[:, GM:GM + 16], [(0, 4), (4, 8), (8, 12), (12, 16)], 512.0)
        SC.barrier()
        A.top = m2
        res = Rot("res", [A.alloc([128, 512], F32) for _ in range(3)])
        osb = Rot("osb", [A.alloc([128, 512], F32) for _ in range(3)])
        xin_v = xin.rearrange("(c p) t -> p c t", p=128)
        xout_v = xout.rearrange("(c p) t -> p c t", p=128)
        for gq in range(4):
            i = wctr[0] % 2
            wctr[0] += 1
            wv = wb[i][:, 0:16 * 512].rearrange("p (c n) -> p c n", c=16)
            wk = ("wb", i)
            DMA("pool", wv, w_out[l, :, gq * 512:(gq + 1) * 512].rearrange("(c p) n -> p c n", p=128), [], [wk])
            for tc in range(2):
                tok = t0 + tc * 512
                for n in range(4):
                    cc = gq * 4 + n
                    r_, rk = res.next()
                    DMA("sp", r_, xin_v[:, cc, tok:tok + 512], [], [rk])
                    pt, pk = psm()
                    for c in range(16):
                        MM(pt, wv[:, c, n * 128:(n + 1) * 128], actT[:, c, tc * 512:(tc + 1) * 512], c == 0, c == 15,
                           [wk, "actT"], [pk])
                    o_, ok_ = osb.next()
                    TT(o_, pt, r_, ALU.add, [pk, rk], [ok_])
                    DMA("sp", xout_v[:, cc, tok:tok + 512], o_, [ok_], [])

    def ffn_block(l, tb, xin, xout):
        t0 = tb * TBK
        A.top = mark
        P = ppt[:, l, :]
        h2T = A.alloc([128, 16, TBK + 2], BF16)
        wb = [A.alloc([128, 8192], BF16) for _ in range(2)]
        gT = A.alloc([128, 44, TBK], BF16)
        m2 = A.top
        A.top = m2 - 44 * TBK * 2
        SC.barrier()
        xin_v = xin.rearrange("(c p) t -> p c t", p=128)
        chunks = [(t0 + i * 256, 256, 2 + i * 256) for i in range(4)]
        if tb == 0:
            MEMSET(h2T[:, :, 0:2], 0.0, ["actT"])
        else:
            chunks = [(t0 - 2, 2, 0)] + chunks
        norm_pro(xin_v, chunks, h2T, P[:, G2:G2 + 16], [(0, 16)], float(D))
        SC.barrier()
        A.top = m2
        U = [Rot(f"U{p}", [A.alloc([128, TBK + 2], F32) for _ in range(2)]) for p in range(2)]
        av = Rot("av", [A.alloc([128, TBK], F32) for _ in range(1)])
        ag = Rot("ag", [A.alloc([128, TBK], F32) for _ in range(1)])
        sg = Rot("sg", [A.alloc([128, TBK], F32) for _ in range(1)])
        res = Rot("res", [A.alloc([128, 512], F32) for _ in range(2)])
        osb = Rot("osb", [A.alloc([128, 512], F32) for _ in range(2)])
        CS = [(0, 342), (342, 342), (684, 342)]
        for jg in range(22):
            i = wctr[0] % 2
            wctr[0] += 1
            wv = wb[i][:, 0:16 * 512].rearrange("p (c n) -> p c n", c=16)
            wk = ("wb", i)
            DMA("pool", wv[:, :, 0:256], w_up[l, :, jg * 256:(jg + 1) * 256].rearrange("(c p) n -> p c n", p=128), [], [wk])
            DMA("pool", wv[:, :, 256:512], w_up[l, :, DFF + jg * 256:DFF + (jg + 1) * 256].rearrange("(c p) n -> p c n", p=128),
                [], [wk])
            for jj in range(2):
                j = jg * 2 + jj
                acc = []
                for part in range(2):
                    u_, uk = U[part].next()
                    for (cs, cn) in CS:
                        pt, pk = psm()
                        for c in range(16):
                            MM(pt[:, 0:cn], wv[:, c, part * 256 + jj * 128:part * 256 + (jj + 1) * 128], h2T[:, c, cs:cs + cn],
                               c == 0, c == 15, [wk, "actT"], [pk])
                        ACT(u_[:, cs:cs + cn], pt[:, 0:cn], AF.Identity, [pk], [uk])
                    ch = part * 44 + j
                    a_, ak = (ag if part == 0 else av).next()
                    TS(a_, u_[:, 2:TBK + 2], P[:, CW + 2 * 88 + ch:CW + 2 * 88 + ch + 1], P[:, CB + ch:CB + ch + 1], ALU.mult, ALU.add,
                       [uk, "ppt"], [ak])
                    STT(a_, u_[:, 1:TBK + 1], P[:, CW + 88 + ch:CW + 88 + ch + 1], a_, ALU.mult, ALU.add, [uk, ak, "ppt"], [ak])
                    STT(a_, u_[:, 0:TBK], P[:, CW + ch:CW + ch + 1], a_, ALU.mult, ALU.add, [uk, ak, "ppt"], [ak])
                    acc.append((a_, ak))
                s_, sk = sg.next()
                ACT(s_, acc[0][0], AF.Silu, [acc[0][1]], [sk])
                TT(gT[:, j, :], s_, acc[1][0], ALU.mult, [sk, acc[1][1]], [("gT", j)])
        xout_v = xout.rearrange("(c p) t -> p c t", p=128)
        gkeys = [("gT", j) for j in range(44)]
        for cc in range(16):
            i = wctr[0] % 2
            wctr[0] += 1
            wv = wb[i][:, 0:44 * 128].rearrange("p (c n) -> p c n", c=44)
            wk = ("wb", i)
            DMA("pool", wv, w_down[l, :, cc * 128:(cc + 1) * 128].rearrange("(c p) n -> p c n", p=128), [], [wk])
            for tc in range(2):
                tok = t0 + tc * 512
                r_, rk = res.next()
                DMA("sp", r_, xin_v[:, cc, tok:tok + 512], [], [rk])
                pt, pk = psm()
                for jx in range(44):
                    MM(pt, wv[:, jx, :], gT[:, jx, tc * 512:(tc + 1) * 512], jx == 0, jx == 43, [wk, gkeys[jx]], [pk])
                o_, ok_ = osb.next()
                TT(o_, pt, r_, ALU.add, [pk, rk], [ok_])
                DMA("sp", xout_v[:, cc, tok:tok + 512], o_, [ok_], [])

    for l in range(L):
        xin = xT if l == 0 else xA
        for tb in range(NTB):
            n1_block(l, tb, xin)
        att_phase(l)
        for tb in range(NTB):
            n2_block(l, tb, xin, xB)
        xo = outT if l == L - 1 else xA
        for tb in range(NTB):
            ffn_block(l, tb, xB, xo)
    SC.barrier()
    nw = SC.emit()
    es.close()
    return nc, len(SC.ops), nw


def host_consts(S):
    bf = ml_dtypes.bfloat16
    cb = np.zeros((128, 768), np.float32)
    cb[:, 0:128] = np.eye(128)
    cb[:, 128:256] = 1.0
    r = np.zeros((128, 128), np.float32)
    for i in range(64):
        r[i + 64, i] = -1.0
        r[i, i + 64] = 1.0
    cb[:, 256:384] = r
    r = np.zeros((128, 128), np.float32)
    for i in range(32):
        r[i + 32, i] = -1.0
        r[i, i + 32] = 1.0
    cb[:, 384:512] = r
    jj, ss = np.meshgrid(np.arange(128), np.arange(128), indexing="ij")
    cb[:, 512:640] = np.where(jj >= ss, -1.0, 0.0)
    cb[:, 640:768] = -1.0
    k = np.arange(128)[:, None]
    q = np.arange(512)[None, :]
    am = np.zeros((128, 12, 512), np.float32)
    for j in range(4):
        am[:, j, :] = np.where(128 * j + k <= q, 0.0, NEG)
        am[:, 4 + j, :] = np.where(128 * j + k < q, 0.0, NEG)
        am[:, 8 + j, :] = np.where(128 * j + k < q, 1.0, 0.0)
    es_ = np.zeros((16, 16, 128), np.float32)
    for n in range(16):
        es_[n, n, :] = -NEG
    bm = np.zeros((128, 16, 16), np.float32)
    for own in range(16):
        bm[:, own, own:] = -1e30
    def rope(dim):
        inv = (10000.0 ** (-np.arange(0, dim, 2, dtype=np.float32) / np.float32(dim))).astype(np.float32)
        ang = np.arange(S, dtype=np.float32)[:, None] * inv[None, :]
        c, s_ = np.cos(ang).astype(np.float32).T, np.sin(ang).astype(np.float32).T
        return np.ascontiguousarray(np.concatenate([c, c], 0)), np.ascontiguousarray(np.concatenate([s_, s_], 0))
    cC, sC = rope(128)
    cR, sR = rope(64)
    return dict(cbf=cb.astype(bf), attm=am.reshape(128, 6144).astype(bf), esel=es_.reshape(16, 2048).astype(bf),
                bmask=bm.reshape(128, 256), cosC=cC, sinC=sC, cosR=cR, sinR=sR)


def pack_params(inp, L):
    pp = np.zeros((128, L, NP), np.float32)
    def cm(a):
        return a.reshape(L, -1, 128).transpose(2, 0, 1)
    pp[:, :, G1:G1 + 16] = cm(inp["attn_norm"])
    pp[:, :, G2:G2 + 16] = cm(inp["ffn_norm"])
    pp[:, :, GM:GM + 16] = cm(inp["mix_out_norm"])
    cw = inp["conv_w"].reshape(L, 3, 88, 128).transpose(3, 0, 1, 2)
    pp[:, :, CW:CW + 264] = cw.reshape(128, L, 264)
    pp[:, :, CB:CB + 88] = cm(inp["conv_b"])
    pp[:, :, FOXQ] = inp["fox_q_norm"].T
    pp[:, :, FOXK] = inp["fox_k_norm"].T
    pp[:, :, MOBQ] = inp["moba_q_norm"].T
    pp[:, :, MOBK] = inp["moba_k_norm"].T
    pp[:, :, CQ:CQ + 4] = cm(inp["mla_cq_norm"])
    pp[:, :, CKV:CKV + 2] = cm(inp["mla_ckv_norm"])
    pp[:, :, MQN] = inp["mla_q_norm"][:, 0:128].T
    pp[0:64, :, MQR] = inp["mla_q_norm"][:, 128:192].T
    pp[:, :, MKN] = inp["mla_k_norm"][:, 0:128].T
    pp[0:64, :, MKR] = inp["mla_k_norm"][:, 128:192].T
    pp[0:4, :, BFC] = inp["b_forget"].T
    return pp


_CACHE = {}


def run(inputs, debug=False, ncores=None):
    inp = {k: np.asarray(v) for k, v in inputs.items()}
    x = inp["x"]
    B, S, _ = x.shape
    L = inp["w_in"].shape[0]
    key = (S, L, debug)
    if key not in _CACHE:
        _CACHE[key] = build(S, L, debug)
    nc = _CACHE[key][0]
    common = host_consts(S)
    common["pp"] = pack_params(inp, L)
    for k in ("w_in", "w_uq", "w_ukv", "w_out", "w_up", "w_down"):
        common[k] = np.ascontiguousarray(inp[k], dtype=np.float32)
    ncores = ncores or B
    in_maps = []
    for c in range(ncores):
        m = dict(common)
        m["xT"] = np.ascontiguousarray(x[c % B].T)
        in_maps.append(m)
    res = run_bass_kernel_spmd(nc, in_maps, core_ids=list(range(ncores)))
    out = np.stack([np.ascontiguousarray(res.results[b]["outT"].T) for b in range(B)], 0)
    return out.astype(np.float32), res


def kernel(**inputs):
    out, _ = run(inputs)
    return out
```

```python
import contextlib
from functools import partial

import ml_dtypes
import numpy as np

import concourse.bass as bass
import concourse.mybir as mybir
from concourse.bass_utils import run_bass_kernel_spmd

F32 = mybir.dt.float32
BF16 = mybir.dt.bfloat16
AF = mybir.ActivationFunctionType
ALU = mybir.AluOpType
AX = mybir.AxisListType

D = 2048
INW = 5444
DFF = 5632
EPS = 1e-6
TBK = 1024
G1, G2, GM, CW, CB = 0, 16, 32, 48, 312
FOXQ, FOXK, MOBQ, MOBK, CQ, CKV, MQN, MQR, MKN, MKR, BFC = 400, 401, 402, 403, 404, 408, 410, 411, 412, 413, 414
NP = 416
NEG = -30000.0


class Op:
    __slots__ = ("eng", "fn", "deps", "signal", "seq", "dma", "sem", "target", "semkey")


class Sched:
    COMPUTE = ("pe", "act", "dve", "pool")

    def __init__(self, nc, es, nslots=None):
        self.nc = nc
        self.engs = {"pe": nc.tensor, "act": nc.scalar, "dve": nc.vector, "pool": nc.gpsimd, "sp": nc.sync}
        self.ops = []
        self.lastw = {}
        self.readers = {}
        self.last_op = {}
        self.esem = {e: es.enter_context(nc.semaphore(f"s_{e}")) for e in self.COMPUTE}
        nslots = nslots or {"sp": 16, "pool": 8}
        self.dsem = {q: [es.enter_context(nc.semaphore(f"d_{q}{i}")) for i in range(n)] for q, n in nslots.items()}
        self.dcount = {q: [0] * n for q, n in nslots.items()}
        self.dlast = {q: [None] * n for q, n in nslots.items()}
        self.dnext = {q: 0 for q in nslots}
        self.outstanding = []

    def add(self, eng, fn, reads=(), writes=(), dma=False):
        op = Op()
        op.eng, op.fn, op.dma, op.signal, op.seq = eng, fn, dma, False, None
        deps = {}

        def dep(d, war):
            if d is None:
                return
            if (not d.dma) and (not dma) and d.eng == eng:
                if eng == "pe" or war:
                    return
            deps[id(d)] = d

        for k in reads:
            dep(self.lastw.get(k), False)
        for k in writes:
            dep(self.lastw.get(k), False)
            rr = self.readers.get(k)
            if rr:
                for r in rr.values():
                    dep(r, True)
        if dma:
            q = eng
            i = self.dnext[q]
            self.dnext[q] = (i + 1) % len(self.dsem[q])
            prev = self.dlast[q][i]
            if prev is not None:
                deps[id(prev)] = prev
            self.dcount[q][i] += 16
            op.sem, op.target, op.semkey = self.dsem[q][i], self.dcount[q][i], (q, i)
            self.dlast[q][i] = op
            self.outstanding.append(op)
        else:
            self.last_op[eng] = op
        rk = id(op) if dma else eng
        for k in reads:
            self.readers.setdefault(k, {})[rk] = op
        for k in writes:
            self.lastw[k] = op
            self.readers[k] = {}
        op.deps = list(deps.values())
        for d in op.deps:
            if not d.dma:
                d.signal = True
        self.ops.append(op)
        return op

    def barrier(self):
        targets = [op for op in self.last_op.values()] + self.outstanding
        for e in ("pe", "act", "dve", "pool", "sp"):
            b = Op()
            b.eng, b.fn, b.dma, b.signal, b.seq = e, None, False, False, None
            b.deps = [t for t in targets if t.dma or t.eng != e]
            for d in b.deps:
                if not d.dma:
                    d.signal = True
            self.ops.append(b)
        self.outstanding = []
        self.lastw = {}
        self.readers = {}

    def emit(self):
        known = {e: {} for e in self.engs}
        seq = {e: 0 for e in self.COMPUTE}
        nwait = 0
        for op in self.ops:
            E = self.engs[op.eng]
            kn = known[op.eng]
            need = {}
            for d in op.deps:
                if d.dma:
                    key, sem, val = d.semkey, d.sem, d.target
                else:
                    key, sem, val = d.eng, self.esem[d.eng], d.seq
                if kn.get(key, 0) < val and need.get(key, (None, 0))[1] < val:
                    need[key] = (sem, val)
            for key, (sem, val) in need.items():
                E.wait_ge(sem, val)
                kn[key] = val
                nwait += 1
            if op.fn is None:
                continue
            inst = op.fn()
            if op.dma:
                inst.then_inc(op.sem, 16)
            elif op.signal:
                seq[op.eng] += 1
                op.seq = seq[op.eng]
                inst.then_inc(self.esem[op.eng], 1)
        return nwait


class Arena:
    def __init__(self, t, nbytes):
        self.t, self.n, self.top = t, nbytes, 0

    def alloc(self, shape, dt):
        esz = 4 if dt == F32 else 2
        n = int(np.prod(shape[1:]))
        nb = n * esz
        off = self.top
        self.top += (nb + 63) // 64 * 64
        assert self.top <= self.n, f"arena overflow {self.top} > {self.n}"
        ap = self.t[0:shape[0], off // 2:(off + nb) // 2]
        if dt == F32:
            ap = ap.bitcast(F32)
        if len(shape) == 3:
            ap = ap.rearrange("p (a b) -> p a b", a=shape[1])
        return ap


class Rot:
    def __init__(self, name, bufs):
        self.name, self.bufs, self.i = name, bufs, 0

    def next(self):
        i = self.i % len(self.bufs)
        self.i += 1
        return self.bufs[i], (self.name, i)


def build(S, L, debug=False):
    assert S % TBK == 0
    NTB = S // TBK
    NQC = S // 512
    nc = bass.Bass("TRN2", target_bir_lowering=False)
    es = contextlib.ExitStack()

    def din(name, shape, dt=F32):
        return nc.dram_tensor(name, list(shape), dt, kind="ExternalInput").ap()

    def dscr(name, shape, dt):
        return nc.dram_tensor(name, list(shape), dt, kind=("ExternalOutput" if debug else "Internal")).ap()

    xT = din("xT", [D, S])
    w_in = din("w_in", [L, D, INW])
    w_uq = din("w_uq", [L, 512, 768])
    w_ukv = din("w_ukv", [L, 256, 1024])
    w_out = din("w_out", [L, D, D])
    w_up = din("w_up", [L, D, 2 * DFF])
    w_down = din("w_down", [L, DFF, D])
    pp_d = din("pp", [128, L, NP])
    cbf_d = din("cbf", [128, 768], BF16)
    attm_d = din("attm", [128, 6144], BF16)
    esel_d = din("esel", [16, 2048], BF16)
    bmask_d = din("bmask", [128, 256])
    cosC_d, sinC_d = din("cosC", [128, S]), din("sinC", [128, S])
    cosR_d, sinR_d = din("cosR", [64, S]), din("sinR", [64, S])
    outT = nc.dram_tensor("outT", [D, S], F32, kind="ExternalOutput").ap()

    xA, xB = dscr("xA", [D, S], F32), dscr("xB", [D, S], F32)
    QT, KT = dscr("QT", [16, 128, S], BF16), dscr("KT", [16, 128, S], BF16)
    QR, KR = dscr("QR", [4, 64, S], BF16), dscr("KR", [4, 64, S], BF16)
    VV = dscr("VV", [16, 128, S // 128, 128], BF16)
    FQ, FK = dscr("FQ", [4, 3, S], BF16), dscr("FK", [4, 3, S], BF16)
    OT = dscr("OT", [16, 128, S], F32)

    ARENA_BYTES = 206 * 1024
    arena_t = es.enter_context(nc.sbuf_tensor("arena", [128, ARENA_BYTES // 2], BF16))
    A = Arena(arena_t, ARENA_BYTES)
    ps = [es.enter_context(nc.psum_tensor(f"ps{i}", [128, 512], F32))[:] for i in range(8)]
    SC = Sched(nc, es)
    engs = SC.engs

    def MM(out, lhsT, rhs, start, stop, reads, writes):
        SC.add("pe", partial(nc.tensor.matmul, out, lhsT=lhsT, rhs=rhs, start=start, stop=stop), reads, writes)

    def ACT(out, in_, func, reads, writes, bias=None, scale=None):
        kw = {}
        if bias is not None:
            kw["bias"] = bias
        if scale is not None:
            kw["scale"] = scale
        SC.add("act", partial(nc.scalar.activation, out=out, in_=in_, func=func, **kw), reads, writes)

    def STT(out, in0, scalar, in1, op0, op1, reads, writes):
        SC.add("dve", partial(nc.vector.scalar_tensor_tensor, out=out, in0=in0, scalar=scalar, in1=in1, op0=op0, op1=op1),
               reads, writes)

    def TT(out, in0, in1, op, reads, writes, eng="dve"):
        SC.add(eng, partial(engs[eng].tensor_tensor, out=out, in0=in0, in1=in1, op=op), reads, writes)

    def TS(out, in0, s1, s2, op0, op1, reads, writes):
        SC.add("dve", partial(nc.vector.tensor_scalar, out=out, in0=in0, scalar1=s1, scalar2=s2, op0=op0, op1=op1),
               reads, writes)

    def DMA(q, out, in_, reads, writes):
        SC.add(q, partial(engs[q].dma_start, out=out, in_=in_), reads, writes, dma=True)

    def MEMSET(ap, val, writes, eng="dve"):
        SC.add(eng, partial(engs[eng].memset, ap, val), (), writes)

    cbf = A.alloc([128, 768], BF16)
    ident, ones_b, r128T, r64T, negtri, negones = (cbf[:, i * 128:(i + 1) * 128] for i in range(6))
    ppt = A.alloc([128, L, NP], F32)
    cf = A.alloc([128, 8], F32)
    DMA("sp", cbf, cbf_d, [], ["cbf"])
    DMA("sp", ppt, pp_d, [], ["ppt"])
    cvals = [EPS, -8.0, float(np.log(128.0 ** -0.5)), float(np.log(192.0 ** -0.5)), 1.0, 0.0]
    for i, v in enumerate(cvals):
        MEMSET(cf[:, i:i + 1], v, ["cf"])
    c_eps, c_m8, c_ls128, c_ls192, c_one = (cf[:, i:i + 1] for i in range(5))
    fcarry = [A.alloc([4, 512], F32) for _ in range(2)]
    ones4 = A.alloc([4, 512], F32)
    MEMSET(ones4, 1.0, ["ones4"])
    mark = A.top
    SC.barrier()

    wctr = [0]
    pmr = Rot("ps", [0, 1, 2, 3])
    par = Rot("ps", [4, 5, 6, 7])

    def psm():
        b, _ = pmr.next()
        return ps[b], ("ps", b)

    def psa():
        b, _ = par.next()
        return ps[b], ("ps", b)

    def norm_stage(ngroups):
        return dict(XS=[A.alloc([128, 16, 256], F32) for _ in range(2)], SQ=A.alloc([128, 16, 256], BF16),
                    LNV=[A.alloc([128, 256], F32) for _ in range(ngroups)], RSTD=[A.alloc([128, 256], F32) for _ in range(ngroups)],
                    i=0)

    def norm_chunk(st, src_v, ts, n, dc, dst, gains, groups, Dg, akey="actT"):
        b = st["i"] % 2
        st["i"] += 1
        xs, SQ, LNV, RSTD = st["XS"][b], st["SQ"], st["LNV"], st["RSTD"]
        DMA("sp", xs[:, :, 0:n], src_v[:, :, ts:ts + n], [], [("xs", b)])
        ACT(SQ[:, :, 0:n], xs[:, :, 0:n], AF.Square, [("xs", b)], ["sq"])
        for gi, (c0, c1) in enumerate(groups):
            pb = 4 + gi
            for c in range(c0, c1):
                MM(ps[pb][:, 0:n], ones_b, SQ[:, c, 0:n], c == c0, c == c1 - 1, ["sq", "cbf"], [("ps", pb)])
            ACT(LNV[gi][:, 0:n], ps[pb][:, 0:n], AF.Ln, [("ps", pb), "cf"], [("lnv", gi)], bias=c_eps, scale=1.0 / Dg)
            ACT(RSTD[gi][:, 0:n], LNV[gi][:, 0:n], AF.Exp, [("lnv", gi)], [("rstd", gi)], scale=-0.5)
            for c in range(c0, c1):
                STT(dst[:, c, dc:dc + n], xs[:, c, 0:n], gains[:, c:c + 1], RSTD[gi][:, 0:n], ALU.mult, ALU.mult,
                    [("xs", b), ("rstd", gi), "ppt"], [akey])

    def norm_pro(src_v, chunks, dst, gains, groups, Dg):
        st = norm_stage(len(groups))
        for (ts, n, dc) in chunks:
            norm_chunk(st, src_v, ts, n, dc, dst, gains, groups, Dg)

    def n1_block(l, tb, xin):
        t0 = tb * TBK
        A.top = mark
        P = ppt[:, l, :]
        actT = A.alloc([128, 16, TBK], BF16)
        wb = [A.alloc([128, 12288], BF16) for _ in range(2)]
        wuq = A.alloc([128, 4, 768], BF16)
        wukv = A.alloc([128, 2, 1024], BF16)
        m2 = A.top
        SC.barrier()
        DMA("pool", wuq, w_uq[l].rearrange("(c p) n -> p c n", p=128), [], ["wuq"])
        DMA("pool", wukv, w_ukv[l].rearrange("(c p) n -> p c n", p=128), [], ["wukv"])
        xin_v = xin.rearrange("(c p) t -> p c t", p=128)
        norm_pro(xin_v, [(t0 + i * 256, 256, i * 256) for i in range(4)], actT, P[:, G1:G1 + 16], [(0, 16)], float(D))
        SC.barrier()
        A.top = m2
        def rot(name, shape, dt, n=2):
            return Rot(name, [A.alloc(shape, dt) for _ in range(n)])
        sqh = rot("sqh", [128, 512], BF16)
        lnv = rot("lnv", [128, 512], F32)
        rstd = rot("rstd", [128, 512], F32)
        xn = rot("xn", [128, 512], F32)
        xnb = rot("xnb", [128, 512], BF16)
        t1 = rot("t1", [128, 512], F32)
        t2 = rot("t2", [128, 512], F32)
        cosb = rot("cosb", [128, 512], F32)
        sinb = rot("sinb", [128, 512], F32)
        outb = rot("outb", [128, 512], BF16, 3)
        vout = rot("vout", [128, 512], BF16)
        fx = rot("fx", [4, 512], F32, 3)
        fqk = rot("fqk", [4, 6, 512], BF16)
        rawcq = A.alloc([128, 4, 512], F32)
        cqn = A.alloc([128, 4, TBK], BF16)
        ckvn = A.alloc([128, 2, TBK], BF16)
        krraw = A.alloc([64, TBK], F32)
        sqkr = A.alloc([64, 512], BF16)

        def wload(col0, ncols):
            i = wctr[0] % 2
            wctr[0] += 1
            v = wb[i][:, 0:16 * ncols].rearrange("p (c n) -> p c n", c=16)
            DMA("pool", v, w_in[l, :, col0:col0 + ncols].rearrange("(c p) n -> p c n", p=128), [], [("wb", i)])
            return v, ("wb", i)

        def rstd_of(ssp, ssk, Dn, lnscale=None):
            lv, lk = lnv.next()
            ACT(lv, ssp, AF.Ln, [ssk, "cf"], [lk], bias=c_eps, scale=1.0 / Dn)
            rs, rk = rstd.next()
            if lnscale is None:
                ACT(rs, lv, AF.Exp, [lk], [rk], scale=-0.5)
            else:
                ACT(rs, lv, AF.Exp, [lk, "cf"], [rk], scale=-0.5, bias=lnscale)
            return rs, rk

        def load_cs(np_, tok, cos_d, sin_d):
            cb, cbk = cosb.next()
            sb, sbk = sinb.next()
            DMA("sp", cb[0:np_], cos_d[:, tok:tok + 512], [], [cbk])
            DMA("sp", sb[0:np_], sin_d[:, tok:tok + 512], [], [sbk])
            return (cb, cbk, sb, sbk)

        def rope_out(xn_t, xn_k, np_, cs, rT, dst):
            cb, cbk, sb, sbk = cs
            xb, xbk = xnb.next()
            ACT(xb[0:np_], xn_t, AF.Identity, [xn_k], [xbk])
            a1, a1k = t1.next()
            TT(a1[0:np_], xn_t, cb[0:np_], ALU.mult, [xn_k, cbk], [a1k])
            yield
            rp, rpk = psa()
            MM(rp[0:np_], rT[0:np_, 0:np_], xb[0:np_], True, True, [xbk, "cbf"], [rpk])
            a2, a2k = t2.next()
            TT(a2[0:np_], rp[0:np_], sb[0:np_], ALU.mult, [rpk, sbk], [a2k])
            ob, obk = outb.next()
            TT(ob[0:np_], a1[0:np_], a2[0:np_], ALU.add, [a1k, a2k], [obk])
            DMA("sp", dst, ob[0:np_], [obk], [])

        def head_fm(pst, psk, cs, kind, gain, lnscale, dst, scale=None):
            if kind == "plain":
                ob, obk = outb.next()
                ACT(ob, pst, AF.Identity, [psk], [obk], scale=scale)
                DMA("sp", dst, ob, [obk], [])
                return
            sq, sqk = sqh.next()
            ACT(sq, pst, AF.Square, [psk], [sqk])
            yield
            sp_, spk = psa()
            MM(sp_, ones_b, sq, True, True, [sqk, "cbf"], [spk])
            rs, rk = rstd_of(sp_, spk, 128.0, lnscale)
            if kind == "norm":
                ob, obk = outb.next()
                STT(ob, pst, gain, rs, ALU.mult, ALU.mult, [psk, rk, "ppt"], [obk])
                DMA("sp", dst, ob, [obk], [])
            else:
                x_, xk = xn.next()
                STT(x_, pst, gain, rs, ALU.mult, ALU.mult, [psk, rk, "ppt"], [xk])
                yield from rope_out(x_, xk, 128, cs, r128T, dst)

        active = []

        def advance(newgen=None):
            for g_ in active[:]:
                try:
                    next(g_)
                except StopIteration:
                    active.remove(g_)
            if newgen is not None:
                try:
                    next(newgen)
                    active.append(newgen)
                except StopIteration:
                    pass

        def flush():
            while active:
                advance()

        def run_gen(gen):
            for _ in gen:
                pass

        def fm_group(col0, kind, gain, lnscale, dstT, hd0, scale=None):
            wv, wk = wload(col0, 512)
            css = [load_cs(128, t0 + tc * 512, cosC_d, sinC_d) if kind == "rope" else None for tc in range(2)]
            for tc in range(2):
                tok = t0 + tc * 512
                for h in range(4):
                    pt, pk = psm()
                    for c in range(16):
                        MM(pt, wv[:, c, h * 128:(h + 1) * 128], actT[:, c, tc * 512:(tc + 1) * 512], c == 0, c == 15,
                           [wk, "actT"], [pk])
                    advance(head_fm(pt, pk, css[tc], kind, gain, lnscale, dstT[hd0 + h, :, tok:tok + 512], scale))
            flush()

        def v_group(col0, ncols, g, fox=False):
            wv, wk = wload(col0, ncols)
            for j in range(TBK // 128):
                pt, pk = psm()
                for c in range(16):
                    MM(pt, actT[:, c, j * 128:(j + 1) * 128], wv[:, c, 0:512], c == 0, c == 15, [wk, "actT"], [pk])
                vo, vk = vout.next()
                ACT(vo, pt, AF.Identity, [pk], [vk])
                jg = (t0 // 128) + j
                DMA("sp", VV[g * 4:(g + 1) * 4, :, jg, :].rearrange("h p d -> p h d"),
                    vo.rearrange("p (h d) -> p h d", h=4), [vk], [])
            if not fox:
                return
            bfv = P[0:4, BFC:BFC + 1]
            for tc in range(2):
                tok = t0 + tc * 512
                ci = tok // 512
                pt, pk = psm()
                for c in range(16):
                    MM(pt[0:4], wv[:, c, 512:516], actT[:, c, tc * 512:(tc + 1) * 512], c == 0, c == 15, [wk, "actT"], [pk])
                e_, ek = fx.next()
                ACT(e_, pt[0:4], AF.Exp, [pk, "ppt"], [ek], bias=bfv)
                s_, sk = fx.next()
                ACT(s_, e_, AF.Ln, [ek, "cf"], [sk], bias=c_one[0:4])
                lf, lfk = fx.next()
                STT(lf, pt[0:4], bfv, s_, ALU.add, ALU.subtract, [pk, sk, "ppt"], [lfk])
                cur, prev = fcarry[ci % 2], fcarry[(ci + 1) % 2]
                init = 0.0 if ci == 0 else prev[:, 511:512]
                SC.add("dve", partial(nc.vector.tensor_tensor_scan, out=cur, data0=ones4, data1=lf, initial=init,
                                      op0=ALU.mult, op1=ALU.add), [lfk, "ones4", ("fc", (ci + 1) % 2)], [("fc", ci % 2)])
                ck = ("fc", ci % 2)
                q6, q6k = fqk.next()
                ACT(q6[:, 0, :], cur, AF.Identity, [ck], [q6k])
                r1, r1k = fx.next()
                TT(r1, cur, q6[:, 0, :], ALU.subtract, [ck, q6k], [r1k])
                ACT(q6[:, 1, :], r1, AF.Identity, [r1k], [q6k])
                r2, r2k = fx.next()
                TT(r2, r1, q6[:, 1, :], ALU.subtract, [r1k, q6k], [r2k])
                ACT(q6[:, 2, :], r2, AF.Identity, [r2k], [q6k])
                ACT(q6[:, 3:6, :], q6[:, 0:3, :], AF.Identity, [q6k], [q6k], scale=-1.0)
                DMA("sp", FQ[:, :, tok:tok + 512], q6[:, 0:3, :], [q6k], [])
                DMA("sp", FK[:, :, tok:tok + 512], q6[:, 3:6, :], [q6k], [])

        fm_group(0, "norm", P[:, FOXQ:FOXQ + 1], c_ls128, QT, 0)
        fm_group(512, "norm", P[:, FOXK:FOXK + 1], None, KT, 0)
        v_group(1024, 516, 0, fox=True)
        fm_group(1540, "plain", None, None, QT, 4, scale=float(128.0 ** -0.5))
        fm_group(2052, "plain", None, None, KT, 4, scale=1.0)
        v_group(2564, 512, 1)
        fm_group(3076, "rope", P[:, MOBQ:MOBQ + 1], c_ls128, QT, 8)
        fm_group(3588, "rope", P[:, MOBK:MOBK + 1], None, KT, 8)
        v_group(4100, 512, 2)

        flush()
        wv, wk = wload(4612, 512)
        for tc in range(2):
            ss, ssk = psa()
            for c4 in range(4):
                pt, pk = psm()
                for c in range(16):
                    MM(pt, wv[:, c, c4 * 128:(c4 + 1) * 128], actT[:, c, tc * 512:(tc + 1) * 512], c == 0, c == 15,
                       [wk, "actT"], [pk])
                ACT(rawcq[:, c4, :], pt, AF.Identity, [pk], [("rawcq", c4)])
                sq, sqk = sqh.next()
                ACT(sq, pt, AF.Square, [pk], [sqk])
                MM(ss, ones_b, sq, c4 == 0, c4 == 3, [sqk, "cbf"], [ssk])
            rs, rk = rstd_of(ss, ssk, 512.0)
            for c4 in range(4):
                STT(cqn[:, c4, tc * 512:(tc + 1) * 512], rawcq[:, c4, :], P[:, CQ + c4:CQ + c4 + 1], rs, ALU.mult, ALU.mult,
                    [("rawcq", c4), rk, "ppt"], [("cqn", tc)])
        wv, wk = wload(5124, 320)
        for tc in range(2):
            ss, ssk = psa()
            for c2 in range(2):
                pt, pk = psm()
                for c in range(16):
                    MM(pt, wv[:, c, c2 * 128:(c2 + 1) * 128], actT[:, c, tc * 512:(tc + 1) * 512], c == 0, c == 15,
                       [wk, "actT"], [pk])
                ACT(rawcq[:, c2, :], pt, AF.Identity, [pk], [("rawcq", c2)])
                sq, sqk = sqh.next()
                ACT(sq, pt, AF.Square, [pk], [sqk])
                MM(ss, ones_b, sq, c2 == 0, c2 == 1, [sqk, "cbf"], [ssk])
            rs, rk = rstd_of(ss, ssk, 256.0)
            for c2 in range(2):
                STT(ckvn[:, c2, tc * 512:(tc + 1) * 512], rawcq[:, c2, :], P[:, CKV + c2:CKV + c2 + 1], rs, ALU.mult, ALU.mult,
                    [("rawcq", c2), rk, "ppt"], [("ckvn", tc)])
            pt, pk = psm()
            for c in range(16):
                MM(pt[0:64], wv[:, c, 256:320], actT[:, c, tc * 512:(tc + 1) * 512], c == 0, c == 15, [wk, "actT"], [pk])
            ACT(krraw[:, tc * 512:(tc + 1) * 512], pt[0:64], AF.Identity, [pk], [("krraw", tc)])
        for tc in range(2):
            tok = t0 + tc * 512
            tsl = slice(tc * 512, (tc + 1) * 512)
            ACT(sqkr, krraw[:, tsl], AF.Square, [("krraw", tc)], ["sqkr"])
            csr = load_cs(64, tok, cosR_d, sinR_d)
            for h in range(4):
                pn, pnk = psm()
                for c4 in range(4):
                    MM(pn, wuq[:, c4, h * 192:h * 192 + 128], cqn[:, c4, tsl], c4 == 0, c4 == 3, ["wuq", ("cqn", tc)], [pnk])
                pr, prk = psm()
                for c4 in range(4):
                    MM(pr[0:64], wuq[:, c4, h * 192 + 128:h * 192 + 192], cqn[:, c4, tsl], c4 == 0, c4 == 3,
                       ["wuq", ("cqn", tc)], [prk])
                sq, sqk = sqh.next()
                ACT(sq, pn, AF.Square, [pnk], [sqk])
                sq2, sq2k = sqh.next()
                ACT(sq2[0:64], pr[0:64], AF.Square, [prk], [sq2k])
                ss, ssk = psa()
                MM(ss, ones_b, sq, True, False, [sqk, "cbf"], [ssk])
                MM(ss, ones_b[0:64, :], sq2[0:64], False, True, [sq2k, "cbf"], [ssk])
                rs, rk = rstd_of(ss, ssk, 192.0, c_ls192)
                ob, obk = outb.next()
                STT(ob, pn, P[:, MQN:MQN + 1], rs, ALU.mult, ALU.mult, [pnk, rk, "ppt"], [obk])
                DMA("sp", QT[12 + h, :, tok:tok + 512], ob, [obk], [])
                x_, xk = xn.next()
                STT(x_[0:64], pr[0:64], P[0:64, MQR:MQR + 1], rs[0:64], ALU.mult, ALU.mult, [prk, rk, "ppt"], [xk])
                run_gen(rope_out(x_[0:64], xk, 64, csr, r64T, QR[h, :, tok:tok + 512]))
                pn, pnk = psm()
                for c2 in range(2):
                    MM(pn, wukv[:, c2, h * 256:h * 256 + 128], ckvn[:, c2, tsl], c2 == 0, c2 == 1, ["wukv", ("ckvn", tc)], [pnk])
                sq, sqk = sqh.next()
                ACT(sq, pn, AF.Square, [pnk], [sqk])
                ss, ssk = psa()
                MM(ss, ones_b, sq, True, False, [sqk, "cbf"], [ssk])
                MM(ss, ones_b[0:64, :], sqkr, False, True, ["sqkr", "cbf"], [ssk])
                rs, rk = rstd_of(ss, ssk, 192.0, None)
                ob, obk = outb.next()
                STT(ob, pn, P[:, MKN:MKN + 1], rs, ALU.mult, ALU.mult, [pnk, rk, "ppt"], [obk])
                DMA("sp", KT[12 + h, :, tok:tok + 512], ob, [obk], [])
                x_, xk = xn.next()
                STT(x_[0:64], krraw[:, tsl], P[0:64, MKR:MKR + 1], rs[0:64], ALU.mult, ALU.mult, [("krraw", tc), rk, "ppt"], [xk])
                run_gen(rope_out(x_[0:64], xk, 64, csr, r64T, KR[h, :, tok:tok + 512]))
            for j in range(4):
                pt, pk = psm()
                jl = tc * 4 + j
                for c2 in range(2):
                    rhs = wukv[:, c2, :].rearrange("p (h x) -> p h x", h=4)[:, :, 128:256]
                    MM(pt, ckvn[:, c2, jl * 128:(jl + 1) * 128], rhs, c2 == 0, c2 == 1, ["wukv", ("ckvn", tc)], [pk])
                vo, vk = vout.next()
                ACT(vo, pt, AF.Identity, [pk], [vk])
                jg = (t0 // 128) + jl
                DMA("sp", VV[12:16, :, jg, :].rearrange("h p d -> p h d"), vo.rearrange("p (h d) -> p h d", h=4), [vk], [])

    def att_phase(l):
        A.top = mark
        SC.barrier()
        attm = A.alloc([128, 12, 512], BF16)
        esel = A.alloc([16, 16, 128], BF16)
        bmask = A.alloc([128, 16, 16], F32)
        DMA("sp", attm, attm_d.rearrange("p (a b) -> p a b", a=12), [], ["attm"])
        DMA("sp", esel, esel_d.rearrange("p (a b) -> p a b", a=16), [], ["esel"])
        DMA("sp", bmask, bmask_d.rearrange("p (a b) -> p a b", a=16), [], ["bmask"])
        Mc = [attm[:, j, :] for j in range(4)]
        Ms = [attm[:, 4 + j, :] for j in range(4)]
        M01 = [attm[:, 8 + j, :] for j in range(4)]
        NJ = S // 128
        qt = Rot("qt", [A.alloc([128, S], BF16) for _ in range(2)])
        kt = Rot("kt", [A.alloc([128, S], BF16) for _ in range(2)])
        vv = Rot("vv", [A.alloc([128, NJ, 128], BF16) for _ in range(2)])
        qrr = Rot("qr", [A.alloc([64, S], BF16) for _ in range(2)])
        krr = Rot("kr", [A.alloc([64, S], BF16) for _ in range(2)])
        kbr = Rot("kb", [A.alloc([6, S], BF16) for _ in range(2)])
        qbr = Rot("qb", [A.alloc([6, S], BF16) for _ in range(2)])
        mbT = A.alloc([16, S], BF16)
        pT = Rot("pT", [A.alloc([128, 512], BF16) for _ in range(5)])
        ef = Rot("ef", [A.alloc([128, 512], F32) for _ in range(3)])
        spb = Rot("spb", [A.alloc([128, 512], BF16) for _ in range(4)])
        spacc = Rot("spacc", [A.alloc([128, 512], BF16) for _ in range(3)])
        ost = Rot("ost", [A.alloc([128, 512], F32) for _ in range(2)])
        rl = Rot("rl", [A.alloc([128, 512], F32) for _ in range(2)])
        ksum = A.alloc([128, 16], F32)
        kmb = A.alloc([128, 16], BF16)
        gm = Rot("gm", [A.alloc([128, 16], F32) for _ in range(2)])
        m8 = Rot("m8", [A.alloc([128, 8], F32) for _ in range(2)])
        mbt = Rot("mbt", [A.alloc([128, 16], BF16) for _ in range(2)])
        sbank = Rot("ps", [0, 1, 6, 7])
        obank = Rot("ps", [2, 3])
        lbank = Rot("ps", [4, 5])
        zbank = Rot("ps", [0, 1])
        xbank = Rot("ps", [6, 7])

        def head_loads(hd):
            g, h = hd // 4, hd % 4
            d = {}
            d["q"] = qt.next()
            d["k"] = kt.next()
            d["v"] = vv.next()
            DMA("sp", d["q"][0], QT[hd], [], [d["q"][1]])
            DMA("sp", d["k"][0], KT[hd], [], [d["k"][1]])
            DMA("sp", d["v"][0], VV[hd], [], [d["v"][1]])
            if g == 3:
                d["qr"] = qrr.next()
                d["kr"] = krr.next()
                DMA("sp", d["qr"][0], QR[h], [], [d["qr"][1]])
                DMA("sp", d["kr"][0], KR[h], [], [d["kr"][1]])
            if g == 0:
                d["kb"] = kbr.next()
                d["qb"] = qbr.next()
                MEMSET(d["kb"][0], 1.0, [d["kb"][1]])
                MEMSET(d["qb"][0], 1.0, [d["qb"][1]])
                DMA("sp", d["kb"][0][0:3, :], FK[h], [], [d["kb"][1]])
                DMA("sp", d["qb"][0][3:6, :], FQ[h], [], [d["qb"][1]])
            return d

        nxt = head_loads(0)
        for hd in range(16):
            g, h = hd // 4, hd % 4
            ld = nxt
            if hd + 1 < 16:
                nxt = head_loads(hd + 1)
            (q_, qk_), (k_, kk_), (v_, vk_) = ld["q"], ld["k"], ld["v"]
            if g == 3:
                (qr, qrk), (kr, krk) = ld["qr"], ld["kr"]
            if g == 0:
                (kb, kbk), (qb, qbk) = ld["kb"], ld["qb"]
            if g == 2:
                SC.add("dve", partial(nc.vector.tensor_reduce, out=ksum[:, 0:S // 256],
                                      in_=k_.rearrange("p (n k) -> p n k", k=256), axis=AX.X, op=ALU.add), [kk_], ["ksum"])
                ACT(kmb[:, 0:S // 256], ksum[:, 0:S // 256], AF.Identity, ["ksum"], ["kmb"], scale=1.0 / 256.0)
                NBK = S // 256
                for j in range(NJ):
                    own = j // 2
                    gb, gk = lbank.next()
                    MM(ps[gb][:, 0:NBK], q_[:, j * 128:(j + 1) * 128], kmb[:, 0:NBK], True, True, [qk_, "kmb"], [("ps", gb)])
                    g_, gmk = gm.next()
                    if NBK < 16:
                        MEMSET(g_, -1e30, [gmk])
                    TT(g_[:, 0:NBK], ps[gb][:, 0:NBK], bmask[:, own, 0:NBK], ALU.add, [("ps", gb), "bmask"], [gmk])
                    m_, mk = m8.next()
                    SC.add("dve", partial(nc.vector.max, out=m_, in_=g_), [gmk], [mk])
                    b_, bk = mbt.next()
                    TS(b_, g_, m_[:, 2:3], 1.0, ALU.is_ge, ALU.subtract, [gmk, mk], [bk])
                    tb_, _ = lbank.next()
                    MM(ps[tb_][0:16, 0:128], b_, ident, True, True, [bk, "cbf"], [("ps", tb_)])
                    ACT(mbT[:, j * 128:(j + 1) * 128], ps[tb_][0:16, 0:128], AF.Identity, [("ps", tb_)], ["mbT"])

            if g != 1:
                LA = 2
                jobs = []
                for qc in range(NQC):
                    tiles = [(kt_i, None) for kt_i in range(4 * qc)] + [(4 * qc + j, j) for j in range(4)]
                    for ti, (kti, dj) in enumerate(tiles):
                        jobs.append((qc, ti, len(tiles), kti, dj))
                pend, cur = {}, {}
                for idx in range(len(jobs) + LA):
                    if idx < len(jobs):
                        qc, ti, ntl, kti, dj = jobs[idx]
                        qs = slice(qc * 512, (qc + 1) * 512)
                        ks = slice(kti * 128, (kti + 1) * 128)
                        sb_, _ = sbank.next()
                        sps, spk = ps[sb_], ("ps", sb_)
                        extra = []
                        if g == 0:
                            extra.append((kb[0:6, ks], qb[0:6, qs], [kbk, qbk], None))
                        if g == 3:
                            extra.append((kr[:, ks], qr[:, qs], [krk, qrk], None))
                        if g == 2:
                            n = kti // 2
                            if dj is None:
                                extra.append((esel[:, n, :], mbT[:, qs], ["esel", "mbT"], None))
                            elif dj < 2:
                                extra.append((esel[:, n, :], mbT[:, qc * 512 + 256:(qc + 1) * 512], ["esel", "mbT"], (256, 512)))
                        if dj is not None:
                            if g == 2 and dj < 2:
                                extra.append((ident, Mc[dj][:, 0:256], ["cbf", "attm"], (0, 256)))
                            else:
                                extra.append((ident, Mc[dj], ["cbf", "attm"], None))
                        MM(sps, k_[:, ks], q_[:, qs], True, len(extra) == 0, [kk_, qk_], [spk])
                        for ei, (lt, rh, rk_, cr) in enumerate(extra):
                            o_ = sps if cr is None else sps[:, cr[0]:cr[1]]
                            MM(o_, lt, rh, False, ei == len(extra) - 1, rk_, [spk])
                        p_, pk_ = pT.next()
                        ACT(p_, sps, AF.Exp, [spk, "cf"], [pk_], bias=c_m8)
                        pend[idx] = (p_, pk_)
                    j2 = idx - LA
                    if j2 >= 0:
                        qc, ti, ntl, kti, dj = jobs[j2]
                        qs = slice(qc * 512, (qc + 1) * 512)
                        if ti == 0:
                            ob_, _ = obank.next()
                            lb_, _ = lbank.next()
                            cur[qc] = (ps[ob_], ("ps", ob_), ps[lb_], ("ps", lb_))
                        ops_, opk, lps, lpk = cur[qc]
                        p_, pk_ = pend.pop(j2)
                        MM(ops_, v_[:, kti, :], p_, ti == 0, ti == ntl - 1, [vk_, pk_], [opk])
                        MM(lps, ones_b, p_, ti == 0, ti == ntl - 1, ["cbf", pk_], [lpk])
                        if ti == ntl - 1:
                            r_, rk2 = rl.next()
                            SC.add("dve", partial(nc.vector.reciprocal, out=r_, in_=lps), [lpk], [rk2])
                            o_, ok_ = ost.next()
                            TT(o_, ops_, r_, ALU.mult, [opk, rk2], [ok_])
                            DMA("sp", OT[hd, :, qs], o_, [ok_], [])
            else:
                jobs = []
                for qc in range(NQC):
                    tiles = [(4 * qc + j, j) for j in (3, 2, 1, 0)] + [(kti, None) for kti in range(4 * qc - 1, -1, -1)]
                    for ti, (kti, dj) in enumerate(tiles):
                        jobs.append((qc, ti, len(tiles), kti, dj))
                spend, wpend, cur = {}, {}, {}
                accst = [None, None]
                for idx in range(len(jobs) + 2):
                    if idx < len(jobs):
                        qc, ti, ntl, kti, dj = jobs[idx]
                        qs = slice(qc * 512, (qc + 1) * 512)
                        ks = slice(kti * 128, (kti + 1) * 128)
                        zb, _ = zbank.next()
                        zps, zk = ps[zb], ("ps", zb)
                        MM(zps, k_[:, ks], q_[:, qs], True, True, [kk_, qk_], [zk])
                        e_, ek = ef.next()
                        ACT(e_, zps, AF.Exp, [zk], [ek])
                        s_, sk = spb.next()
                        ACT(s_, e_, AF.Ln, [ek, "cf"], [sk], bias=c_one)
                        if dj is not None:
                            TT(s_, s_, M01[dj], ALU.mult, [sk, "attm"], [sk])
                        spend[idx] = (s_, sk)
                    j1 = idx - 1
                    if 0 <= j1 < len(jobs):
                        qc, ti, ntl, kti, dj = jobs[j1]
                        qs = slice(qc * 512, (qc + 1) * 512)
                        ks = slice(kti * 128, (kti + 1) * 128)
                        s_, sk = spend.pop(j1)
                        xb_, _ = xbank.next()
                        xps, xk = ps[xb_], ("ps", xb_)
                        MM(xps, k_[:, ks], q_[:, qs], True, False, [kk_, qk_], [xk])
                        MM(xps, negtri, s_, False, False, ["cbf", sk], [xk])
                        if ti > 0:
                            MM(xps, negones, accst[0], False, dj is None, ["cbf", accst[1]], [xk])
                        if dj is not None:
                            MM(xps, ident, Ms[dj], False, True, ["cbf", "attm"], [xk])
                        w_, wk_ = pT.next()
                        ACT(w_, xps, AF.Exp, [xk], [wk_])
                        wpend[j1] = (w_, wk_)
                        if ti == 0:
                            na, nk = spacc.next()
                            SC.add("dve", partial(nc.vector.tensor_copy, out=na, in_=s_), [sk], [nk])
                            accst = [na, nk]
                        elif ti < ntl - 1:
                            na, nk = spacc.next()
                            TT(na, accst[0], s_, ALU.add, [accst[1], sk], [nk])
                            accst = [na, nk]
                    j2 = idx - 2
                    if j2 >= 0:
                        qc, ti, ntl, kti, dj = jobs[j2]
                        qs = slice(qc * 512, (qc + 1) * 512)
                        if ti == 0:
                            ob_, _ = obank.next()
                            cur[qc] = (ps[ob_], ("ps", ob_))
                        ops_, opk = cur[qc]
                        w_, wk_ = wpend.pop(j2)
                        MM(ops_, v_[:, kti, :], w_, ti == 0, ti == ntl - 1, [vk_, wk_], [opk])
                        if ti == ntl - 1:
                            o_, ok_ = ost.next()
                            ACT(o_, ops_, AF.Identity, [opk], [ok_])
                            DMA("sp", OT[hd, :, qs], o_, [ok_], [])

    def n2_phase(l, xin, xout):
        A.top = mark
        P = ppt[:, l, :]
        SC.barrier()
        actTs = [A.alloc([128, 16, TBK], BF16) for _ in range(2)]
        wb = [A.alloc([128, 8192], BF16) for _ in range(2)]
        st = norm_stage(4)
        res = Rot("res", [A.alloc([128, 512], F32) for _ in range(5)])
        osb = Rot("osb", [A.alloc([128, 512], F32) for _ in range(3)])
        xin_v = xin.rearrange("(c p) t -> p c t", p=128)
        xout_v = xout.rearrange("(c p) t -> p c t", p=128)
        ot_v = OT.rearrange("c p t -> p c t")
        G4 = [(0, 4), (4, 8), (8, 12), (12, 16)]

        def pro_chunk(tb, i):
            norm_chunk(st, ot_v, tb * TBK + i * 256, 256, i * 256, actTs[tb % 2], P[:, GM:GM + 16], G4, 512.0,
                       akey=("actT", tb % 2))

        order = [(tb, gq, tc, n) for tb in range(NTB) for gq in range(4) for tc in range(2) for n in range(4)]
        resq = {}
        issued = [0]

        def ensure(k):
            while issued[0] <= min(k, len(order) - 1):
                tb_, gq_, tc_, n_ = order[issued[0]]
                r_, rk = res.next()
                tok_ = tb_ * TBK + tc_ * 512
                DMA("sp", r_, xin_v[:, gq_ * 4 + n_, tok_:tok_ + 512], [], [rk])
                resq[issued[0]] = (r_, rk)
                issued[0] += 1

        for i in range(4):
            pro_chunk(0, i)
        idx = 0
        for tb in range(NTB):
            t0 = tb * TBK
            actT, ak = actTs[tb % 2], ("actT", tb % 2)
            for gq in range(4):
                i = wctr[0] % 2
                wctr[0] += 1
                wv = wb[i][:, 0:16 * 512].rearrange("p (c n) -> p c n", c=16)
                wk = ("wb", i)
                DMA("pool", wv, w_out[l, :, gq * 512:(gq + 1) * 512].rearrange("(c p) n -> p c n", p=128), [], [wk])
                for tc in range(2):
                    tok = t0 + tc * 512
                    for n in range(4):
                        cc = gq * 4 + n
                        ensure(idx + 2)
                        r_, rk = resq.pop(idx)
                        idx += 1
                        pt, pk = psm()
                        for c in range(16):
                            MM(pt, wv[:, c, n * 128:(n + 1) * 128], actT[:, c, tc * 512:(tc + 1) * 512], c == 0, c == 15,
                               [wk, ak], [pk])
                        o_, ok_ = osb.next()
                        TT(o_, pt, r_, ALU.add, [pk, rk], [ok_])
                        DMA("sp", xout_v[:, cc, tok:tok + 512], o_, [ok_], [])
                if tb + 1 < NTB:
                    pro_chunk(tb + 1, gq)

    def ffn_block(l, tb, xin, xout):
        t0 = tb * TBK
        A.top = mark
        P = ppt[:, l, :]
        h2T = A.alloc([128, 16, TBK + 2], BF16)
        wb = [A.alloc([128, 8192], BF16) for _ in range(2)]
        gT = A.alloc([128, 44, TBK], BF16)
        m2 = A.top
        A.top = m2 - 44 * TBK * 2
        SC.barrier()
        xin_v = xin.rearrange("(c p) t -> p c t", p=128)
        chunks = [(t0 + i * 256, 256, 2 + i * 256) for i in range(4)]
        if tb == 0:
            MEMSET(h2T[:, :, 0:2], 0.0, ["actT"])
        else:
            chunks = [(t0 - 2, 2, 0)] + chunks
        norm_pro(xin_v, chunks, h2T, P[:, G2:G2 + 16], [(0, 16)], float(D))
        SC.barrier()
        A.top = m2
        U = [Rot(f"U{p}", [A.alloc([128, TBK + 2], F32) for _ in range(2)]) for p in range(2)]
        av = Rot("av", [A.alloc([128, TBK], F32) for _ in range(1)])
        ag = Rot("ag", [A.alloc([128, TBK], F32) for _ in range(1)])
        sg = Rot("sg", [A.alloc([128, TBK], F32) for _ in range(1)])
        res = Rot("res", [A.alloc([128, 512], F32) for _ in range(2)])
        osb = Rot("osb", [A.alloc([128, 512], F32) for _ in range(2)])
        CS = [(0, 342), (342, 342), (684, 342)]
        for jg in range(22):
            i = wctr[0] % 2
            wctr[0] += 1
            wv = wb[i][:, 0:16 * 512].rearrange("p (c n) -> p c n", c=16)
            wk = ("wb", i)
            DMA("pool", wv[:, :, 0:256], w_up[l, :, jg * 256:(jg + 1) * 256].rearrange("(c p) n -> p c n", p=128), [], [wk])
            DMA("pool", wv[:, :, 256:512], w_up[l, :, DFF + jg * 256:DFF + (jg + 1) * 256].rearrange("(c p) n -> p c n", p=128),
                [], [wk])
            for jj in range(2):
                j = jg * 2 + jj
                acc = []
                for part in range(2):
                    u_, uk = U[part].next()
                    for (cs, cn) in CS:
                        pt, pk = psm()
                        for c in range(16):
                            MM(pt[:, 0:cn], wv[:, c, part * 256 + jj * 128:part * 256 + (jj + 1) * 128], h2T[:, c, cs:cs + cn],
                               c == 0, c == 15, [wk, "actT"], [pk])
                        ACT(u_[:, cs:cs + cn], pt[:, 0:cn], AF.Identity, [pk], [uk])
                    ch = part * 44 + j
                    a_, ak = (ag if part == 0 else av).next()
                    TS(a_, u_[:, 2:TBK + 2], P[:, CW + 2 * 88 + ch:CW + 2 * 88 + ch + 1], P[:, CB + ch:CB + ch + 1], ALU.mult, ALU.add,
                       [uk, "ppt"], [ak])
                    STT(a_, u_[:, 1:TBK + 1], P[:, CW + 88 + ch:CW + 88 + ch + 1], a_, ALU.mult, ALU.add, [uk, ak, "ppt"], [ak])
                    STT(a_, u_[:, 0:TBK], P[:, CW + ch:CW + ch + 1], a_, ALU.mult, ALU.add, [uk, ak, "ppt"], [ak])
                    acc.append((a_, ak))
                s_, sk = sg.next()
                ACT(s_, acc[0][0], AF.Silu, [acc[0][1]], [sk])
                TT(gT[:, j, :], s_, acc[1][0], ALU.mult, [sk, acc[1][1]], [("gT", j)])
        xout_v = xout.rearrange("(c p) t -> p c t", p=128)
        gkeys = [("gT", j) for j in range(44)]
        for cc in range(16):
            i = wctr[0] % 2
            wctr[0] += 1
            wv = wb[i][:, 0:44 * 128].rearrange("p (c n) -> p c n", c=44)
            wk = ("wb", i)
            DMA("pool", wv, w_down[l, :, cc * 128:(cc + 1) * 128].rearrange("(c p) n -> p c n", p=128), [], [wk])
            for tc in range(2):
                tok = t0 + tc * 512
                r_, rk = res.next()
                DMA("sp", r_, xin_v[:, cc, tok:tok + 512], [], [rk])
                pt, pk = psm()
                for jx in range(44):
                    MM(pt, wv[:, jx, :], gT[:, jx, tc * 512:(tc + 1) * 512], jx == 0, jx == 43, [wk, gkeys[jx]], [pk])
                o_, ok_ = osb.next()
                TT(o_, pt, r_, ALU.add, [pk, rk], [ok_])
                DMA("sp", xout_v[:, cc, tok:tok + 512], o_, [ok_], [])

    for l in range(L):
        xin = xT if l == 0 else xA
        for tb in range(NTB):
            n1_block(l, tb, xin)
        att_phase(l)
        n2_phase(l, xin, xB)
        xo = outT if l == L - 1 else xA
        for tb in range(NTB):
            ffn_block(l, tb, xB, xo)
    SC.barrier()
    nw = SC.emit()
    es.close()
    return nc, len(SC.ops), nw


def host_consts(S):
    bf = ml_dtypes.bfloat16
    cb = np.zeros((128, 768), np.float32)
    cb[:, 0:128] = np.eye(128)
    cb[:, 128:256] = 1.0
    r = np.zeros((128, 128), np.float32)
    for i in range(64):
        r[i + 64, i] = -1.0
        r[i, i + 64] = 1.0
    cb[:, 256:384] = r
    r = np.zeros((128, 128), np.float32)
    for i in range(32):
        r[i + 32, i] = -1.0
        r[i, i + 32] = 1.0
    cb[:, 384:512] = r
    jj, ss = np.meshgrid(np.arange(128), np.arange(128), indexing="ij")
    cb[:, 512:640] = np.where(jj >= ss, -1.0, 0.0)
    cb[:, 640:768] = -1.0
    k = np.arange(128)[:, None]
    q = np.arange(512)[None, :]
    am = np.zeros((128, 12, 512), np.float32)
    for j in range(4):
        am[:, j, :] = np.where(128 * j + k <= q, 0.0, NEG)
        am[:, 4 + j, :] = np.where(128 * j + k < q, 0.0, NEG)
        am[:, 8 + j, :] = np.where(128 * j + k < q, 1.0, 0.0)
    es_ = np.zeros((16, 16, 128), np.float32)
    for n in range(16):
        es_[n, n, :] = -NEG
    bm = np.zeros((128, 16, 16), np.float32)
    for own in range(16):
        bm[:, own, own:] = -1e30
    def rope(dim):
        inv = (10000.0 ** (-np.arange(0, dim, 2, dtype=np.float32) / np.float32(dim))).astype(np.float32)
        ang = np.arange(S, dtype=np.float32)[:, None] * inv[None, :]
        c, s_ = np.cos(ang).astype(np.float32).T, np.sin(ang).astype(np.float32).T
        return np.ascontiguousarray(np.concatenate([c, c], 0)), np.ascontiguousarray(np.concatenate([s_, s_], 0))
    cC, sC = rope(128)
    cR, sR = rope(64)
    return dict(cbf=cb.astype(bf), attm=am.reshape(128, 6144).astype(bf), esel=es_.reshape(16, 2048).astype(bf),
                bmask=bm.reshape(128, 256), cosC=cC, sinC=sC, cosR=cR, sinR=sR)


def pack_params(inp, L):
    pp = np.zeros((128, L, NP), np.float32)
    def cm(a):
        return a.reshape(L, -1, 128).transpose(2, 0, 1)
    pp[:, :, G1:G1 + 16] = cm(inp["attn_norm"])
    pp[:, :, G2:G2 + 16] = cm(inp["ffn_norm"])
    pp[:, :, GM:GM + 16] = cm(inp["mix_out_norm"])
    cw = inp["conv_w"].reshape(L, 3, 88, 128).transpose(3, 0, 1, 2)
    pp[:, :, CW:CW + 264] = cw.reshape(128, L, 264)
    pp[:, :, CB:CB + 88] = cm(inp["conv_b"])
    pp[:, :, FOXQ] = inp["fox_q_norm"].T
    pp[:, :, FOXK] = inp["fox_k_norm"].T
    pp[:, :, MOBQ] = inp["moba_q_norm"].T
    pp[:, :, MOBK] = inp["moba_k_norm"].T
    pp[:, :, CQ:CQ + 4] = cm(inp["mla_cq_norm"])
    pp[:, :, CKV:CKV + 2] = cm(inp["mla_ckv_norm"])
    pp[:, :, MQN] = inp["mla_q_norm"][:, 0:128].T
    pp[0:64, :, MQR] = inp["mla_q_norm"][:, 128:192].T
    pp[:, :, MKN] = inp["mla_k_norm"][:, 0:128].T
    pp[0:64, :, MKR] = inp["mla_k_norm"][:, 128:192].T
    pp[0:4, :, BFC] = inp["b_forget"].T
    return pp


_CACHE = {}


def run(inputs, debug=False, ncores=None):
    inp = {k: np.asarray(v) for k, v in inputs.items()}
    x = inp["x"]
    B, S, _ = x.shape
    L = inp["w_in"].shape[0]
    key = (S, L, debug)
    if key not in _CACHE:
        _CACHE[key] = build(S, L, debug)
    nc = _CACHE[key][0]
    common = host_consts(S)
    common["pp"] = pack_params(inp, L)
    for k in ("w_in", "w_uq", "w_ukv", "w_out", "w_up", "w_down"):
        common[k] = np.ascontiguousarray(inp[k], dtype=np.float32)
    ncores = ncores or B
    in_maps = []
    for c in range(ncores):
        m = dict(common)
        m["xT"] = np.ascontiguousarray(x[c % B].T)
        in_maps.append(m)
    res = run_bass_kernel_spmd(nc, in_maps, core_ids=list(range(ncores)))
    out = np.stack([np.ascontiguousarray(res.results[b]["outT"].T) for b in range(B)], 0)
    return out.astype(np.float32), res


def kernel(**inputs):
    out, _ = run(inputs)
    return out
```

```python
import contextlib
from functools import partial

import ml_dtypes
import numpy as np

import concourse.bass as bass
import concourse.mybir as mybir
from concourse.bass_utils import run_bass_kernel_spmd

F32 = mybir.dt.float32
BF16 = mybir.dt.bfloat16
AF = mybir.ActivationFunctionType
ALU = mybir.AluOpType
AX = mybir.AxisListType

D = 2048
INW = 5444
DFF = 5632
EPS = 1e-6
TBK = 1024
G1, G2, GM, CW, CB = 0, 16, 32, 48, 312
FOXQ, FOXK, MOBQ, MOBK, CQ, CKV, MQN, MQR, MKN, MKR, BFC = 400, 401, 402, 403, 404, 408, 410, 411, 412, 413, 414
NP = 416
NEG = -30000.0


class Op:
    __slots__ = ("eng", "fn", "deps", "signal", "seq", "dma", "sem", "target", "semkey")


class Sched:
    COMPUTE = ("pe", "act", "dve", "pool")

    def __init__(self, nc, es, nslots=None):
        self.nc = nc
        self.engs = {"pe": nc.tensor, "act": nc.scalar, "dve": nc.vector, "pool": nc.gpsimd, "sp": nc.sync}
        self.ops = []
        self.lastw = {}
        self.readers = {}
        self.last_op = {}
        self.esem = {e: es.enter_context(nc.semaphore(f"s_{e}")) for e in self.COMPUTE}
        nslots = nslots or {"sp": 16, "pool": 8}
        self.dsem = {q: [es.enter_context(nc.semaphore(f"d_{q}{i}")) for i in range(n)] for q, n in nslots.items()}
        self.dcount = {q: [0] * n for q, n in nslots.items()}
        self.dlast = {q: [None] * n for q, n in nslots.items()}
        self.dnext = {q: 0 for q in nslots}
        self.outstanding = []

    def add(self, eng, fn, reads=(), writes=(), dma=False):
        op = Op()
        op.eng, op.fn, op.dma, op.signal, op.seq = eng, fn, dma, False, None
        deps = {}

        def dep(d, war):
            if d is None:
                return
            if (not d.dma) and (not dma) and d.eng == eng:
                if eng == "pe" or war:
                    return
            deps[id(d)] = d

        for k in reads:
            dep(self.lastw.get(k), False)
        for k in writes:
            dep(self.lastw.get(k), False)
            rr = self.readers.get(k)
            if rr:
                for r in rr.values():
                    dep(r, True)
        if dma:
            q = eng
            i = self.dnext[q]
            self.dnext[q] = (i + 1) % len(self.dsem[q])
            prev = self.dlast[q][i]
            if prev is not None:
                deps[id(prev)] = prev
            self.dcount[q][i] += 16
            op.sem, op.target, op.semkey = self.dsem[q][i], self.dcount[q][i], (q, i)
            self.dlast[q][i] = op
            self.outstanding.append(op)
        else:
            self.last_op[eng] = op
        rk = id(op) if dma else eng
        for k in reads:
            self.readers.setdefault(k, {})[rk] = op
        for k in writes:
            self.lastw[k] = op
            self.readers[k] = {}
        op.deps = list(deps.values())
        for d in op.deps:
            if not d.dma:
                d.signal = True
        self.ops.append(op)
        return op

    def barrier(self):
        targets = [op for op in self.last_op.values()] + self.outstanding
        for e in ("pe", "act", "dve", "pool", "sp"):
            b = Op()
            b.eng, b.fn, b.dma, b.signal, b.seq = e, None, False, False, None
            b.deps = [t for t in targets if t.dma or t.eng != e]
            for d in b.deps:
                if not d.dma:
                    d.signal = True
            self.ops.append(b)
        self.outstanding = []
        self.lastw = {}
        self.readers = {}

    def emit(self):
        known = {e: {} for e in self.engs}
        seq = {e: 0 for e in self.COMPUTE}
        nwait = 0
        for op in self.ops:
            E = self.engs[op.eng]
            kn = known[op.eng]
            need = {}
            for d in op.deps:
                if d.dma:
                    key, sem, val = d.semkey, d.sem, d.target
                else:
                    key, sem, val = d.eng, self.esem[d.eng], d.seq
                if kn.get(key, 0) < val and need.get(key, (None, 0))[1] < val:
                    need[key] = (sem, val)
            for key, (sem, val) in need.items():
                E.wait_ge(sem, val)
                kn[key] = val
                nwait += 1
            if op.fn is None:
                continue
            inst = op.fn()
            if op.dma:
                inst.then_inc(op.sem, 16)
            elif op.signal:
                seq[op.eng] += 1
                op.seq = seq[op.eng]
                inst.then_inc(self.esem[op.eng], 1)
        return nwait


class Arena:
    def __init__(self, t, nbytes):
        self.t, self.n, self.top = t, nbytes, 0

    def alloc(self, shape, dt):
        esz = 4 if dt == F32 else 2
        n = int(np.prod(shape[1:]))
        nb = n * esz
        off = self.top
        self.top += (nb + 63) // 64 * 64
        assert self.top <= self.n, f"arena overflow {self.top} > {self.n}"
        ap = self.t[0:shape[0], off // 2:(off + nb) // 2]
        if dt == F32:
            ap = ap.bitcast(F32)
        if len(shape) == 3:
            ap = ap.rearrange("p (a b) -> p a b", a=shape[1])
        return ap


class Rot:
    def __init__(self, name, bufs):
        self.name, self.bufs, self.i = name, bufs, 0

    def next(self):
        i = self.i % len(self.bufs)
        self.i += 1
        return self.bufs[i], (self.name, i)


def build(S, L, debug=False):
    assert S % TBK == 0
    NTB = S // TBK
    NQC = S // 512
    nc = bass.Bass("TRN2", target_bir_lowering=False)
    es = contextlib.ExitStack()

    def din(name, shape, dt=F32):
        return nc.dram_tensor(name, list(shape), dt, kind="ExternalInput").ap()

    def dscr(name, shape, dt):
        return nc.dram_tensor(name, list(shape), dt, kind=("ExternalOutput" if debug else "Internal")).ap()

    xT = din("xT", [D, S])
    w_in = din("w_in", [L, D, INW])
    w_uq = din("w_uq", [L, 512, 768])
    w_ukv = din("w_ukv", [L, 256, 1024])
    w_out = din("w_out", [L, D, D])
    w_up = din("w_up", [L, D, 2 * DFF])
    w_down = din("w_down", [L, DFF, D])
    pp_d = din("pp", [128, L, NP])
    cbf_d = din("cbf", [128, 768], BF16)
    attm_d = din("attm", [128, 6144], BF16)
    esel_d = din("esel", [16, 2048], BF16)
    bmask_d = din("bmask", [128, 256])
    cosC_d, sinC_d = din("cosC", [128, S]), din("sinC", [128, S])
    cosR_d, sinR_d = din("cosR", [64, S]), din("sinR", [64, S])
    outT = nc.dram_tensor("outT", [D, S], F32, kind="ExternalOutput").ap()

    xA, xB = dscr("xA", [D, S], F32), dscr("xB", [D, S], F32)
    QT, KT = dscr("QT", [16, 128, S], BF16), dscr("KT", [16, 128, S], BF16)
    QR, KR = dscr("QR", [4, 64, S], BF16), dscr("KR", [4, 64, S], BF16)
    VV = dscr("VV", [16, 128, S // 128, 128], BF16)
    FQ, FK = dscr("FQ", [4, 3, S], BF16), dscr("FK", [4, 3, S], BF16)
    OT = dscr("OT", [16, 128, S], F32)

    ARENA_BYTES = 206 * 1024
    arena_t = es.enter_context(nc.sbuf_tensor("arena", [128, ARENA_BYTES // 2], BF16))
    A = Arena(arena_t, ARENA_BYTES)
    ps = [es.enter_context(nc.psum_tensor(f"ps{i}", [128, 512], F32))[:] for i in range(8)]
    SC = Sched(nc, es)
    engs = SC.engs

    def MM(out, lhsT, rhs, start, stop, reads, writes):
        SC.add("pe", partial(nc.tensor.matmul, out, lhsT=lhsT, rhs=rhs, start=start, stop=stop), reads, writes)

    def ACT(out, in_, func, reads, writes, bias=None, scale=None):
        kw = {}
        if bias is not None:
            kw["bias"] = bias
        if scale is not None:
            kw["scale"] = scale
        SC.add("act", partial(nc.scalar.activation, out=out, in_=in_, func=func, **kw), reads, writes)

    def STT(out, in0, scalar, in1, op0, op1, reads, writes):
        SC.add("dve", partial(nc.vector.scalar_tensor_tensor, out=out, in0=in0, scalar=scalar, in1=in1, op0=op0, op1=op1),
               reads, writes)

    def TT(out, in0, in1, op, reads, writes, eng="dve"):
        SC.add(eng, partial(engs[eng].tensor_tensor, out=out, in0=in0, in1=in1, op=op), reads, writes)

    def TS(out, in0, s1, s2, op0, op1, reads, writes):
        SC.add("dve", partial(nc.vector.tensor_scalar, out=out, in0=in0, scalar1=s1, scalar2=s2, op0=op0, op1=op1),
               reads, writes)

    def DMA(q, out, in_, reads, writes):
        SC.add(q, partial(engs[q].dma_start, out=out, in_=in_), reads, writes, dma=True)

    def MEMSET(ap, val, writes, eng="dve"):
        SC.add(eng, partial(engs[eng].memset, ap, val), (), writes)

    cbf = A.alloc([128, 768], BF16)
    ident, ones_b, r128T, r64T, negtri, negones = (cbf[:, i * 128:(i + 1) * 128] for i in range(6))
    ppt = A.alloc([128, L, NP], F32)
    cf = A.alloc([128, 8], F32)
    DMA("sp", cbf, cbf_d, [], ["cbf"])
    DMA("sp", ppt, pp_d, [], ["ppt"])
    cvals = [EPS, -8.0, float(np.log(128.0 ** -0.5)), float(np.log(192.0 ** -0.5)), 1.0, 0.0]
    for i, v in enumerate(cvals):
        MEMSET(cf[:, i:i + 1], v, ["cf"])
    c_eps, c_m8, c_ls128, c_ls192, c_one = (cf[:, i:i + 1] for i in range(5))
    fcarry = [A.alloc([4, 512], F32) for _ in range(2)]
    ones4 = A.alloc([4, 512], F32)
    MEMSET(ones4, 1.0, ["ones4"])
    mark = A.top
    SC.barrier()

    wctr = [0]
    pmr = Rot("ps", [0, 1, 2, 3])
    par = Rot("ps", [4, 5, 6, 7])

    def psm():
        b, _ = pmr.next()
        return ps[b], ("ps", b)

    def psa():
        b, _ = par.next()
        return ps[b], ("ps", b)

    def norm_stage(ngroups):
        return dict(XS=[A.alloc([128, 16, 256], F32) for _ in range(2)], SQ=A.alloc([128, 16, 256], BF16),
                    LNV=[A.alloc([128, 256], F32) for _ in range(ngroups)], RSTD=[A.alloc([128, 256], F32) for _ in range(ngroups)],
                    i=0)

    def norm_chunk(st, src_v, ts, n, dc, dst, gains, groups, Dg, akey="actT"):
        b = st["i"] % 2
        st["i"] += 1
        xs, SQ, LNV, RSTD = st["XS"][b], st["SQ"], st["LNV"], st["RSTD"]
        DMA("sp", xs[:, :, 0:n], src_v[:, :, ts:ts + n], [], [("xs", b)])
        ACT(SQ[:, :, 0:n], xs[:, :, 0:n], AF.Square, [("xs", b)], ["sq"])
        for gi, (c0, c1) in enumerate(groups):
            pb = 4 + gi
            for c in range(c0, c1):
                MM(ps[pb][:, 0:n], ones_b, SQ[:, c, 0:n], c == c0, c == c1 - 1, ["sq", "cbf"], [("ps", pb)])
            ACT(LNV[gi][:, 0:n], ps[pb][:, 0:n], AF.Ln, [("ps", pb), "cf"], [("lnv", gi)], bias=c_eps, scale=1.0 / Dg)
            ACT(RSTD[gi][:, 0:n], LNV[gi][:, 0:n], AF.Exp, [("lnv", gi)], [("rstd", gi)], scale=-0.5)
            for c in range(c0, c1):
                STT(dst[:, c, dc:dc + n], xs[:, c, 0:n], gains[:, c:c + 1], RSTD[gi][:, 0:n], ALU.mult, ALU.mult,
                    [("xs", b), ("rstd", gi), "ppt"], [akey])

    def norm_pro(src_v, chunks, dst, gains, groups, Dg):
        st = norm_stage(len(groups))
        for (ts, n, dc) in chunks:
            norm_chunk(st, src_v, ts, n, dc, dst, gains, groups, Dg)

    def n1_block(l, tb, xin):
        t0 = tb * TBK
        A.top = mark
        P = ppt[:, l, :]
        actT = A.alloc([128, 16, TBK], BF16)
        wb = [A.alloc([128, 12288], BF16) for _ in range(2)]
        wuq = A.alloc([128, 4, 768], BF16)
        wukv = A.alloc([128, 2, 1024], BF16)
        m2 = A.top
        SC.barrier()
        DMA("pool", wuq, w_uq[l].rearrange("(c p) n -> p c n", p=128), [], ["wuq"])
        DMA("pool", wukv, w_ukv[l].rearrange("(c p) n -> p c n", p=128), [], ["wukv"])
        xin_v = xin.rearrange("(c p) t -> p c t", p=128)
        norm_pro(xin_v, [(t0 + i * 256, 256, i * 256) for i in range(4)], actT, P[:, G1:G1 + 16], [(0, 16)], float(D))
        SC.barrier()
        A.top = m2
        def rot(name, shape, dt, n=2):
            return Rot(name, [A.alloc(shape, dt) for _ in range(n)])
        sqh = rot("sqh", [128, 512], BF16)
        lnv = rot("lnv", [128, 512], F32)
        rstd = rot("rstd", [128, 512], F32)
        xn = rot("xn", [128, 512], F32)
        xnb = rot("xnb", [128, 512], BF16)
        t1 = rot("t1", [128, 512], F32)
        t2 = rot("t2", [128, 512], F32)
        cosb = rot("cosb", [128, 512], F32)
        sinb = rot("sinb", [128, 512], F32)
        outb = rot("outb", [128, 512], BF16, 3)
        vout = rot("vout", [128, 512], BF16)
        fx = rot("fx", [4, 512], F32, 3)
        fqk = rot("fqk", [4, 6, 512], BF16)
        rawcq = A.alloc([128, 4, 512], F32)
        cqn = A.alloc([128, 4, TBK], BF16)
        ckvn = A.alloc([128, 2, TBK], BF16)
        krraw = A.alloc([64, TBK], F32)
        sqkr = A.alloc([64, 512], BF16)

        def wload(col0, ncols):
            i = wctr[0] % 2
            wctr[0] += 1
            v = wb[i][:, 0:16 * ncols].rearrange("p (c n) -> p c n", c=16)
            DMA("pool", v, w_in[l, :, col0:col0 + ncols].rearrange("(c p) n -> p c n", p=128), [], [("wb", i)])
            return v, ("wb", i)

        def rstd_of(ssp, ssk, Dn, lnscale=None):
            lv, lk = lnv.next()
            ACT(lv, ssp, AF.Ln, [ssk, "cf"], [lk], bias=c_eps, scale=1.0 / Dn)
            rs, rk = rstd.next()
            if lnscale is None:
                ACT(rs, lv, AF.Exp, [lk], [rk], scale=-0.5)
            else:
                ACT(rs, lv, AF.Exp, [lk, "cf"], [rk], scale=-0.5, bias=lnscale)
            return rs, rk

        def load_cs(np_, tok, cos_d, sin_d):
            cb, cbk = cosb.next()
            sb, sbk = sinb.next()
            DMA("sp", cb[0:np_], cos_d[:, tok:tok + 512], [], [cbk])
            DMA("sp", sb[0:np_], sin_d[:, tok:tok + 512], [], [sbk])
            return (cb, cbk, sb, sbk)

        def rope_out(xn_t, xn_k, np_, cs, rT, dst):
            cb, cbk, sb, sbk = cs
            xb, xbk = xnb.next()
            ACT(xb[0:np_], xn_t, AF.Identity, [xn_k], [xbk])
            a1, a1k = t1.next()
            TT(a1[0:np_], xn_t, cb[0:np_], ALU.mult, [xn_k, cbk], [a1k])
            yield
            rp, rpk = psa()
            MM(rp[0:np_], rT[0:np_, 0:np_], xb[0:np_], True, True, [xbk, "cbf"], [rpk])
            a2, a2k = t2.next()
            TT(a2[0:np_], rp[0:np_], sb[0:np_], ALU.mult, [rpk, sbk], [a2k])
            ob, obk = outb.next()
            TT(ob[0:np_], a1[0:np_], a2[0:np_], ALU.add, [a1k, a2k], [obk])
            DMA("sp", dst, ob[0:np_], [obk], [])

        def head_fm(pst, psk, cs, kind, gain, lnscale, dst, scale=None):
            if kind == "plain":
                ob, obk = outb.next()
                ACT(ob, pst, AF.Identity, [psk], [obk], scale=scale)
                DMA("sp", dst, ob, [obk], [])
                return
            sq, sqk = sqh.next()
            ACT(sq, pst, AF.Square, [psk], [sqk])
            yield
            sp_, spk = psa()
            MM(sp_, ones_b, sq, True, True, [sqk, "cbf"], [spk])
            rs, rk = rstd_of(sp_, spk, 128.0, lnscale)
            if kind == "norm":
                ob, obk = outb.next()
                STT(ob, pst, gain, rs, ALU.mult, ALU.mult, [psk, rk, "ppt"], [obk])
                DMA("sp", dst, ob, [obk], [])
            else:
                x_, xk = xn.next()
                STT(x_, pst, gain, rs, ALU.mult, ALU.mult, [psk, rk, "ppt"], [xk])
                yield from rope_out(x_, xk, 128, cs, r128T, dst)

        active = []

        def advance(newgen=None):
            for g_ in active[:]:
                try:
                    next(g_)
                except StopIteration:
                    active.remove(g_)
            if newgen is not None:
                try:
                    next(newgen)
                    active.append(newgen)
                except StopIteration:
                    pass

        def flush():
            while active:
                advance()

        def run_gen(gen):
            for _ in gen:
                pass

        def fm_group(col0, kind, gain, lnscale, dstT, hd0, scale=None):
            wv, wk = wload(col0, 512)
            css = [load_cs(128, t0 + tc * 512, cosC_d, sinC_d) if kind == "rope" else None for tc in range(2)]
            for tc in range(2):
                tok = t0 + tc * 512
                for h in range(4):
                    pt, pk = psm()
                    for c in range(16):
                        MM(pt, wv[:, c, h * 128:(h + 1) * 128], actT[:, c, tc * 512:(tc + 1) * 512], c == 0, c == 15,
                           [wk, "actT"], [pk])
                    advance(head_fm(pt, pk, css[tc], kind, gain, lnscale, dstT[hd0 + h, :, tok:tok + 512], scale))
            flush()

        def v_group(col0, ncols, g, fox=False):
            wv, wk = wload(col0, ncols)
            for j in range(TBK // 128):
                pt, pk = psm()
                for c in range(16):
                    MM(pt, actT[:, c, j * 128:(j + 1) * 128], wv[:, c, 0:512], c == 0, c == 15, [wk, "actT"], [pk])
                vo, vk = vout.next()
                ACT(vo, pt, AF.Identity, [pk], [vk])
                jg = (t0 // 128) + j
                DMA("sp", VV[g * 4:(g + 1) * 4, :, jg, :].rearrange("h p d -> p h d"),
                    vo.rearrange("p (h d) -> p h d", h=4), [vk], [])
            if not fox:
                return
            bfv = P[0:4, BFC:BFC + 1]
            for tc in range(2):
                tok = t0 + tc * 512
                ci = tok // 512
                pt, pk = psm()
                for c in range(16):
                    MM(pt[0:4], wv[:, c, 512:516], actT[:, c, tc * 512:(tc + 1) * 512], c == 0, c == 15, [wk, "actT"], [pk])
                e_, ek = fx.next()
                ACT(e_, pt[0:4], AF.Exp, [pk, "ppt"], [ek], bias=bfv)
                s_, sk = fx.next()
                ACT(s_, e_, AF.Ln, [ek, "cf"], [sk], bias=c_one[0:4])
                lf, lfk = fx.next()
                STT(lf, pt[0:4], bfv, s_, ALU.add, ALU.subtract, [pk, sk, "ppt"], [lfk])
                cur, prev = fcarry[ci % 2], fcarry[(ci + 1) % 2]
                init = 0.0 if ci == 0 else prev[:, 511:512]
                SC.add("dve", partial(nc.vector.tensor_tensor_scan, out=cur, data0=ones4, data1=lf, initial=init,
                                      op0=ALU.mult, op1=ALU.add), [lfk, "ones4", ("fc", (ci + 1) % 2)], [("fc", ci % 2)])
                ck = ("fc", ci % 2)
                q6, q6k = fqk.next()
                ACT(q6[:, 0, :], cur, AF.Identity, [ck], [q6k])
                r1, r1k = fx.next()
                TT(r1, cur, q6[:, 0, :], ALU.subtract, [ck, q6k], [r1k])
                ACT(q6[:, 1, :], r1, AF.Identity, [r1k], [q6k])
                r2, r2k = fx.next()
                TT(r2, r1, q6[:, 1, :], ALU.subtract, [r1k, q6k], [r2k])
                ACT(q6[:, 2, :], r2, AF.Identity, [r2k], [q6k])
                ACT(q6[:, 3:6, :], q6[:, 0:3, :], AF.Identity, [q6k], [q6k], scale=-1.0)
                DMA("sp", FQ[:, :, tok:tok + 512], q6[:, 0:3, :], [q6k], [])
                DMA("sp", FK[:, :, tok:tok + 512], q6[:, 3:6, :], [q6k], [])

        fm_group(0, "norm", P[:, FOXQ:FOXQ + 1], c_ls128, QT, 0)
        fm_group(512, "norm", P[:, FOXK:FOXK + 1], None, KT, 0)
        v_group(1024, 516, 0, fox=True)
        fm_group(1540, "plain", None, None, QT, 4, scale=float(128.0 ** -0.5))
        fm_group(2052, "plain", None, None, KT, 4, scale=1.0)
        v_group(2564, 512, 1)
        fm_group(3076, "rope", P[:, MOBQ:MOBQ + 1], c_ls128, QT, 8)
        fm_group(3588, "rope", P[:, MOBK:MOBK + 1], None, KT, 8)
        v_group(4100, 512, 2)

        flush()
        wv, wk = wload(4612, 512)
        for tc in range(2):
            ss, ssk = psa()
            for c4 in range(4):
                pt, pk = psm()
                for c in range(16):
                    MM(pt, wv[:, c, c4 * 128:(c4 + 1) * 128], actT[:, c, tc * 512:(tc + 1) * 512], c == 0, c == 15,
                       [wk, "actT"], [pk])
                ACT(rawcq[:, c4, :], pt, AF.Identity, [pk], [("rawcq", c4)])
                sq, sqk = sqh.next()
                ACT(sq, pt, AF.Square, [pk], [sqk])
                MM(ss, ones_b, sq, c4 == 0, c4 == 3, [sqk, "cbf"], [ssk])
            rs, rk = rstd_of(ss, ssk, 512.0)
            for c4 in range(4):
                STT(cqn[:, c4, tc * 512:(tc + 1) * 512], rawcq[:, c4, :], P[:, CQ + c4:CQ + c4 + 1], rs, ALU.mult, ALU.mult,
                    [("rawcq", c4), rk, "ppt"], [("cqn", tc)])
        wv, wk = wload(5124, 320)
        for tc in range(2):
            ss, ssk = psa()
            for c2 in range(2):
                pt, pk = psm()
                for c in range(16):
                    MM(pt, wv[:, c, c2 * 128:(c2 + 1) * 128], actT[:, c, tc * 512:(tc + 1) * 512], c == 0, c == 15,
                       [wk, "actT"], [pk])
                ACT(rawcq[:, c2, :], pt, AF.Identity, [pk], [("rawcq", c2)])
                sq, sqk = sqh.next()
                ACT(sq, pt, AF.Square, [pk], [sqk])
                MM(ss, ones_b, sq, c2 == 0, c2 == 1, [sqk, "cbf"], [ssk])
            rs, rk = rstd_of(ss, ssk, 256.0)
            for c2 in range(2):
                STT(ckvn[:, c2, tc * 512:(tc + 1) * 512], rawcq[:, c2, :], P[:, CKV + c2:CKV + c2 + 1], rs, ALU.mult, ALU.mult,
                    [("rawcq", c2), rk, "ppt"], [("ckvn", tc)])
            pt, pk = psm()
            for c in range(16):
                MM(pt[0:64], wv[:, c, 256:320], actT[:, c, tc * 512:(tc + 1) * 512], c == 0, c == 15, [wk, "actT"], [pk])
            ACT(krraw[:, tc * 512:(tc + 1) * 512], pt[0:64], AF.Identity, [pk], [("krraw", tc)])
        for tc in range(2):
            tok = t0 + tc * 512
            tsl = slice(tc * 512, (tc + 1) * 512)
            ACT(sqkr, krraw[:, tsl], AF.Square, [("krraw", tc)], ["sqkr"])
            csr = load_cs(64, tok, cosR_d, sinR_d)
            for h in range(4):
                pn, pnk = psm()
                for c4 in range(4):
                    MM(pn, wuq[:, c4, h * 192:h * 192 + 128], cqn[:, c4, tsl], c4 == 0, c4 == 3, ["wuq", ("cqn", tc)], [pnk])
                pr, prk = psm()
                for c4 in range(4):
                    MM(pr[0:64], wuq[:, c4, h * 192 + 128:h * 192 + 192], cqn[:, c4, tsl], c4 == 0, c4 == 3,
                       ["wuq", ("cqn", tc)], [prk])
                sq, sqk = sqh.next()
                ACT(sq, pn, AF.Square, [pnk], [sqk])
                sq2, sq2k = sqh.next()
                ACT(sq2[0:64], pr[0:64], AF.Square, [prk], [sq2k])
                ss, ssk = psa()
                MM(ss, ones_b, sq, True, False, [sqk, "cbf"], [ssk])
                MM(ss, ones_b[0:64, :], sq2[0:64], False, True, [sq2k, "cbf"], [ssk])
                rs, rk = rstd_of(ss, ssk, 192.0, c_ls192)
                ob, obk = outb.next()
                STT(ob, pn, P[:, MQN:MQN + 1], rs, ALU.mult, ALU.mult, [pnk, rk, "ppt"], [obk])
                DMA("sp", QT[12 + h, :, tok:tok + 512], ob, [obk], [])
                x_, xk = xn.next()
                STT(x_[0:64], pr[0:64], P[0:64, MQR:MQR + 1], rs[0:64], ALU.mult, ALU.mult, [prk, rk, "ppt"], [xk])
                run_gen(rope_out(x_[0:64], xk, 64, csr, r64T, QR[h, :, tok:tok + 512]))
                pn, pnk = psm()
                for c2 in range(2):
                    MM(pn, wukv[:, c2, h * 256:h * 256 + 128], ckvn[:, c2, tsl], c2 == 0, c2 == 1, ["wukv", ("ckvn", tc)], [pnk])
                sq, sqk = sqh.next()
                ACT(sq, pn, AF.Square, [pnk], [sqk])
                ss, ssk = psa()
                MM(ss, ones_b, sq, True, False, [sqk, "cbf"], [ssk])
                MM(ss, ones_b[0:64, :], sqkr, False, True, ["sqkr", "cbf"], [ssk])
                rs, rk = rstd_of(ss, ssk, 192.0, None)
                ob, obk = outb.next()
                STT(ob, pn, P[:, MKN:MKN + 1], rs, ALU.mult, ALU.mult, [pnk, rk, "ppt"], [obk])
                DMA("sp", KT[12 + h, :, tok:tok + 512], ob, [obk], [])
                x_, xk = xn.next()
                STT(x_[0:64], krraw[:, tsl], P[0:64, MKR:MKR + 1], rs[0:64], ALU.mult, ALU.mult, [("krraw", tc), rk, "ppt"], [xk])
                run_gen(rope_out(x_[0:64], xk, 64, csr, r64T, KR[h, :, tok:tok + 512]))
            for j in range(4):
                pt, pk = psm()
                jl = tc * 4 + j
                for c2 in range(2):
                    rhs = wukv[:, c2, :].rearrange("p (h x) -> p h x", h=4)[:, :, 128:256]
                    MM(pt, ckvn[:, c2, jl * 128:(jl + 1) * 128], rhs, c2 == 0, c2 == 1, ["wukv", ("ckvn", tc)], [pk])
                vo, vk = vout.next()
                ACT(vo, pt, AF.Identity, [pk], [vk])
                jg = (t0 // 128) + jl
                DMA("sp", VV[12:16, :, jg, :].rearrange("h p d -> p h d"), vo.rearrange("p (h d) -> p h d", h=4), [vk], [])

    def att_phase(l):
        A.top = mark
        SC.barrier()
        attm = A.alloc([128, 12, 512], BF16)
        esel = A.alloc([16, 16, 128], BF16)
        bmask = A.alloc([128, 16, 16], F32)
        DMA("sp", attm, attm_d.rearrange("p (a b) -> p a b", a=12), [], ["attm"])
        DMA("sp", esel, esel_d.rearrange("p (a b) -> p a b", a=16), [], ["esel"])
        DMA("sp", bmask, bmask_d.rearrange("p (a b) -> p a b", a=16), [], ["bmask"])
        Mc = [attm[:, j, :] for j in range(4)]
        Ms = [attm[:, 4 + j, :] for j in range(4)]
        M01 = [attm[:, 8 + j, :] for j in range(4)]
        NJ = S // 128
        qt = Rot("qt", [A.alloc([128, S], BF16) for _ in range(2)])
        kt = Rot("kt", [A.alloc([128, S], BF16) for _ in range(2)])
        vv = Rot("vv", [A.alloc([128, NJ, 128], BF16) for _ in range(2)])
        qrr = Rot("qr", [A.alloc([64, S], BF16) for _ in range(2)])
        krr = Rot("kr", [A.alloc([64, S], BF16) for _ in range(2)])
        kbr = Rot("kb", [A.alloc([6, S], BF16) for _ in range(2)])
        qbr = Rot("qb", [A.alloc([6, S], BF16) for _ in range(2)])
        mbT = A.alloc([16, S], BF16)
        pT = Rot("pT", [A.alloc([128, 512], BF16) for _ in range(5)])
        ef = Rot("ef", [A.alloc([128, 512], F32) for _ in range(3)])
        spb = Rot("spb", [A.alloc([128, 512], BF16) for _ in range(4)])
        spacc = Rot("spacc", [A.alloc([128, 512], BF16) for _ in range(3)])
        ost = Rot("ost", [A.alloc([128, 512], F32) for _ in range(2)])
        rl = Rot("rl", [A.alloc([128, 512], F32) for _ in range(2)])
        ksum = A.alloc([128, 16], F32)
        kmb = A.alloc([128, 16], BF16)
        gm = Rot("gm", [A.alloc([128, 16], F32) for _ in range(2)])
        m8 = Rot("m8", [A.alloc([128, 8], F32) for _ in range(2)])
        mbt = Rot("mbt", [A.alloc([128, 16], BF16) for _ in range(2)])
        sbank = Rot("ps", [0, 1, 6, 7])
        obank = Rot("ps", [2, 3])
        lbank = Rot("ps", [4, 5])
        zbank = Rot("ps", [0, 1])
        xbank = Rot("ps", [6, 7])

        def head_loads(hd):
            g, h = hd // 4, hd % 4
            d = {}
            d["q"] = qt.next()
            d["k"] = kt.next()
            d["v"] = vv.next()
            DMA("sp", d["q"][0], QT[hd], [], [d["q"][1]])
            DMA("sp", d["k"][0], KT[hd], [], [d["k"][1]])
            DMA("sp", d["v"][0], VV[hd], [], [d["v"][1]])
            if g == 3:
                d["qr"] = qrr.next()
                d["kr"] = krr.next()
                DMA("sp", d["qr"][0], QR[h], [], [d["qr"][1]])
                DMA("sp", d["kr"][0], KR[h], [], [d["kr"][1]])
            if g == 0:
                d["kb"] = kbr.next()
                d["qb"] = qbr.next()
                MEMSET(d["kb"][0], 1.0, [d["kb"][1]])
                MEMSET(d["qb"][0], 1.0, [d["qb"][1]])
                DMA("sp", d["kb"][0][0:3, :], FK[h], [], [d["kb"][1]])
                DMA("sp", d["qb"][0][3:6, :], FQ[h], [], [d["qb"][1]])
            return d

        nxt = head_loads(0)
        for hd in range(16):
            g, h = hd // 4, hd % 4
            ld = nxt
            if hd + 1 < 16:
                nxt = head_loads(hd + 1)
            (q_, qk_), (k_, kk_), (v_, vk_) = ld["q"], ld["k"], ld["v"]
            if g == 3:
                (qr, qrk), (kr, krk) = ld["qr"], ld["kr"]
            if g == 0:
                (kb, kbk), (qb, qbk) = ld["kb"], ld["qb"]
            if g == 2:
                SC.add("dve", partial(nc.vector.tensor_reduce, out=ksum[:, 0:S // 256],
                                      in_=k_.rearrange("p (n k) -> p n k", k=256), axis=AX.X, op=ALU.add), [kk_], ["ksum"])
                ACT(kmb[:, 0:S // 256], ksum[:, 0:S // 256], AF.Identity, ["ksum"], ["kmb"], scale=1.0 / 256.0)
                NBK = S // 256
                for j in range(NJ):
                    own = j // 2
                    gb, gk = lbank.next()
                    MM(ps[gb][:, 0:NBK], q_[:, j * 128:(j + 1) * 128], kmb[:, 0:NBK], True, True, [qk_, "kmb"], [("ps", gb)])
                    g_, gmk = gm.next()
                    if NBK < 16:
                        MEMSET(g_, -1e30, [gmk])
                    TT(g_[:, 0:NBK], ps[gb][:, 0:NBK], bmask[:, own, 0:NBK], ALU.add, [("ps", gb), "bmask"], [gmk])
                    m_, mk = m8.next()
                    SC.add("dve", partial(nc.vector.max, out=m_, in_=g_), [gmk], [mk])
                    b_, bk = mbt.next()
                    TS(b_, g_, m_[:, 2:3], 1.0, ALU.is_ge, ALU.subtract, [gmk, mk], [bk])
                    tb_, _ = lbank.next()
                    MM(ps[tb_][0:16, 0:128], b_, ident, True, True, [bk, "cbf"], [("ps", tb_)])
                    ACT(mbT[:, j * 128:(j + 1) * 128], ps[tb_][0:16, 0:128], AF.Identity, [("ps", tb_)], ["mbT"])

            if g != 1:
                LA = 2
                jobs = []
                for qc in range(NQC):
                    tiles = [(kt_i, None) for kt_i in range(4 * qc)] + [(4 * qc + j, j) for j in range(4)]
                    for ti, (kti, dj) in enumerate(tiles):
                        jobs.append((qc, ti, len(tiles), kti, dj))
                pend, cur = {}, {}
                for idx in range(len(jobs) + LA):
                    if idx < len(jobs):
                        qc, ti, ntl, kti, dj = jobs[idx]
                        qs = slice(qc * 512, (qc + 1) * 512)
                        ks = slice(kti * 128, (kti + 1) * 128)
                        sb_, _ = sbank.next()
                        c0 = 128 * dj if (dj is not None and not (g == 2 and dj < 2)) else 0
                        qs = slice(qc * 512 + c0, (qc + 1) * 512)
                        sps, spk = ps[sb_][:, c0:512], ("ps", sb_)
                        extra = []
                        if g == 0:
                            extra.append((kb[0:6, ks], qb[0:6, qs], [kbk, qbk], None))
                        if g == 3:
                            extra.append((kr[:, ks], qr[:, qs], [krk, qrk], None))
                        if g == 2:
                            n = kti // 2
                            if dj is None:
                                extra.append((esel[:, n, :], mbT[:, qs], ["esel", "mbT"], None))
                            elif dj < 2:
                                extra.append((esel[:, n, :], mbT[:, qc * 512 + 256:(qc + 1) * 512], ["esel", "mbT"], (256, 512)))
                        if dj is not None:
                            if g == 2 and dj < 2:
                                extra.append((ident, Mc[dj][:, 0:256], ["cbf", "attm"], (0, 256)))
                            else:
                                extra.append((ident, Mc[dj][:, c0:512], ["cbf", "attm"], None))
                        MM(sps, k_[:, ks], q_[:, qs], True, len(extra) == 0, [kk_, qk_], [spk])
                        for ei, (lt, rh, rk_, cr) in enumerate(extra):
                            o_ = sps if cr is None else sps[:, cr[0]:cr[1]]
                            MM(o_, lt, rh, False, ei == len(extra) - 1, rk_, [spk])
                        p_, pk_ = pT.next()
                        ACT(p_[:, c0:512], sps, AF.Exp, [spk, "cf"], [pk_], bias=c_m8)
                        pend[idx] = (p_, pk_, c0)
                    j2 = idx - LA
                    if j2 >= 0:
                        qc, ti, ntl, kti, dj = jobs[j2]
                        qs = slice(qc * 512, (qc + 1) * 512)
                        if ti == 0:
                            ob_, _ = obank.next()
                            lb_, _ = lbank.next()
                            cur[qc] = (ps[ob_], ("ps", ob_), ps[lb_], ("ps", lb_))
                        ops_, opk, lps, lpk = cur[qc]
                        p_, pk_, c0 = pend.pop(j2)
                        MM(ops_[:, c0:512], v_[:, kti, :], p_[:, c0:512], ti == 0, ti == ntl - 1, [vk_, pk_], [opk])
                        MM(lps[:, c0:512], ones_b, p_[:, c0:512], ti == 0, ti == ntl - 1, ["cbf", pk_], [lpk])
                        if ti == ntl - 1:
                            r_, rk2 = rl.next()
                            SC.add("dve", partial(nc.vector.reciprocal, out=r_, in_=lps), [lpk], [rk2])
                            o_, ok_ = ost.next()
                            TT(o_, ops_, r_, ALU.mult, [opk, rk2], [ok_])
                            DMA("sp", OT[hd, :, qs], o_, [ok_], [])
            else:
                jobs = []
                for qc in range(NQC):
                    tiles = [(4 * qc + j, j) for j in (3, 2, 1, 0)] + [(kti, None) for kti in range(4 * qc - 1, -1, -1)]
                    for ti, (kti, dj) in enumerate(tiles):
                        jobs.append((qc, ti, len(tiles), kti, dj))
                spend, wpend, cur = {}, {}, {}
                accst = [None, None]
                for idx in range(len(jobs) + 2):
                    if idx < len(jobs):
                        qc, ti, ntl, kti, dj = jobs[idx]
                        qs = slice(qc * 512, (qc + 1) * 512)
                        ks = slice(kti * 128, (kti + 1) * 128)
                        zb, _ = zbank.next()
                        zps, zk = ps[zb], ("ps", zb)
                        MM(zps, k_[:, ks], q_[:, qs], True, True, [kk_, qk_], [zk])
                        e_, ek = ef.next()
                        ACT(e_, zps, AF.Exp, [zk], [ek])
                        s_, sk = spb.next()
                        ACT(s_, e_, AF.Ln, [ek, "cf"], [sk], bias=c_one)
                        if dj is not None:
                            TT(s_, s_, M01[dj], ALU.mult, [sk, "attm"], [sk])
                        spend[idx] = (s_, sk)
                    j1 = idx - 1
                    if 0 <= j1 < len(jobs):
                        qc, ti, ntl, kti, dj = jobs[j1]
                        qs = slice(qc * 512, (qc + 1) * 512)
                        ks = slice(kti * 128, (kti + 1) * 128)
                        s_, sk = spend.pop(j1)
                        xb_, _ = xbank.next()
                        xps, xk = ps[xb_], ("ps", xb_)
                        MM(xps, k_[:, ks], q_[:, qs], True, False, [kk_, qk_], [xk])
                        MM(xps, negtri, s_, False, False, ["cbf", sk], [xk])
                        if ti > 0:
                            MM(xps, negones, accst[0], False, dj is None, ["cbf", accst[1]], [xk])
                        if dj is not None:
                            MM(xps, ident, Ms[dj], False, True, ["cbf", "attm"], [xk])
                        w_, wk_ = pT.next()
                        ACT(w_, xps, AF.Exp, [xk], [wk_])
                        wpend[j1] = (w_, wk_)
                        if ti == 0:
                            na, nk = spacc.next()
                            SC.add("dve", partial(nc.vector.tensor_copy, out=na, in_=s_), [sk], [nk])
                            accst = [na, nk]
                        elif ti < ntl - 1:
                            na, nk = spacc.next()
                            TT(na, accst[0], s_, ALU.add, [accst[1], sk], [nk])
                            accst = [na, nk]
                    j2 = idx - 2
                    if j2 >= 0:
                        qc, ti, ntl, kti, dj = jobs[j2]
                        qs = slice(qc * 512, (qc + 1) * 512)
                        if ti == 0:
                            ob_, _ = obank.next()
                            cur[qc] = (ps[ob_], ("ps", ob_))
                        ops_, opk = cur[qc]
                        w_, wk_ = wpend.pop(j2)
                        MM(ops_, v_[:, kti, :], w_, ti == 0, ti == ntl - 1, [vk_, wk_], [opk])
                        if ti == ntl - 1:
                            o_, ok_ = ost.next()
                            ACT(o_, ops_, AF.Identity, [opk], [ok_])
                            DMA("sp", OT[hd, :, qs], o_, [ok_], [])

    def n2_phase(l, xin, xout):
        A.top = mark
        P = ppt[:, l, :]
        SC.barrier()
        actTs = [A.alloc([128, 16, TBK], BF16) for _ in range(2)]
        wb = [A.alloc([128, 8192], BF16) for _ in range(2)]
        st = norm_stage(4)
        res = Rot("res", [A.alloc([128, 512], F32) for _ in range(5)])
        osb = Rot("osb", [A.alloc([128, 512], F32) for _ in range(3)])
        xin_v = xin.rearrange("(c p) t -> p c t", p=128)
        xout_v = xout.rearrange("(c p) t -> p c t", p=128)
        ot_v = OT.rearrange("c p t -> p c t")
        G4 = [(0, 4), (4, 8), (8, 12), (12, 16)]

        def pro_chunk(tb, i):
            norm_chunk(st, ot_v, tb * TBK + i * 256, 256, i * 256, actTs[tb % 2], P[:, GM:GM + 16], G4, 512.0,
                       akey=("actT", tb % 2))

        order = [(tb, gq, tc, n) for tb in range(NTB) for gq in range(4) for tc in range(2) for n in range(4)]
        resq = {}
        issued = [0]

        def ensure(k):
            while issued[0] <= min(k, len(order) - 1):
                tb_, gq_, tc_, n_ = order[issued[0]]
                r_, rk = res.next()
                tok_ = tb_ * TBK + tc_ * 512
                DMA("sp", r_, xin_v[:, gq_ * 4 + n_, tok_:tok_ + 512], [], [rk])
                resq[issued[0]] = (r_, rk)
                issued[0] += 1

        for i in range(4):
            pro_chunk(0, i)
        idx = 0
        for tb in range(NTB):
            t0 = tb * TBK
            actT, ak = actTs[tb % 2], ("actT", tb % 2)
            for gq in range(4):
                i = wctr[0] % 2
                wctr[0] += 1
                wv = wb[i][:, 0:16 * 512].rearrange("p (c n) -> p c n", c=16)
                wk = ("wb", i)
                DMA("pool", wv, w_out[l, :, gq * 512:(gq + 1) * 512].rearrange("(c p) n -> p c n", p=128), [], [wk])
                for tc in range(2):
                    tok = t0 + tc * 512
                    for n in range(4):
                        cc = gq * 4 + n
                        ensure(idx + 2)
                        r_, rk = resq.pop(idx)
                        idx += 1
                        pt, pk = psm()
                        for c in range(16):
                            MM(pt, wv[:, c, n * 128:(n + 1) * 128], actT[:, c, tc * 512:(tc + 1) * 512], c == 0, c == 15,
                               [wk, ak], [pk])
                        o_, ok_ = osb.next()
                        TT(o_, pt, r_, ALU.add, [pk, rk], [ok_])
                        DMA("sp", xout_v[:, cc, tok:tok + 512], o_, [ok_], [])
                if tb + 1 < NTB:
                    pro_chunk(tb + 1, gq)

    def ffn_block(l, tb, xin, xout):
        t0 = tb * TBK
        A.top = mark
        P = ppt[:, l, :]
        h2T = A.alloc([128, 16, TBK + 2], BF16)
        wb = [A.alloc([128, 8192], BF16) for _ in range(2)]
        gT = A.alloc([128, 44, TBK], BF16)
        m2 = A.top
        A.top = m2 - 44 * TBK * 2
        SC.barrier()
        xin_v = xin.rearrange("(c p) t -> p c t", p=128)
        chunks = [(t0 + i * 256, 256, 2 + i * 256) for i in range(4)]
        if tb == 0:
            MEMSET(h2T[:, :, 0:2], 0.0, ["actT"])
        else:
            chunks = [(t0 - 2, 2, 0)] + chunks
        norm_pro(xin_v, chunks, h2T, P[:, G2:G2 + 16], [(0, 16)], float(D))
        SC.barrier()
        A.top = m2
        U = [Rot(f"U{p}", [A.alloc([128, TBK + 2], F32) for _ in range(2)]) for p in range(2)]
        av = Rot("av", [A.alloc([128, TBK], F32) for _ in range(1)])
        ag = Rot("ag", [A.alloc([128, TBK], F32) for _ in range(1)])
        sg = Rot("sg", [A.alloc([128, TBK], F32) for _ in range(1)])
        res = Rot("res", [A.alloc([128, 512], F32) for _ in range(2)])
        osb = Rot("osb", [A.alloc([128, 512], F32) for _ in range(2)])
        CS = [(0, 342), (342, 342), (684, 342)]
        for jg in range(22):
            i = wctr[0] % 2
            wctr[0] += 1
            wv = wb[i][:, 0:16 * 512].rearrange("p (c n) -> p c n", c=16)
            wk = ("wb", i)
            DMA("pool", wv[:, :, 0:256], w_up[l, :, jg * 256:(jg + 1) * 256].rearrange("(c p) n -> p c n", p=128), [], [wk])
            DMA("pool", wv[:, :, 256:512], w_up[l, :, DFF + jg * 256:DFF + (jg + 1) * 256].rearrange("(c p) n -> p c n", p=128),
                [], [wk])
            for jj in range(2):
                j = jg * 2 + jj
                acc = []
                for part in range(2):
                    u_, uk = U[part].next()
                    for (cs, cn) in CS:
                        pt, pk = psm()
                        for c in range(16):
                            MM(pt[:, 0:cn], wv[:, c, part * 256 + jj * 128:part * 256 + (jj + 1) * 128], h2T[:, c, cs:cs + cn],
                               c == 0, c == 15, [wk, "actT"], [pk])
                        ACT(u_[:, cs:cs + cn], pt[:, 0:cn], AF.Identity, [pk], [uk])
                    ch = part * 44 + j
                    a_, ak = (ag if part == 0 else av).next()
                    TS(a_, u_[:, 2:TBK + 2], P[:, CW + 2 * 88 + ch:CW + 2 * 88 + ch + 1], P[:, CB + ch:CB + ch + 1], ALU.mult, ALU.add,
                       [uk, "ppt"], [ak])
                    STT(a_, u_[:, 1:TBK + 1], P[:, CW + 88 + ch:CW + 88 + ch + 1], a_, ALU.mult, ALU.add, [uk, ak, "ppt"], [ak])
                    STT(a_, u_[:, 0:TBK], P[:, CW + ch:CW + ch + 1], a_, ALU.mult, ALU.add, [uk, ak, "ppt"], [ak])
                    acc.append((a_, ak))
                s_, sk = sg.next()
                ACT(s_, acc[0][0], AF.Silu, [acc[0][1]], [sk])
                TT(gT[:, j, :], s_, acc[1][0], ALU.mult, [sk, acc[1][1]], [("gT", j)])
        xout_v = xout.rearrange("(c p) t -> p c t", p=128)
        gkeys = [("gT", j) for j in range(44)]
        for cc in range(16):
            i = wctr[0] % 2
            wctr[0] += 1
            wv = wb[i][:, 0:44 * 128].rearrange("p (c n) -> p c n", c=44)
            wk = ("wb", i)
            DMA("pool", wv, w_down[l, :, cc * 128:(cc + 1) * 128].rearrange("(c p) n -> p c n", p=128), [], [wk])
            for tc in range(2):
                tok = t0 + tc * 512
                r_, rk = res.next()
                DMA("sp", r_, xin_v[:, cc, tok:tok + 512], [], [rk])
                pt, pk = psm()
                for jx in range(44):
                    MM(pt, wv[:, jx, :], gT[:, jx, tc * 512:(tc + 1) * 512], jx == 0, jx == 43, [wk, gkeys[jx]], [pk])
                o_, ok_ = osb.next()
                TT(o_, pt, r_, ALU.add, [pk, rk], [ok_])
                DMA("sp", xout_v[:, cc, tok:tok + 512], o_, [ok_], [])

    for l in range(L):
        xin = xT if l == 0 else xA
        for tb in range(NTB):
            n1_block(l, tb, xin)
        att_phase(l)
        n2_phase(l, xin, xB)
        xo = outT if l == L - 1 else xA
        for tb in range(NTB):
            ffn_block(l, tb, xB, xo)
    SC.barrier()
    nw = SC.emit()
    es.close()
    return nc, len(SC.ops), nw


def host_consts(S):
    bf = ml_dtypes.bfloat16
    cb = np.zeros((128, 768), np.float32)
    cb[:, 0:128] = np.eye(128)
    cb[:, 128:256] = 1.0
    r = np.zeros((128, 128), np.float32)
    for i in range(64):
        r[i + 64, i] = -1.0
        r[i, i + 64] = 1.0
    cb[:, 256:384] = r
    r = np.zeros((128, 128), np.float32)
    for i in range(32):
        r[i + 32, i] = -1.0
        r[i, i + 32] = 1.0
    cb[:, 384:512] = r
    jj, ss = np.meshgrid(np.arange(128), np.arange(128), indexing="ij")
    cb[:, 512:640] = np.where(jj >= ss, -1.0, 0.0)
    cb[:, 640:768] = -1.0
    k = np.arange(128)[:, None]
    q = np.arange(512)[None, :]
    am = np.zeros((128, 12, 512), np.float32)
    for j in range(4):
        am[:, j, :] = np.where(128 * j + k <= q, 0.0, NEG)
        am[:, 4 + j, :] = np.where(128 * j + k < q, 0.0, NEG)
        am[:, 8 + j, :] = np.where(128 * j + k < q, 1.0, 0.0)
    es_ = np.zeros((16, 16, 128), np.float32)
    for n in range(16):
        es_[n, n, :] = -NEG
    bm = np.zeros((128, 16, 16), np.float32)
    for own in range(16):
        bm[:, own, own:] = -1e30
    def rope(dim):
        inv = (10000.0 ** (-np.arange(0, dim, 2, dtype=np.float32) / np.float32(dim))).astype(np.float32)
        ang = np.arange(S, dtype=np.float32)[:, None] * inv[None, :]
        c, s_ = np.cos(ang).astype(np.float32).T, np.sin(ang).astype(np.float32).T
        return np.ascontiguousarray(np.concatenate([c, c], 0)), np.ascontiguousarray(np.concatenate([s_, s_], 0))
    cC, sC = rope(128)
    cR, sR = rope(64)
    return dict(cbf=cb.astype(bf), attm=am.reshape(128, 6144).astype(bf), esel=es_.reshape(16, 2048).astype(bf),
                bmask=bm.reshape(128, 256), cosC=cC, sinC=sC, cosR=cR, sinR=sR)


def pack_params(inp, L):
    pp = np.zeros((128, L, NP), np.float32)
    def cm(a):
        return a.reshape(L, -1, 128).transpose(2, 0, 1)
    pp[:, :, G1:G1 + 16] = cm(inp["attn_norm"])
    pp[:, :, G2:G2 + 16] = cm(inp["ffn_norm"])
    pp[:, :, GM:GM + 16] = cm(inp["mix_out_norm"])
    cw = inp["conv_w"].reshape(L, 3, 88, 128).transpose(3, 0, 1, 2)
    pp[:, :, CW:CW + 264] = cw.reshape(128, L, 264)
    pp[:, :, CB:CB + 88] = cm(inp["conv_b"])
    pp[:, :, FOXQ] = inp["fox_q_norm"].T
    pp[:, :, FOXK] = inp["fox_k_norm"].T
    pp[:, :, MOBQ] = inp["moba_q_norm"].T
    pp[:, :, MOBK] = inp["moba_k_norm"].T
    pp[:, :, CQ:CQ + 4] = cm(inp["mla_cq_norm"])
    pp[:, :, CKV:CKV + 2] = cm(inp["mla_ckv_norm"])
    pp[:, :, MQN] = inp["mla_q_norm"][:, 0:128].T
    pp[0:64, :, MQR] = inp["mla_q_norm"][:, 128:192].T
    pp[:, :, MKN] = inp["mla_k_norm"][:, 0:128].T
    pp[0:64, :, MKR] = inp["mla_k_norm"][:, 128:192].T
    pp[0:4, :, BFC] = inp["b_forget"].T
    return pp


_CACHE = {}


def run(inputs, debug=False, ncores=None):
    inp = {k: np.asarray(v) for k, v in inputs.items()}
    x = inp["x"]
    B, S, _ = x.shape
    L = inp["w_in"].shape[0]
    key = (S, L, debug)
    if key not in _CACHE:
        _CACHE[key] = build(S, L, debug)
    nc = _CACHE[key][0]
    common = host_consts(S)
    common["pp"] = pack_params(inp, L)
    for k in ("w_in", "w_uq", "w_ukv", "w_out", "w_up", "w_down"):
        common[k] = np.ascontiguousarray(inp[k], dtype=np.float32)
    ncores = ncores or B
    in_maps = []
    for c in range(ncores):
        m = dict(common)
        m["xT"] = np.ascontiguousarray(x[c % B].T)
        in_maps.append(m)
    res = run_bass_kernel_spmd(nc, in_maps, core_ids=list(range(ncores)))
    out = np.stack([np.ascontiguousarray(res.results[b]["outT"].T) for b in range(B)], 0)
    return out.astype(np.float32), res


def kernel(**inputs):
    out, _ = run(inputs)
    return out
```
